# Optimizing a Trainium2 kernel written in Bass

```python
import jax
import jax.numpy as jnp
from jax import lax
import numpy as np


D_MODEL = 1024
BATCH = 8
SEQ = 8192
DEPTH = 2

CTX_LEN = 256
GRID_W = 64

HEAD_DIM = 64
ATTN_HEADS = 8
KV_HEADS = 2
GQA_GROUP = ATTN_HEADS // KV_HEADS
ATTN_WIDTH = ATTN_HEADS * HEAD_DIM
KV_WIDTH = KV_HEADS * HEAD_DIM
CONV_GROUPS = 4
CONV_WIDTH = CONV_GROUPS * HEAD_DIM
LRU_BLOCKS = 4
LRU_BLOCK = HEAD_DIM
LRU_WIDTH = LRU_BLOCKS * LRU_BLOCK
MIX_WIDTH = ATTN_WIDTH + CONV_WIDTH + LRU_WIDTH
IN_SPLITS = (ATTN_WIDTH, KV_WIDTH, KV_WIDTH, CONV_WIDTH, CONV_WIDTH, CONV_WIDTH, LRU_WIDTH, LRU_WIDTH)
IN_WIDTH = sum(IN_SPLITS)
SHORT_CONV_K = 3
SHORT_PAD = (SHORT_CONV_K // 2, SHORT_CONV_K // 2)
LRU_CONV_K = 4
LRU_PAD = (2, 1)
LRU_C = 8.0
ROPE_THETA = 10000.0
Q_BLOCK = 128
N_EXPERTS = 16
EXPERT_FF = 1024
CAPACITY_FACTOR = 2
N_MOD = 6
EPS = 1e-6

kernel_name = 'hybrid_diffusion_parallel_heads_ec_moe'


def rms_norm(x, g):
    xf = x.astype(jnp.float32)
    y = xf * lax.rsqrt(jnp.mean(xf * xf, axis=-1, keepdims=True) + EPS)
    return (y * g.astype(jnp.float32)).astype(x.dtype)


def modulate(x, g, shift, scale):
    return rms_norm(x, g) * (1 + scale) + shift


def axial_rope_tables(n_tokens):
    rows = n_tokens // GRID_W
    row = jnp.repeat(jnp.arange(rows, dtype=jnp.float32), GRID_W)
    col = jnp.tile(jnp.arange(GRID_W, dtype=jnp.float32), rows)
    n_freq = HEAD_DIM // 4
    inv = ROPE_THETA ** (-jnp.arange(n_freq, dtype=jnp.float32) / n_freq)
    ang = jnp.concatenate([row[:, None] * inv, col[:, None] * inv], axis=-1)
    return jnp.cos(ang), jnp.sin(ang)


def apply_rope(t, cos, sin):
    tf = t.astype(jnp.float32)
    half = HEAD_DIM // 2
    t1, t2 = tf[..., :half], tf[..., half:]
    cs = cos[None, :, None, :]
    sn = sin[None, :, None, :]
    return jnp.concatenate([t1 * cs - t2 * sn, t1 * sn + t2 * cs], axis=-1).astype(t.dtype)


def heads(t, n):
    return t.reshape(t.shape[:-1] + (n, HEAD_DIM))


def gqa_attend(q, k, v):
    s = jnp.einsum('bqhgd,bkhd->bhgqk', q, k, preferred_element_type=jnp.float32) * (HEAD_DIM ** -0.5)
    p = jax.nn.softmax(s, axis=-1).astype(v.dtype)
    return jnp.einsum('bhgqk,bkhd->bqhgd', p, v)


def depthwise_conv(u, w, pad_l, pad_r):
    return lax.conv_general_dilated(u, w[:, None, :].astype(u.dtype), window_strides=(1,), padding=[(pad_l, pad_r)], dimension_numbers=('NWC', 'WIO', 'NWC'), feature_group_count=u.shape[-1])


def block_diag_linear(t, w, b):
    tb = t.reshape(t.shape[:-1] + (LRU_BLOCKS, LRU_BLOCK))
    return jnp.einsum('...nc,ncd->...nd', tb, w).reshape(t.shape) + b


def rg_lru_coeffs(u, wa, ba, wi, bi, lam):
    uf = u.astype(jnp.float32)
    r = jax.nn.sigmoid(block_diag_linear(uf, wa.astype(jnp.float32), ba.astype(jnp.float32)))
    i = jax.nn.sigmoid(block_diag_linear(uf, wi.astype(jnp.float32), bi.astype(jnp.float32)))
    log_a = -LRU_C * r * jax.nn.softplus(-lam.astype(jnp.float32))
    a = jnp.exp(log_a)
    b = jnp.sqrt(-jnp.expm1(2.0 * log_a)) * (i * uf)
    return a, b


def linear_scan(a, b, reverse):
    def combine(left, right):
        a_l, b_l = left
        a_r, b_r = right
        return a_l * a_r, a_r * b_l + b_r
    return lax.associative_scan(combine, (a, b), axis=1, reverse=reverse)


def bidirectional_rg_lru(u_x, g_x, u_c, g_c, conv_w, conv_b, wa, ba, wi, bi, lam, need_ctx):
    uc_x = depthwise_conv(u_x, conv_w, LRU_PAD[0], LRU_PAD[1]) + conv_b
    uc_c = depthwise_conv(u_c, conv_w, LRU_PAD[0], LRU_PAD[1]) + conv_b
    h_x = 0.0
    h_c = 0.0
    for d, rev in enumerate((False, True)):
        a_c, b_c = rg_lru_coeffs(uc_c, wa[d], ba[d], wi[d], bi[d], lam[d])
        _, hs_c = linear_scan(a_c, b_c, rev)
        h_end = hs_c[:, 0] if rev else hs_c[:, -1]
        a_x, b_x = rg_lru_coeffs(uc_x, wa[d], ba[d], wi[d], bi[d], lam[d])
        a_cum, hs_x = linear_scan(a_x, b_x, rev)
        h_x = h_x + a_cum * h_end[:, None, :] + hs_x
        if need_ctx:
            h_c = h_c + hs_c
    out_x = (h_x * jax.nn.gelu(g_x.astype(jnp.float32))).astype(u_x.dtype)
    out_c = (h_c * jax.nn.gelu(g_c.astype(jnp.float32))).astype(u_c.dtype) if need_ctx else None
    return out_x, out_c


def token_mixer(hx, hc, cos, sin, w_in, q_g, k_g, conv_w, lru_conv_w, lru_conv_b, lru_wa, lru_ba, lru_wi, lru_bi, lru_lam, w_out, need_ctx):
    bsz, n_tok, _ = hx.shape
    offs = np.cumsum(IN_SPLITS)[:-1].tolist()
    q_x, k_x, v_x, cb_x, cc_x, ch_x, lu_x, lg_x = jnp.split(hx @ w_in, offs, axis=-1)
    q_c, k_c, v_c, cb_c, cc_c, ch_c, lu_c, lg_c = jnp.split(hc @ w_in, offs, axis=-1)

    k_lat = apply_rope(rms_norm(heads(k_x, KV_HEADS), k_g), cos, sin)
    k_ctx = rms_norm(heads(k_c, KV_HEADS), k_g)
    v_ctx = heads(v_c, KV_HEADS)
    k_all = jnp.concatenate([k_ctx, k_lat], axis=1)
    v_all = jnp.concatenate([v_ctx, heads(v_x, KV_HEADS)], axis=1)
    q_lat = apply_rope(rms_norm(heads(q_x, ATTN_HEADS), q_g), cos, sin)
    n_blk = n_tok // Q_BLOCK
    q_blocks = q_lat.reshape(bsz, n_blk, Q_BLOCK, KV_HEADS, GQA_GROUP, HEAD_DIM).swapaxes(0, 1)
    att_x = lax.map(lambda qb: gqa_attend(qb, k_all, v_all), q_blocks)
    att_x = att_x.swapaxes(0, 1).reshape(bsz, n_tok, ATTN_WIDTH)

    conv_x = cb_x * depthwise_conv(cc_x * ch_x, conv_w, SHORT_PAD[0], SHORT_PAD[1])

    lru_x, lru_c = bidirectional_rg_lru(lu_x, lg_x, lu_c, lg_c, lru_conv_w, lru_conv_b, lru_wa, lru_ba, lru_wi, lru_bi, lru_lam, need_ctx)

    y_x = jnp.concatenate([att_x, conv_x, lru_x], axis=-1) @ w_out
    if not need_ctx:
        return y_x, None
    q_ctx = rms_norm(heads(q_c, ATTN_HEADS), q_g).reshape(bsz, -1, KV_HEADS, GQA_GROUP, HEAD_DIM)
    att_c = gqa_attend(q_ctx, k_ctx, v_ctx).reshape(bsz, -1, ATTN_WIDTH)
    conv_c = cb_c * depthwise_conv(cc_c * ch_c, conv_w, SHORT_PAD[0], SHORT_PAD[1])
    y_c = jnp.concatenate([att_c, conv_c, lru_c], axis=-1) @ w_out
    return y_x, y_c


def expert_choice_ffn(h, w_router, w_gate, w_up, w_down):
    bsz, n_tok, _ = h.shape
    cap = CAPACITY_FACTOR * n_tok // N_EXPERTS
    aff = jax.nn.softmax(jnp.einsum('btd,de->bte', h, w_router, preferred_element_type=jnp.float32), axis=-1)
    gate, idx = lax.top_k(jnp.swapaxes(aff, 1, 2), cap)
    bidx = jnp.arange(bsz)[:, None, None]
    xs = h[bidx, idx]
    hg = jnp.einsum('becd,edf->becf', xs, w_gate)
    hu = jnp.einsum('becd,edf->becf', xs, w_up)
    y = jnp.einsum('becf,efd->becd', jax.nn.silu(hg) * hu, w_down)
    y = y * gate[..., None].astype(y.dtype)
    return jnp.zeros_like(h).at[bidx, idx].add(y)


def setup_inputs(seed: int = 0) -> dict:
    key = jax.random.key(seed)
    ks = jax.random.split(key, 26)
    f32 = jnp.float32

    def nrm(k, shape, scale):
        return jax.random.normal(k, shape, f32) * scale

    L = DEPTH
    u = jax.random.uniform(ks[18], (L, 2, LRU_WIDTH), f32, 0.9, 0.999)
    root = u ** (1.0 / LRU_C)
    lru_lam = jnp.log(root) - jnp.log1p(-root)
    return {
        'x': nrm(ks[0], (BATCH, SEQ, D_MODEL), 1.0),
        'c': nrm(ks[1], (BATCH, D_MODEL), 1.0),
        'ctx': nrm(ks[2], (BATCH, CTX_LEN, D_MODEL), 1.0),
        'c_ctx': nrm(ks[3], (D_MODEL,), 1.0),
        'w_mod': nrm(ks[4], (L, D_MODEL, N_MOD * D_MODEL), 0.5 * D_MODEL ** -0.5),
        'b_mod': nrm(ks[5], (L, N_MOD * D_MODEL), 0.01),
        'norm1_g': 1.0 + nrm(ks[6], (L, D_MODEL), 0.02),
        'norm2_g': 1.0 + nrm(ks[7], (L, D_MODEL), 0.02),
        'w_in': nrm(ks[8], (L, D_MODEL, IN_WIDTH), D_MODEL ** -0.5),
        'q_norm_g': 1.0 + nrm(ks[9], (L, HEAD_DIM), 0.02),
        'k_norm_g': 1.0 + nrm(ks[10], (L, HEAD_DIM), 0.02),
        'conv_w': nrm(ks[11], (L, SHORT_CONV_K, CONV_WIDTH), SHORT_CONV_K ** -0.5),
        'lru_conv_w': nrm(ks[12], (L, LRU_CONV_K, LRU_WIDTH), LRU_CONV_K ** -0.5),
        'lru_conv_b': nrm(ks[13], (L, LRU_WIDTH), 0.01),
        'lru_wa': nrm(ks[14], (L, 2, LRU_BLOCKS, LRU_BLOCK, LRU_BLOCK), LRU_BLOCK ** -0.5),
        'lru_ba': nrm(ks[15], (L, 2, LRU_WIDTH), 0.01),
        'lru_wi': nrm(ks[16], (L, 2, LRU_BLOCKS, LRU_BLOCK, LRU_BLOCK), LRU_BLOCK ** -0.5),
        'lru_bi': nrm(ks[17], (L, 2, LRU_WIDTH), 0.01),
        'lru_lam': lru_lam,
        'w_out': nrm(ks[19], (L, MIX_WIDTH, D_MODEL), MIX_WIDTH ** -0.5),
        'w_router': nrm(ks[20], (L, D_MODEL, N_EXPERTS), D_MODEL ** -0.5),
        'w_gate': nrm(ks[21], (L, N_EXPERTS, D_MODEL, EXPERT_FF), D_MODEL ** -0.5),
        'w_up': nrm(ks[22], (L, N_EXPERTS, D_MODEL, EXPERT_FF), D_MODEL ** -0.5),
        'w_down': nrm(ks[23], (L, N_EXPERTS, EXPERT_FF, D_MODEL), EXPERT_FF ** -0.5),
        'final_g': 1.0 + nrm(ks[24], (D_MODEL,), 0.02),
    }


def reference(x, c, ctx, c_ctx, w_mod, b_mod, norm1_g, norm2_g, w_in, q_norm_g, k_norm_g, conv_w, lru_conv_w, lru_conv_b, lru_wa, lru_ba, lru_wi, lru_bi, lru_lam, w_out, w_router, w_gate, w_up, w_down, final_g):
    cos, sin = axial_rope_tables(x.shape[1])
    h_ctx = ctx
    for l in range(DEPTH):
        need_ctx = l < DEPTH - 1
        mod_x = jnp.split((jax.nn.silu(c) @ w_mod[l] + b_mod[l])[:, None, :], N_MOD, axis=-1)
        mod_c = jnp.split(jax.nn.silu(c_ctx) @ w_mod[l] + b_mod[l], N_MOD, axis=-1)
        hx = modulate(x, norm1_g[l], mod_x[0], mod_x[1])
        hc = modulate(h_ctx, norm1_g[l], mod_c[0], mod_c[1])
        y_x, y_c = token_mixer(hx, hc, cos, sin, w_in[l], q_norm_g[l], k_norm_g[l], conv_w[l], lru_conv_w[l], lru_conv_b[l], lru_wa[l], lru_ba[l], lru_wi[l], lru_bi[l], lru_lam[l], w_out[l], need_ctx)
        x = x + mod_x[2] * y_x
        x = x + mod_x[5] * expert_choice_ffn(modulate(x, norm2_g[l], mod_x[3], mod_x[4]), w_router[l], w_gate[l], w_up[l], w_down[l])
        if need_ctx:
            h_ctx = h_ctx + mod_c[2] * y_c
            h_ctx = h_ctx + mod_c[5] * expert_choice_ffn(modulate(h_ctx, norm2_g[l], mod_c[3], mod_c[4]), w_router[l], w_gate[l], w_up[l], w_down[l])
    return rms_norm(x, final_g)
```

```python
import contextlib
import numpy as np
import concourse.bass as bass
import concourse.mybir as mybir
from concourse.bass_utils import run_bass_kernel_spmd

F32 = mybir.dt.float32
BF = mybir.dt.bfloat16
I32 = mybir.dt.int32
U32 = mybir.dt.uint32
ALU = mybir.AluOpType
AF = mybir.ActivationFunctionType
AX = mybir.AxisListType

D = 1024
SEQ = 8192
CTX = 256
NTOK = SEQ + CTX
NT = NTOK // 128
NE = 16
CAP = 1024
CAPC = 32
EPS = 1e-6
ROWW = 1064
XS_ROWS = NE * CAP + NE * CAPC
BIGIDX = 1.0e6
ENGS = ("pe", "act", "dve", "pool", "sp")


class Sched:
    def __init__(self, nc, nd=None):
        self.nc = nc
        self.q = {e: [] for e in ENGS}
        self.nd = nd or {"sp": 16, "pool": 16, "act": 4}
        self._stack = []
        self.esem = {}
        self.cnt = {}
        self.dsem = {}
        self.dcnt = {}
        self.dnext = {}
        self.waited = {e: {} for e in ENGS}
        self.last_w = {}
        self.readers = {}
        self.nsem = 0
        self.n_instr = 0
        self.excl = set()

    def _new_sem(self, name):
        cm = self.nc.semaphore(name)
        s = cm.__enter__()
        self._stack.append(cm)
        self.nsem += 1
        return s

    def start(self):
        for e in ENGS:
            self.esem[e] = (self._new_sem(f"es_{e}_0"), 0)
            self.cnt[e] = 0
        for e, n in self.nd.items():
            self.dsem[e] = [self._new_sem(f"ds_{e}_{i}") for i in range(n)]
            self.dcnt[e] = [0] * n
            self.dnext[e] = 0

    def close(self):
        for cm in reversed(self._stack):
            cm.__exit__(None, None, None)
        self._stack = []

    def _need(self, eng, tok):
        kind = tok[0]
        if kind == "e":
            a, gen, n, sem = tok[1], tok[2], tok[3], tok[4]
            if a == eng and eng == "pe":
                return
            key = ("e", a, gen)
        else:
            _, a, i, n = tok
            key = ("d", a, i)
            sem = self.dsem[a][i]
        if self.waited[eng].get(key, 0) >= n:
            return
        self.waited[eng][key] = n
        self.q[eng].append(("wait", sem, n))

    def _deps(self, eng, reads, writes):
        for r in reads:
            t = self.last_w.get(r)
            if t is not None:
                self._need(eng, t)
        for w in writes:
            t = self.last_w.get(w)
            if t is not None:
                self._need(eng, t)
            for t in self.readers.get(w, ()):
                self._need(eng, t)

    def _record(self, tok, reads, writes):
        for r in reads:
            self.readers.setdefault(r, []).append(tok)
        for w in writes:
            self.last_w[w] = tok
            self.readers[w] = []

    def op(self, eng, fn, reads=(), writes=()):
        if self.excl:
            ex = [r for r in reads if r in self.excl]
            if ex:
                reads = [r for r in reads if r not in self.excl]
                writes = list(writes) + ex
        self._deps(eng, reads, writes)
        self.cnt[eng] += 1
        sem, gen = self.esem[eng]
        self.q[eng].append(("op", fn, sem))
        tok = ("e", eng, gen, self.cnt[eng], sem)
        self._record(tok, reads, writes)
        self.n_instr += 1
        return tok

    def dma(self, eng, fn, reads=(), writes=()):
        idx = self.dnext[eng]
        self.dnext[eng] = (idx + 1) % self.nd[eng]
        if self.dcnt[eng][idx] > 0:
            self._need(eng, ("d", eng, idx, self.dcnt[eng][idx] * 16))
        self._deps(eng, reads, writes)
        self.dcnt[eng][idx] += 1
        tok = ("d", eng, idx, self.dcnt[eng][idx] * 16)
        self.q[eng].append(("dma", fn, self.dsem[eng][idx]))
        self._record(tok, reads, writes)
        self.n_instr += 1
        return tok

    def barrier(self):
        for e in ENGS:
            for e2 in ENGS:
                if e2 != e and self.cnt[e2] > 0:
                    sem, gen = self.esem[e2]
                    self._need(e, ("e", e2, gen, self.cnt[e2], sem))
            for e2 in self.nd:
                for i in range(self.nd[e2]):
                    if self.dcnt[e2][i] > 0:
                        self._need(e, ("d", e2, i, self.dcnt[e2][i] * 16))
        self.last_w = {}
        self.readers = {}
        for e in ENGS:
            if self.cnt[e] > 20000:
                gen = self.esem[e][1] + 1
                self.esem[e] = (self._new_sem(f"es_{e}_{gen}"), gen)
                self.cnt[e] = 0

    def emit(self):
        with self.nc.Block() as block:
            def play(engname):
                def body(engine):
                    for item in self.q[engname]:
                        if item[0] == "wait":
                            engine.wait_ge(item[1], item[2])
                        elif item[0] == "op":
                            item[1](engine).then_inc(item[2], 1)
                        else:
                            item[1](engine).then_inc(item[2], 16)
                return body
            block.tensor(play("pe"))
            block.scalar(play("act"))
            block.vector(play("dve"))
            block.gpsimd(play("pool"))
            block.sync(play("sp"))


def run_streams(gens):
    gens = list(gens)
    while gens:
        for g in list(gens):
            try:
                next(g)
            except StopIteration:
                gens.remove(g)


class Ring:
    def __init__(self, tiles, name):
        self.tiles = tiles
        self.name = name
        self.i = -1

    def next(self):
        self.i = (self.i + 1) % len(self.tiles)
        return self.tiles[self.i], (self.name, self.i)


def build(stop_after=None, debug=False, nlayers=2):
    nc = bass.Bass("TRN2", target_bir_lowering=False)
    S = Sched(nc)

    def din(name, shape, dt=F32):
        return nc.dram_tensor(name, list(shape), dt, kind="ExternalInput").ap()

    def dscratch(name, shape, dt):
        kind = "ExternalOutput" if debug else "Internal"
        return nc.dram_tensor(name, list(shape), dt, kind=kind).ap()

    x_in = din("x", [SEQ, D])
    ctx_in = din("ctx", [CTX, D])
    cT_in = din("cT", [128, 8, 2])
    wmod_in = din("w_mod", [2, D, 6 * D])
    bmodT_in = din("b_modT", [2, 128, 48])
    g1T_in = din("g1T", [2, 128, 8])
    g2T_in = din("g2T", [2, 128, 8])
    win_in = din("w_in", [2, D, 2048])
    qkg_in = din("qkg", [2, 2, 64])
    convw_in = din("convw", [2, 128, 2, 3])
    lcw_in = din("lcw", [2, 128, 2, 5])
    lvec_in = din("lvec", [2, 128, 2, 2, 3])
    lwa_in = din("lwa", [2, 2, 4, 64, 64])
    lwi_in = din("lwi", [2, 2, 4, 64, 64])
    wout_in = din("w_out", [2, D, D])
    wr_in = din("w_r", [2, D, NE])
    ew = [2, NE, D, D] if not DBG.get("small_w") else [2, NE, 8, 8]
    wg_in = din("w_gate", ew)
    wu_in = din("w_up", ew)
    wd_in = din("w_down", ew)
    fg_in = din("fg", [1, D])
    rope_in = din("rope", [SEQ, 64])
    cst_in = din("cst", [128, 3, 128])
    out = nc.dram_tensor("out", [SEQ, D], F32, kind="ExternalOutput").ap()

    xa = dscratch("xa", [NTOK, D], F32)
    qT = dscratch("qT", [4, 128, NTOK], BF)
    featT = dscratch("featT", [10, 128, NTOK], F32)
    mixT = dscratch("mixT", [8, 128, NTOK], BF)
    h2D = dscratch("h2D", [NTOK, ROWW], BF)
    xs = dscratch("xs", [XS_ROWS, ROWW], BF)
    hD = dscratch("hD", [2, 2, 128, NTOK], F32)
    wbf = nc.dram_tensor("wbf", [3, NE, D, D], BF, kind="Internal").ap() if not DBG.get("small_w") else None
    woutbf = nc.dram_tensor("woutbf", [D, D], BF, kind="Internal").ap()
    winbf = nc.dram_tensor("winbf", [D, 2048], BF, kind="Internal").ap()
    dbg = dscratch("dbg", [128, 4096], F32) if debug else None

    top = contextlib.ExitStack()

    uniq = [0]

    def sb(es, name, shape, dt):
        uniq[0] += 1
        return es.enter_context(nc.sbuf_tensor(f"s{uniq[0]}_{name}", list(shape), dt))

    def ps(es, name, shape, dt, key=None):
        nbytes = int(np.prod(shape[1:])) * (4 if dt in (F32, I32, U32) else 2)
        assert nbytes in (2048, 4096), (name, shape, nbytes)
        S.excl.add(key if key is not None else name)
        uniq[0] += 1
        return es.enter_context(nc.psum_tensor(f"p{uniq[0]}_{name}", list(shape), dt))

    def ring(es, name, n, shape, dt, psum=False):
        if psum:
            return Ring([ps(es, f"{name}{i}", shape, dt, key=(name, i)) for i in range(n)], name)
        return Ring([sb(es, f"{name}{i}", shape, dt) for i in range(n)], name)

    pool_regs = {}

    def preg(en, val):
        if val not in pool_regs:
            pool_regs[val] = en.to_reg(val)
        return pool_regs[val]

    def I(eng, name, reads, writes, **kw):
        return S.op(eng, lambda e, kw=kw, name=name: getattr(e, name)(**kw), reads, writes)

    def DMA(eng, reads, writes, **kw):
        return S.dma(eng, lambda e, kw=kw: e.dma_start(**kw), reads, writes)

    S.start()

    cst_f = sb(top, "cst_f", [128, 3, 128], F32)
    ident_b = sb(top, "ident_b", [128, 128], BF)
    ltri_b = sb(top, "ltri_b", [128, 128], BF)
    ones_b = sb(top, "ones_b", [128, 128], BF)
    modT = sb(top, "modT", [128, 2, 48, 2], F32)
    aff_all = sb(top, "aff_all", [128, NT, NE], F32)
    tokid = sb(top, "tokid", [128, NT], I32)
    zcol = sb(top, "zcol", [128, 1], F32)
    ident_f = cst_f[:, 0, :]
    ltri_f = cst_f[:, 1, :]
    ones_f = cst_f[:, 2, :]

    DMA("sp", [], ["cst_f"], out=cst_f[:], in_=cst_in)
    I("dve", "tensor_copy", ["cst_f"], ["ident_b"], out=ident_b[:], in_=ident_f)
    I("dve", "tensor_copy", ["cst_f"], ["ltri_b"], out=ltri_b[:], in_=ltri_f)
    I("dve", "tensor_copy", ["cst_f"], ["ones_b"], out=ones_b[:], in_=ones_f)
    I("pool", "iota", [], ["tokid"], out=tokid[:], pattern=[[128, NT]], base=0, channel_multiplier=1)
    I("pool", "memset", [], ["zcol"], ap=zcol[:], constant=0.0)

    def rstd_chain(ss, ssk, tmp, tmpk, rs, rsk, n, inv_n):
        I("dve", "tensor_scalar", [ssk], [tmpk], out=tmp, in0=ss, scalar1=inv_n, scalar2=EPS,
          op0=ALU.mult, op1=ALU.add)
        I("act", "activation", [tmpk], [tmpk], out=tmp, in_=tmp, func=AF.Sqrt)
        I("dve", "reciprocal", [tmpk], [rsk], out=rs, in_=tmp)

    with contextlib.ExitStack() as es:
        cT_sb = sb(es, "cT_sb", [128, 8, 2], F32)
        sc = sb(es, "sc", [128, 8, 2], F32)
        bT = sb(es, "bT", [128, 2, 48], F32)
        wm = ring(es, "wm", 2, [128, 6 * D], F32)
        pm = ring(es, "pm", 2, [128, 512], F32, psum=True)
        DMA("sp", [], ["cT_sb"], out=cT_sb[:], in_=cT_in)
        DMA("sp", [], ["bT"], out=bT[:], in_=bmodT_in.rearrange("l p n -> p l n"))
        I("act", "activation", ["cT_sb"], ["sc"], out=sc[:], in_=cT_sb[:], func=AF.Silu)
        for l in range(nlayers):
            acc = modT[:, l].rearrange("p n s -> p (n s)")
            for kc in range(8):
                w, wk = wm.next()
                DMA("sp", [], [wk], out=w[:], in_=wmod_in[l, kc * 128:(kc + 1) * 128, :])
                p, pk = pm.next()
                for n in range(48):
                    I("pe", "matmul", [wk, "sc"], [pk], out=p[:, 2 * n:2 * n + 2],
                      lhsT=w[:, n * 128:(n + 1) * 128], rhs=sc[:, kc, :], start=True, stop=True)
                if kc == 0:
                    I("dve", "tensor_copy", [pk], [("modT", l)], out=acc, in_=p[:, 0:96])
                else:
                    I("dve", "tensor_tensor", [pk, ("modT", l)], [("modT", l)], out=acc, in0=acc, in1=p[:, 0:96],
                      op=ALU.add)
            I("dve", "tensor_tensor", ["bT", ("modT", l)], [("modT", l)], out=modT[:, l], in0=modT[:, l],
              in1=bT[:, l, :].unsqueeze(2).to_broadcast([128, 48, 2]), op=ALU.add)
        if debug:
            DMA("sp", [("modT", 0), ("modT", 1)], ["dbg"], out=dbg[:, 0:192],
                in_=modT[:].rearrange("p l n s -> p (l n s)"))
    S.barrier()
    if stop_after == "p0":
        return finish(nc, S, top)

    def src_rows(l, r0, n):
        if l == 0:
            if r0 < CTX:
                return ctx_in[r0:r0 + n, :]
            return x_in[r0 - CTX:r0 - CTX + n, :]
        return xa[r0:r0 + n, :]

    def make_bc(es_unused, dst, dstk, src_col, srck, tmpD, tmpDk, pbc, pbck, eng_toggle=[0]):
        for h in range(2):
            for j in range(4):
                kc = h * 4 + j
                I("dve", "tensor_scalar", ["cst_f", srck], [tmpDk], out=tmpD[:], in0=ident_f,
                  scalar1=src_col(kc), scalar2=None, op0=ALU.mult)
                I("pe", "matmul", [tmpDk, "cst_f"], [pbck], out=pbc[:, j * 128:(j + 1) * 128], lhsT=ones_f,
                  rhs=tmpD[:], start=True, stop=True)
            I("act", "copy", [pbck], [dstk], out=dst[:, h * 512:(h + 1) * 512], in_=pbc[:])

    units = [(0, 2, True)] + [(CTX + 512 * i, 4, False) for i in range(16)]

    for l in range(nlayers):
        need_ctx = l < nlayers - 1 or (nlayers == 1 and debug)
        last_layer = (l == 1)
        with contextlib.ExitStack() as LS:
            G1T = sb(LS, "G1T", [128, 8, 2], F32)
            G2T = sb(LS, "G2T", [128, 8, 2], F32)
            gT = sb(LS, "gT", [128, 2, 8], F32)
            DMA("sp", [], ["gT"], out=gT[:, 0, :], in_=g1T_in[l])
            DMA("sp", [], ["gT"], out=gT[:, 1, :], in_=g2T_in[l])

            def modsl(i):
                return modT[:, l, i * 8:(i + 1) * 8, :]
            I("dve", "tensor_scalar", [], ["G1T"], out=G1T[:], in0=modsl(1), scalar1=1.0, scalar2=None, op0=ALU.add)
            I("dve", "tensor_tensor", ["G1T", "gT"], ["G1T"], out=G1T[:], in0=G1T[:],
              in1=gT[:, 0, :].unsqueeze(2).to_broadcast([128, 8, 2]), op=ALU.mult)
            I("dve", "tensor_scalar", [], ["G2T"], out=G2T[:], in0=modsl(4), scalar1=1.0, scalar2=None, op0=ALU.add)
            I("dve", "tensor_tensor", ["G2T", "gT"], ["G2T"], out=G2T[:], in0=G2T[:],
              in1=gT[:, 1, :].unsqueeze(2).to_broadcast([128, 8, 2]), op=ALU.mult)
            sh1T = modsl(0)
            sh2T = modsl(3)

            with contextlib.ExitStack() as KV:
                KTz = [sb(KV, f"KTz{i}", [128, NTOK], BF) for i in range(2)]
                Vp = sb(KV, "Vp", [128, NT, 2, 128], BF)
                if "memsets" not in DBG["skip"]:
                    I("pool", "memset", [], ["KTz0z"], ap=KTz[0][64:128, :], constant=0.0)
                    I("pool", "memset", [], ["KTz1z"], ap=KTz[1][0:64, :], constant=0.0)
                    I("pool", "memset", [], ["Vp1"], ap=Vp[:, :, 0, 64:128], constant=1.0)
                    I("pool", "memset", [], ["Vp1"], ap=Vp[:, :, 1, 0:64], constant=1.0)

                with contextlib.ExitStack() as es:
                    win = sb(es, "win", [128, 8, 2048], BF)
                    if l == 0:
                        for kc in range(8):
                            for hh in range(2):
                                DMA("pool", [], ["win"], out=win[:, kc, hh * 1024:(hh + 1) * 1024],
                                    in_=win_in[l, kc * 128:(kc + 1) * 128, hh * 1024:(hh + 1) * 1024])
                    else:
                        for h_ in range(2):
                            DMA("sp", [], ["win"], out=win[:, 4 * h_:4 * h_ + 4, :],
                                in_=winbf[512 * h_:512 * (h_ + 1), :].rearrange("(k p) n -> p k n", p=128))
                    gqk = sb(es, "gqk", [128, 2, 64], F32)
                    if "gqk" not in DBG["skip"]:
                        DMA("sp", [], ["gqk"], out=gqk[:].rearrange("p a b -> p (a b)"),
                            in_=qkg_in[l:l + 1].rearrange("o a b -> o (a b)").to_broadcast([128, 128]))
                    I("dve", "tensor_scalar", ["gqk"], ["gqk"], out=gqk[:, 0, :], in0=gqk[:, 0, :], scalar1=0.125,
                      scalar2=None, op0=ALU.mult)
                    xt_r = ring(es, "xt", 3, [128, D], F32)
                    xn_r = ring(es, "xn", 2, [128, D], BF)
                    junk = sb(es, "junk", [128, D], BF)
                    sm_r = ring(es, "sm", 3, [128, 4], F32)
                    NHX = 3
                    hx_t = [sb(es, f"hxT{i}", [128, 8, 512], BF) for i in range(NHX)]
                    qTs_t = [sb(es, f"qTs{i}", [128, 4, 512], BF) for i in range(2)]
                    fs_r = ring(es, "fs", 3, [128, 512], F32)
                    pT_r = ring(es, "pT", 1, [128, 8, 128], BF, psum=True)
                    pf_r = ring(es, "pf", 1, [128, 512], F32, psum=True)
                    gq_bc = gqk[:, 0, :].unsqueeze(1).to_broadcast([128, 8, 64])
                    gk_bc = gqk[:, 1, :].unsqueeze(1).to_broadcast([128, 2, 64])
                    ulist = units if DBG["units"] is None else units[:DBG["units"]]
                    prog1 = {"a": 0, "b1": 0, "b2": 0, "b1n": {}}

                    def a_stream():
                        for ui, (tok0, ntl, is_ctx) in enumerate(ulist):
                            while min(prog1["b1"], prog1["b2"]) < ui - (NHX - 1):
                                yield
                            s = 1 if is_ctx else 0
                            hx, hxk = hx_t[ui % NHX], ("hx", ui % NHX)
                            for ti in range(ntl):
                                t = tok0 // 128 + ti
                                xt, xtk = xt_r.next()
                                DMA("sp", [], [xtk], out=xt[:], in_=src_rows(l, tok0 + ti * 128, 128))
                                sm, smk = sm_r.next()
                                I("pool", "memset", [], [smk], ap=sm[:, 0:1], constant=0.0)
                                yield
                                I("act", "activation", [xtk, smk], ["junk", smk], out=junk[:], in_=xt[:], func=AF.Square,
                                  accum_out=sm[:, 0:1])
                                yield
                                I("dve", "tensor_scalar", [smk], [smk], out=sm[:, 1:2], in0=sm[:, 0:1], scalar1=1.0 / D,
                                  scalar2=EPS, op0=ALU.mult, op1=ALU.add)
                                yield
                                I("act", "activation", [smk], [smk], out=sm[:, 1:2], in_=sm[:, 1:2], func=AF.Sqrt)
                                yield
                                I("dve", "reciprocal", [smk], [smk], out=sm[:, 2:3], in_=sm[:, 1:2])
                                yield
                                xn, xnk = xn_r.next()
                                I("dve", "tensor_scalar", [xtk, smk], [xnk], out=xn[:], in0=xt[:], scalar1=sm[:, 2:3],
                                  scalar2=None, op0=ALU.mult)
                                yield
                                pT, pTk = pT_r.next()
                                for kc in range(8):
                                    I("pe", "transpose", [xnk, "ident_b"], [pTk], out=pT[:, kc, :],
                                      in_=xn[:, kc * 128:(kc + 1) * 128], identity=ident_b[:])
                                yield
                                for kc in range(8):
                                    dst = hx[:, kc, ti * 128:(ti + 1) * 128]
                                    if t % 2 == 0:
                                        I("act", "activation", [pTk, "G1T"], [hxk], out=dst, in_=pT[:, kc, :],
                                          func=AF.Identity, scale=G1T[:, kc, s:s + 1], bias=sh1T[:, kc, s:s + 1])
                                    else:
                                        I("dve", "tensor_scalar", [pTk, "G1T"], [hxk], out=dst, in0=pT[:, kc, :],
                                          scalar1=G1T[:, kc, s:s + 1], scalar2=sh1T[:, kc, s:s + 1],
                                          op0=ALU.mult, op1=ALU.add)
                                    if kc % 2 == 1:
                                        yield
                            prog1["a"] = ui + 1

                    def b1_stream(par):
                        P = f"b1{par}_"
                        pq = ps(es, P + "pq", [128, 512], F32)
                        pkv = ps(es, P + "pkv", [128, 512], F32)
                        pqT = ps(es, P + "pqT", [128, 8, 128], BF)
                        pqk, pkvk, pqTk = P + "pq", P + "pkv", P + "pqT"
                        sq_r = ring(es, P + "sq", 1, [128, 640], F32)
                        qs_r = ring(es, P + "qs", 1, [128, 32], F32)
                        qn_r = ring(es, P + "qn", 1, [128, 10, 64], F32)
                        ra_r = ring(es, P + "ra", 1, [128, 10, 32], F32)
                        rb_r = ring(es, P + "rb", 1, [128, 10, 32], F32)
                        qr_r = ring(es, P + "qr", 1, [128, 10, 64], BF)
                        rope_r = ring(es, P + "rope", 1, [128, 64], F32)
                        for ui, (tok0, ntl, is_ctx) in enumerate(ulist):
                            while prog1["a"] <= ui:
                                yield
                            NTu = ntl * 128
                            hx, hxk = hx_t[ui % NHX], ("hx", ui % NHX)
                            qTs, qTsk = qTs_t[ui % 2], ("qTs", ui % 2)
                            for ti in range(par, ntl, 2):
                                t = tok0 // 128 + ti
                                for kc in range(8):
                                    I("pe", "matmul", [hxk, "win"], [pqk], out=pq[:],
                                      lhsT=hx[:, kc, ti * 128:(ti + 1) * 128], rhs=win[:, kc, 0:512],
                                      start=(kc == 0), stop=(kc == 7))
                                yield
                                for kc in range(8):
                                    I("pe", "matmul", [hxk, "win"], [pkvk], out=pkv[:, 0:256],
                                      lhsT=hx[:, kc, ti * 128:(ti + 1) * 128], rhs=win[:, kc, 512:768],
                                      start=(kc == 0), stop=(kc == 7))
                                yield
                                sq, sqk = sq_r.next()
                                qs, qsk = qs_r.next()
                                qn, qnk = qn_r.next()
                                qr, qrk = qr_r.next()
                                I("act", "activation", [pqk], [sqk], out=sq[:, 0:512], in_=pq[:], func=AF.Square)
                                I("act", "activation", [pkvk], [sqk], out=sq[:, 512:640], in_=pkv[:, 0:128],
                                  func=AF.Square)
                                yield
                                I("dve", "reduce_sum", [sqk], [qsk], out=qs[:, 0:10],
                                  in_=sq[:].rearrange("p (h d) -> p h d", d=64), axis=AX.X)
                                yield
                                I("dve", "tensor_scalar", [qsk], [qsk], out=qs[:, 10:20], in0=qs[:, 0:10], scalar1=1.0 / 64,
                                  scalar2=EPS, op0=ALU.mult, op1=ALU.add)
                                yield
                                I("act", "activation", [qsk], [qsk], out=qs[:, 10:20], in_=qs[:, 10:20], func=AF.Sqrt)
                                yield
                                I("dve", "reciprocal", [qsk], [qsk], out=qs[:, 20:30], in_=qs[:, 10:20])
                                yield
                                I("dve", "tensor_tensor", [pqk, qsk], [qnk], out=qn[:, 0:8, :],
                                  in0=pq[:].rearrange("p (h d) -> p h d", d=64),
                                  in1=qs[:, 20:28].unsqueeze(2).to_broadcast([128, 8, 64]), op=ALU.mult)
                                I("dve", "tensor_tensor", [pkvk, qsk, qnk], [qnk], out=qn[:, 8:10, :],
                                  in0=pkv[:, 0:128].rearrange("p (h d) -> p h d", d=64),
                                  in1=qs[:, 28:30].unsqueeze(2).to_broadcast([128, 2, 64]), op=ALU.mult)
                                I("act", "copy", [pkvk], [("Vp", t)], out=Vp[:, t, 0, 0:64], in_=pkv[:, 128:192])
                                I("act", "copy", [pkvk, ("Vp", t)], [("Vp", t)], out=Vp[:, t, 1, 64:128],
                                  in_=pkv[:, 192:256])
                                yield
                                if is_ctx:
                                    I("pool", "tensor_tensor", [qnk, "gqk"], [qrk], out=qr[:, 0:8, :], in0=qn[:, 0:8, :],
                                      in1=gq_bc, op=ALU.mult)
                                    I("pool", "tensor_tensor", [qnk, "gqk", qrk], [qrk], out=qr[:, 8:10, :],
                                      in0=qn[:, 8:10, :], in1=gk_bc, op=ALU.mult)
                                    yield
                                else:
                                    I("pool", "tensor_tensor", [qnk, "gqk"], [qnk], out=qn[:, 0:8, :], in0=qn[:, 0:8, :],
                                      in1=gq_bc, op=ALU.mult)
                                    I("pool", "tensor_tensor", [qnk, "gqk"], [qnk], out=qn[:, 8:10, :], in0=qn[:, 8:10, :],
                                      in1=gk_bc, op=ALU.mult)
                                    rp, rpk = rope_r.next()
                                    DMA("sp", [], [rpk], out=rp[:], in_=rope_in[(t - 2) * 128:(t - 1) * 128, :])
                                    yield
                                    ra, rak = ra_r.next()
                                    rb, rbk = rb_r.next()
                                    cosb = rp[:, 0:32].unsqueeze(1).to_broadcast([128, 10, 32])
                                    sinb = rp[:, 32:64].unsqueeze(1).to_broadcast([128, 10, 32])
                                    t1 = qn[:, :, 0:32]
                                    t2 = qn[:, :, 32:64]
                                    I("dve", "tensor_tensor", [qnk, rpk], [rak], out=ra[:], in0=t1, in1=cosb, op=ALU.mult)
                                    I("pool", "tensor_tensor", [qnk, rpk], [rbk], out=rb[:], in0=t2, in1=sinb, op=ALU.mult)
                                    yield
                                    I("dve", "tensor_tensor", [rak, rbk], [qrk], out=qr[:, :, 0:32], in0=ra[:], in1=rb[:],
                                      op=ALU.subtract)
                                    yield
                                    I("dve", "tensor_tensor", [qnk, rpk, rak], [rak], out=ra[:], in0=t1, in1=sinb,
                                      op=ALU.mult)
                                    I("pool", "tensor_tensor", [qnk, rpk, rbk], [rbk], out=rb[:], in0=t2, in1=cosb,
                                      op=ALU.mult)
                                    yield
                                    I("dve", "tensor_tensor", [rak, rbk, qrk], [qrk], out=qr[:, :, 32:64], in0=ra[:],
                                      in1=rb[:], op=ALU.add)
                                    yield
                                qrf = qr[:].rearrange("p h d -> p (h d)")
                                for j in range(5):
                                    I("pe", "transpose", [qrk, "ident_b"], [pqTk], out=pqT[:, j, :],
                                      in_=qrf[:, j * 128:(j + 1) * 128], identity=ident_b[:])
                                yield
                                I("act", "copy", [pqTk], [(qTsk, ti)], out=qTs[:, :, ti * 128:(ti + 1) * 128],
                                  in_=pqT[:, 0:4, :])
                                I("act", "copy", [pqTk], [("KT", t)], out=KTz[0][0:64, t * 128:(t + 1) * 128],
                                  in_=pqT[0:64, 4, :])
                                I("act", "copy", [pqTk, ("KT", t)], [("KT", t)],
                                  out=KTz[1][64:128, t * 128:(t + 1) * 128], in_=pqT[64:128, 4, :])
                                yield
                            prog1["b1n"][ui] = prog1["b1n"].get(ui, 0) + 1
                            if prog1["b1n"][ui] == 2:
                                DMA("pool", [(qTsk, ti) for ti in range(ntl)], [("qT", tok0)],
                                    out=qT[:, :, tok0:tok0 + NTu].rearrange("j p n -> p j n"), in_=qTs[:, :, 0:NTu])
                                prog1["b1"] = ui + 1

                    def b2_stream():
                        for ui, (tok0, ntl, is_ctx) in enumerate(ulist):
                            while prog1["a"] <= ui:
                                yield
                            NTu = ntl * 128
                            hx, hxk = hx_t[ui % NHX], ("hx", ui % NHX)
                            for n in range(10):
                                pf, pfk = pf_r.next()
                                for kc in range(8):
                                    I("pe", "matmul", [hxk, "win"], [pfk], out=pf[:, 0:NTu],
                                      lhsT=win[:, kc, 768 + n * 128:768 + (n + 1) * 128], rhs=hx[:, kc, 0:NTu],
                                      start=(kc == 0), stop=(kc == 7))
                                    if kc % 4 == 3:
                                        yield
                                fs, fsk = fs_r.next()
                                if n % 2 == 0:
                                    I("act", "copy", [pfk], [fsk], out=fs[:, 0:NTu], in_=pf[:, 0:NTu])
                                else:
                                    I("dve", "tensor_copy", [pfk], [fsk], out=fs[:, 0:NTu], in_=pf[:, 0:NTu])
                                DMA("pool", [fsk], [("featT", n, tok0)], out=featT[n, :, tok0:tok0 + NTu],
                                    in_=fs[:, 0:NTu])
                                yield
                            prog1["b2"] = ui + 1
                    run_streams([a_stream(), b1_stream(0), b1_stream(1), b2_stream()])
                S.barrier()
                if stop_after == f"p1_{l}":
                    if debug:
                        with contextlib.ExitStack() as es:
                            kd = sb(es, "kd", [128, 1024], F32)
                            I("dve", "tensor_copy", [], ["kd"], out=kd[:, 0:512], in_=KTz[0][:, 0:512])
                            I("dve", "tensor_copy", ["kd"], ["kd"], out=kd[:, 512:1024], in_=KTz[1][:, 0:512])
                            DMA("sp", ["kd"], ["dbg"], out=dbg[:, 0:1024], in_=kd[:])
                            vd = sb(es, "vd", [128, 1024], F32)
                            I("dve", "tensor_copy", [], ["vd"], out=vd[:],
                              in_=Vp[:, 0:4].rearrange("p t h d -> p (t h d)"))
                            DMA("sp", ["vd"], ["dbg"], out=dbg[:, 1024:2048], in_=vd[:])
                    return finish(nc, S, top)

                def seq_bounds(t0):
                    return (0, CTX) if t0 < CTX else (CTX, NTOK)

                CSEG = 512
                segs_c = [(0, CTX)] + [(CTX + CSEG * i, CSEG) for i in range(SEQ // CSEG)]
                SEG = 256
                segs_l = [(0, CTX)] + [(CTX + SEG * i, SEG) for i in range(SEQ // SEG)]
                GSEG = 512
                segs_g = [(0, CTX)] + [(CTX + GSEG * i, GSEG) for i in range(SEQ // GSEG)]
                with contextlib.ExitStack() as es:
                    cw = sb(es, "cw", [128, 2, 3], F32)
                    DMA("sp", [], ["cw"], out=cw[:], in_=convw_in[l])
                    lcw = sb(es, "lcw", [128, 2, 5], F32)
                    lvec = sb(es, "lvec", [128, 2, 2, 3], F32)
                    nlv = sb(es, "nlv", [128, 2, 2, 2], F32)
                    cneg = sb(es, "cneg", [128, 2, 2], F32)
                    DMA("sp", [], ["lcw"], out=lcw[:], in_=lcw_in[l])
                    DMA("sp", [], ["lvec"], out=lvec[:], in_=lvec_in[l])
                    I("dve", "tensor_scalar", ["lvec"], ["nlv"], out=nlv[:], in0=lvec[:, :, :, 0:2], scalar1=-1.0,
                      scalar2=None, op0=ALU.mult)
                    I("act", "activation", ["lvec"], ["cneg"], out=cneg[:], in_=lvec[:, :, :, 2], func=AF.Exp, scale=-1.0)
                    I("dve", "tensor_scalar", ["cneg"], ["cneg"], out=cneg[:], in0=cneg[:], scalar1=1.0, scalar2=None,
                      op0=ALU.add)
                    I("act", "activation", ["cneg"], ["cneg"], out=cneg[:], in_=cneg[:], func=AF.Ln)
                    I("dve", "tensor_scalar", ["cneg"], ["cneg"], out=cneg[:], in0=cneg[:], scalar1=-8.0, scalar2=None,
                      op0=ALU.mult)
                    Wblk = sb(es, "Wblk", [128, 2, 2, 2, 128], F32)
                    I("pool", "memset", [], ["Wblk"], ap=Wblk[:], constant=0.0)
                    for c in range(2):
                        for d in range(2):
                            for bi_ in range(2):
                                blk = 2 * c + bi_
                                DMA("sp", [], ["Wblk"],
                                    out=Wblk[bi_ * 64:(bi_ + 1) * 64, c, d, 0, bi_ * 64:(bi_ + 1) * 64], in_=lwa_in[l, d, blk])
                                DMA("sp", [], ["Wblk"],
                                    out=Wblk[bi_ * 64:(bi_ + 1) * 64, c, d, 1, bi_ * 64:(bi_ + 1) * 64], in_=lwi_in[l, d, blk])
                    prog2 = {"scan": [0, 0]}

                    def conv_stream(c):
                        P = f"cv{c}_"
                        ccs = sb(es, P + "ccs", [128, CSEG + 2], F32)
                        chs = sb(es, P + "chs", [128, CSEG + 2], F32)
                        cbs = sb(es, P + "cbs", [128, CSEG], F32)
                        uu = sb(es, P + "uu", [128, CSEG + 2], F32)
                        yy = sb(es, P + "yy", [128, CSEG], F32)
                        oo = sb(es, P + "oo", [128, CSEG], BF)
                        for (t0, n) in segs_c:
                            if t0 < CTX and not need_ctx:
                                continue
                            lo_s, hi_s = seq_bounds(t0)
                            lo = max(t0 - 1, lo_s)
                            hi = min(t0 + n + 1, hi_s)
                            off = lo - (t0 - 1)
                            DMA("sp", [], [P + "ccs"], out=ccs[:, off:off + hi - lo], in_=featT[2 + c, :, lo:hi])
                            DMA("sp", [], [P + "chs"], out=chs[:, off:off + hi - lo], in_=featT[4 + c, :, lo:hi])
                            DMA("sp", [], [P + "cbs"], out=cbs[:, 0:n], in_=featT[0 + c, :, t0:t0 + n])
                            yield
                            I("pool", "tensor_tensor", [P + "ccs", P + "chs"], [P + "uu"], out=uu[:, off:off + hi - lo],
                              in0=ccs[:, off:off + hi - lo], in1=chs[:, off:off + hi - lo], op=ALU.mult)
                            if off == 1:
                                I("pool", "memset", [P + "uu"], [P + "uu"], ap=uu[:, 0:1], constant=0.0)
                            if hi < t0 + n + 1:
                                I("pool", "memset", [P + "uu"], [P + "uu"], ap=uu[:, n + 1:n + 2], constant=0.0)
                            yield
                            I("dve", "tensor_scalar", [P + "uu", "cw"], [P + "yy"], out=yy[:, 0:n], in0=uu[:, 1:n + 1],
                              scalar1=cw[:, c, 1:2], scalar2=None, op0=ALU.mult)
                            yield
                            I("dve", "scalar_tensor_tensor", [P + "uu", "cw", P + "yy"], [P + "yy"], out=yy[:, 0:n],
                              in0=uu[:, 0:n], scalar=cw[:, c, 0:1], in1=yy[:, 0:n], op0=ALU.mult, op1=ALU.add)
                            yield
                            I("dve", "scalar_tensor_tensor", [P + "uu", "cw", P + "yy"], [P + "yy"], out=yy[:, 0:n],
                              in0=uu[:, 2:n + 2], scalar=cw[:, c, 2:3], in1=yy[:, 0:n], op0=ALU.mult, op1=ALU.add)
                            yield
                            I("dve", "tensor_tensor", [P + "yy", P + "cbs"], [P + "oo"], out=oo[:, 0:n], in0=yy[:, 0:n],
                              in1=cbs[:, 0:n], op=ALU.mult)
                            DMA("pool", [P + "oo"], [("mixT", 4 + c, t0)], out=mixT[4 + c, :, t0:t0 + n], in_=oo[:, 0:n])
                            yield

                    def scan_lane(d):
                        P = f"ls{d}_"
                        lus = sb(es, P + "lus", [128, SEG + 3], F32)
                        uc = sb(es, P + "uc", [128, SEG], F32)
                        rr = sb(es, P + "rr", [128, SEG], F32)
                        ii = sb(es, P + "ii", [128, SEG], F32)
                        tt = sb(es, P + "tt", [128, SEG], F32)
                        hh = sb(es, P + "hh", [128, SEG], F32)
                        carry = sb(es, P + "carry", [128, 1], F32)
                        pp = ps(es, P + "pp", [128, 512], F32)
                        order = segs_l if d == 0 else [segs_l[0]] + segs_l[:0:-1]
                        for c in range(2):
                            I("dve", "tensor_copy", ["zcol"], [P + "carry"], out=carry[:], in_=zcol[:])
                            for (t0, n) in order:
                                lo_s, hi_s = seq_bounds(t0)
                                lo = max(t0 - 2, lo_s)
                                hi = min(t0 + n + 1, hi_s)
                                off = lo - (t0 - 2)
                                if off > 0:
                                    I("pool", "memset", [], [P + "lus"], ap=lus[:, 0:2], constant=0.0)
                                if hi < t0 + n + 1:
                                    I("pool", "memset", [], [P + "lus"], ap=lus[:, n + 2:n + 3], constant=0.0)
                                DMA("sp", [], [P + "lus"], out=lus[:, off:off + hi - lo], in_=featT[6 + c, :, lo:hi])
                                yield
                                I("dve", "tensor_scalar", [P + "lus", "lcw"], [P + "uc"], out=uc[:, 0:n], in0=lus[:, 2:n + 2],
                                  scalar1=lcw[:, c, 2:3], scalar2=lcw[:, c, 4:5], op0=ALU.mult, op1=ALU.add)
                                yield
                                for k_, o_ in ((0, 0), (1, 1), (3, 3)):
                                    I("dve", "scalar_tensor_tensor", [P + "lus", "lcw", P + "uc"], [P + "uc"], out=uc[:, 0:n],
                                      in0=lus[:, o_:o_ + n], scalar=lcw[:, c, k_:k_ + 1], in1=uc[:, 0:n],
                                      op0=ALU.mult, op1=ALU.add)
                                    yield
                                I("pe", "matmul", ["Wblk", P + "uc"], [P + "pp"], out=pp[:, 0:n], lhsT=Wblk[:, c, d, 0, :],
                                  rhs=uc[:, 0:n], start=True, stop=True)
                                I("pe", "matmul", ["Wblk", P + "uc"], [P + "pp"], out=pp[:, 256:256 + n],
                                  lhsT=Wblk[:, c, d, 1, :], rhs=uc[:, 0:n], start=True, stop=True)
                                yield
                                I("act", "activation", [P + "pp", "nlv"], [P + "rr"], out=rr[:, 0:n], in_=pp[:, 0:n],
                                  func=AF.Exp, scale=-1.0, bias=nlv[:, d, c, 0:1])
                                I("act", "activation", [P + "pp", "nlv"], [P + "ii"], out=ii[:, 0:n], in_=pp[:, 256:256 + n],
                                  func=AF.Exp, scale=-1.0, bias=nlv[:, d, c, 1:2])
                                yield
                                I("dve", "tensor_scalar", [P + "rr"], [P + "rr"], out=rr[:, 0:n], in0=rr[:, 0:n], scalar1=1.0,
                                  scalar2=None, op0=ALU.add)
                                yield
                                I("dve", "reciprocal", [P + "rr"], [P + "rr"], out=rr[:, 0:n], in_=rr[:, 0:n])
                                yield
                                I("pool", "tensor_scalar", [P + "ii"], [P + "ii"], out=ii[:, 0:n], in0=ii[:, 0:n], scalar1=1.0,
                                  scalar2=None, op0=ALU.add)
                                yield
                                I("dve", "reciprocal", [P + "ii"], [P + "ii"], out=ii[:, 0:n], in_=ii[:, 0:n])
                                yield
                                I("act", "activation", [P + "rr", "cneg"], [P + "rr"], out=rr[:, 0:n], in_=rr[:, 0:n],
                                  func=AF.Exp, scale=cneg[:, d, c:c + 1])
                                I("pool", "tensor_tensor", [P + "ii", P + "uc"], [P + "ii"], out=ii[:, 0:n], in0=ii[:, 0:n],
                                  in1=uc[:, 0:n], op=ALU.mult)
                                yield
                                I("pool", "tensor_tensor", [P + "rr"], [P + "tt"], out=tt[:, 0:n], in0=rr[:, 0:n],
                                  in1=rr[:, 0:n], op=ALU.mult)
                                yield
                                I("act", "activation", [P + "tt"], [P + "tt"], out=tt[:, 0:n], in_=tt[:, 0:n], func=AF.Ln,
                                  scale=-1.0, bias=1.0)
                                I("act", "activation", [P + "tt"], [P + "tt"], out=tt[:, 0:n], in_=tt[:, 0:n], func=AF.Exp,
                                  scale=0.5)
                                yield
                                I("dve", "tensor_tensor", [P + "ii", P + "tt"], [P + "ii"], out=ii[:, 0:n], in0=ii[:, 0:n],
                                  in1=tt[:, 0:n], op=ALU.mult)
                                yield
                                if d == 0:
                                    I("dve", "tensor_tensor_scan", [P + "rr", P + "ii", P + "carry"], [P + "hh"],
                                      out=hh[:, 0:n], data0=rr[:, 0:n], data1=ii[:, 0:n], initial=carry[:, 0:1],
                                      op0=ALU.mult, op1=ALU.add)
                                    I("dve", "tensor_copy", [P + "hh", P + "carry"], [P + "carry"], out=carry[:],
                                      in_=hh[:, n - 1:n])
                                else:
                                    I("dve", "tensor_tensor_scan", [P + "rr", P + "ii", P + "carry"], [P + "hh"],
                                      out=hh[:, 0:n][:, ::-1], data0=rr[:, 0:n][:, ::-1], data1=ii[:, 0:n][:, ::-1],
                                      initial=carry[:, 0:1], op0=ALU.mult, op1=ALU.add)
                                    I("dve", "tensor_copy", [P + "hh", P + "carry"], [P + "carry"], out=carry[:],
                                      in_=hh[:, 0:1])
                                if not (t0 < CTX and not need_ctx):
                                    DMA("pool", [P + "hh"], [("hD", d, c, t0)], out=hD[d, c, :, t0:t0 + n], in_=hh[:, 0:n])
                                yield
                            prog2["scan"][c] += 1

                    def comb_stream(c):
                        P = f"cb{c}_"
                        h0 = sb(es, P + "h0", [128, GSEG], F32)
                        h1 = sb(es, P + "h1", [128, GSEG], F32)
                        lgs = sb(es, P + "lgs", [128, GSEG], F32)
                        tt = sb(es, P + "tt", [128, GSEG], F32)
                        ob = sb(es, P + "ob", [128, GSEG], BF)
                        while prog2["scan"][c] < 2:
                            yield
                        for si, (t0, n) in enumerate(segs_g):
                            if t0 < CTX and not need_ctx:
                                continue
                            rk = [("hD", d_, c, t0 + o_) for d_ in range(2) for o_ in range(0, n, SEG)]
                            DMA("sp", rk, [P + "h0"], out=h0[:, 0:n], in_=hD[0, c, :, t0:t0 + n])
                            DMA("sp", rk, [P + "h1"], out=h1[:, 0:n], in_=hD[1, c, :, t0:t0 + n])
                            DMA("sp", [], [P + "lgs"], out=lgs[:, 0:n], in_=featT[8 + c, :, t0:t0 + n])
                            yield
                            I("pool", "tensor_tensor", [P + "h0", P + "h1"], [P + "h0"], out=h0[:, 0:n], in0=h0[:, 0:n],
                              in1=h1[:, 0:n], op=ALU.add)
                            I("dve", "tensor_tensor", [P + "lgs"], [P + "tt"], out=tt[:, 0:n], in0=lgs[:, 0:n],
                              in1=lgs[:, 0:n], op=ALU.mult)
                            yield
                            I("dve", "tensor_scalar", [P + "tt"], [P + "tt"], out=tt[:, 0:n], in0=tt[:, 0:n],
                              scalar1=0.044715, scalar2=1.0, op0=ALU.mult, op1=ALU.add)
                            yield
                            I("pool", "tensor_tensor", [P + "tt", P + "lgs"], [P + "tt"], out=tt[:, 0:n], in0=tt[:, 0:n],
                              in1=lgs[:, 0:n], op=ALU.mult)
                            yield
                            I("act", "activation", [P + "tt"], [P + "tt"], out=tt[:, 0:n], in_=tt[:, 0:n],
                              func=AF.Exp, scale=-1.5957691216057308)
                            yield
                            I("dve", "tensor_scalar", [P + "tt"], [P + "tt"], out=tt[:, 0:n], in0=tt[:, 0:n], scalar1=1.0,
                              scalar2=None, op0=ALU.add)
                            yield
                            I("dve", "reciprocal", [P + "tt"], [P + "tt"], out=tt[:, 0:n], in_=tt[:, 0:n])
                            yield
                            I("pool", "tensor_tensor", [P + "tt", P + "lgs"], [P + "tt"], out=tt[:, 0:n], in0=tt[:, 0:n],
                              in1=lgs[:, 0:n], op=ALU.mult)
                            yield
                            I("dve", "tensor_tensor", [P + "tt", P + "h0"], [P + "ob"], out=ob[:, 0:n], in0=tt[:, 0:n],
                              in1=h0[:, 0:n], op=ALU.mult)
                            DMA("pool", [P + "ob"], [("mixT", 6 + c, t0)], out=mixT[6 + c, :, t0:t0 + n], in_=ob[:, 0:n])
                            yield

                    def att_stream():
                        qt_r = ring(es, "qt", 2, [128, 512], BF)
                        mo_r = ring(es, "mo", 2, [128, 512], BF)
                        pe_r = ring(es, "pex", 2, [128, 2, 512], BF)
                        rec_r = ring(es, "rec", 2, [128, 512], F32)
                        ps_r = ring(es, "pss", 2, [128, 2, 512], F32, psum=True)
                        po_r = ring(es, "po", 2, [128, 512], F32, psum=True)
                        qblocks = []
                        if need_ctx:
                            qblocks.append((0, CTX, [0, 1]))
                        for i in range(16):
                            qblocks.append((CTX + 512 * i, 512, list(range(NT))))
                        for (q0, N, kts) in qblocks:
                            for j in range(4):
                                qt, qtk = qt_r.next()
                                DMA("sp", [], [qtk], out=qt[:, 0:N], in_=qT[j, :, q0:q0 + N])
                                mo, mok = mo_r.next()
                                for half in range(2):
                                    po, pok = po_r.next()
                                    npair = len(kts) // 2

                                    def emit_s(pi_):
                                        p_, pk_ = ps_r.next()
                                        for u_ in range(2):
                                            kt = kts[2 * pi_ + u_]
                                            I("pe", "matmul", [qtk, ("KTz", half)], [pk_], out=p_[:, u_, 0:N],
                                              lhsT=KTz[half][:, kt * 128:(kt + 1) * 128], rhs=qt[:, 0:N], start=True, stop=True)
                                        return p_, pk_
                                    cur = emit_s(0)
                                    for pi_ in range(npair):
                                        nxt = emit_s(pi_ + 1) if pi_ + 1 < npair else None
                                        p_, pk_ = cur
                                        ex, exk = pe_r.next()
                                        I("act", "activation", [pk_], [exk], out=ex[:, :, 0:N], in_=p_[:, :, 0:N], func=AF.Exp)
                                        for u_ in range(2):
                                            i = 2 * pi_ + u_
                                            I("pe", "matmul", [exk, "Vp"], [pok], out=po[:, 0:N], lhsT=Vp[:, kts[i], half, :],
                                              rhs=ex[:, u_, 0:N], start=(i == 0), stop=(i == len(kts) - 1))
                                        cur = nxt
                                        yield
                                    rec, reck = rec_r.next()
                                    o_sl = slice(0, 64) if half == 0 else slice(64, 128)
                                    s_sl = slice(64, 128) if half == 0 else slice(0, 64)
                                    I("dve", "reciprocal", [pok], [reck], out=rec[o_sl, 0:N], in_=po[s_sl, 0:N])
                                    I("dve", "tensor_tensor", [pok, reck], [mok], out=mo[o_sl, 0:N], in0=po[o_sl, 0:N],
                                      in1=rec[o_sl, 0:N], op=ALU.mult)
                                DMA("pool", [mok], [("mixT", j, q0)], out=mixT[j, :, q0:q0 + N], in_=mo[:, 0:N])
                                yield

                    def wconv_stream():
                        for q4 in range(4):
                            DMA("pool", [], [("woutbf", q4)], out=woutbf[q4 * 256:(q4 + 1) * 256, :],
                                in_=wout_in[l, q4 * 256:(q4 + 1) * 256, :])
                            yield
                        if l + 1 < nlayers:
                            for kc in range(8):
                                DMA("pool", [], [("winbf", kc)], out=winbf[kc * 128:(kc + 1) * 128, :],
                                    in_=win_in[l + 1, kc * 128:(kc + 1) * 128, :])
                                yield
                        if wbf is None:
                            return
                        for e in range(NE):
                            for m_, src in enumerate((wg_in, wu_in, wd_in)):
                                for q4 in range(4):
                                    for _ in range(18):
                                        yield
                                    DMA("pool", [], [("wbf", m_, e, q4)], out=wbf[m_, e, q4 * 256:(q4 + 1) * 256, :],
                                        in_=src[l, e, q4 * 256:(q4 + 1) * 256, :])

                    run_streams([att_stream(), conv_stream(0), conv_stream(1), scan_lane(0), scan_lane(1),
                                 comb_stream(0), comb_stream(1), wconv_stream()])
            S.barrier()
            if stop_after == f"p2c_{l}":
                return finish(nc, S, top)

            with contextlib.ExitStack() as es:
                wr = sb(es, "wr", [128, 8, NE], F32)
                DMA("sp", [], ["wr"], out=wr[:], in_=wr_in[l].rearrange("(k p) e -> p k e", p=128))
                nstream = 2 if need_ctx else 1
                wout_s = [sb(es, f"wout_s{s}", [128, 8, D], BF) for s in range(nstream)]
                G2_bc = [sb(es, f"G2_bc{s}", [128, D], F32) for s in range(nstream)]
                sh2_bc = [sb(es, f"sh2_bc{s}", [128, D], F32) for s in range(nstream)]
                with contextlib.ExitStack() as es2:
                    wout = sb(es2, "wout", [128, 8, D], BF)
                    for h_ in range(2):
                        DMA("sp", [], ["wout"], out=wout[:, 4 * h_:4 * h_ + 4, :],
                            in_=woutbf[512 * h_:512 * (h_ + 1), :].rearrange("(k p) n -> p k n", p=128))
                    gate_bc = [sb(es2, f"gate_bc{s}", [128, D], F32) for s in range(nstream)]
                    tmpD = sb(es2, "tmpD", [128, 128], F32)
                    pbc = ps(es2, "pbc", [128, 512], F32)
                    for s in range(nstream):
                        make_bc(es, gate_bc[s], f"gate_bc{s}", lambda kc, s=s: modsl(2)[:, kc, s:s + 1], ("modT", l), tmpD,
                                "tmpD", pbc, "pbc")
                        make_bc(es, G2_bc[s], f"G2_bc{s}", lambda kc, s=s: G2T[:, kc, s:s + 1], "G2T", tmpD, "tmpD", pbc,
                                "pbc")
                        make_bc(es, sh2_bc[s], f"sh2_bc{s}", lambda kc, s=s: sh2T[:, kc, s:s + 1], ("modT", l), tmpD,
                                "tmpD", pbc, "pbc")
                        for kc in range(8):
                            I("dve" if kc % 2 == 0 else "pool", "tensor_tensor", ["wout", f"gate_bc{s}"], [f"wout_s{s}"],
                              out=wout_s[s][:, kc, :], in0=wout[:, kc, :], in1=gate_bc[s][:], op=ALU.mult)
                    S.barrier()
                junk3 = sb(es, "junk3", [128, D], BF)

                def p3_stream(k, nk):
                    P = f"p3{k}_"
                    mx = sb(es, P + "mx", [128, 8, 512], BF)
                    xt = sb(es, P + "xt", [128, D], F32)
                    x1 = sb(es, P + "x1", [128, D], F32)
                    h2 = sb(es, P + "h2", [128, D], F32)
                    row = sb(es, P + "row", [128, ROWW], BF)
                    h2T = sb(es, P + "h2T", [128, 8, 128], F32)
                    sm = sb(es, P + "sm", [128, 8], F32)
                    ex = sb(es, P + "ex", [128, NE], F32)
                    py = ps(es, P + "py", [128, 512], F32)
                    pta = ps(es, P + "pta", [128, 4, 128], F32)
                    ptb = ps(es, P + "ptb", [128, 4, 128], F32)
                    plog_t = ps(es, P + "plog", [128, 512], F32)
                    plog = plog_t[:, 0:NE]
                    mxk, xtk, x1k, h2k, rowk, h2Tk, smk, exk = (P + n_ for n_ in ("mx", "xt", "x1", "h2", "row", "h2T", "sm", "ex"))
                    pyk, ptak, ptbk, plogk = P + "py", P + "pta", P + "ptb", P + "plog"
                    ulist = [u_ for u_ in units if not (u_[2] and not need_ctx)]
                    for ui, (tok0, ntl, is_ctx) in enumerate(ulist):
                        if ui % nk != k:
                            continue
                        s = 1 if is_ctx else 0
                        NTu = ntl * 128
                        DMA("sp", [], [mxk], out=mx[:, :, 0:NTu], in_=mixT[:, :, tok0:tok0 + NTu].rearrange("c p n -> p c n"))
                        for ti in range(ntl):
                            t = tok0 // 128 + ti
                            DMA("sp", [], [xtk], out=xt[:], in_=src_rows(l, tok0 + ti * 128, 128))
                            yield
                            for nh in range(2):
                                cs = slice(nh * 512, (nh + 1) * 512)
                                for kc in range(8):
                                    I("pe", "matmul", [mxk, f"wout_s{s}"], [pyk], out=py[:],
                                      lhsT=mx[:, kc, ti * 128:(ti + 1) * 128], rhs=wout_s[s][:, kc, cs],
                                      start=(kc == 0), stop=(kc == 7))
                                yield
                                I("dve", "tensor_tensor", [pyk, xtk], [x1k], out=x1[:, cs], in0=py[:], in1=xt[:, cs],
                                  op=ALU.add)
                                yield
                            DMA("pool", [x1k], [("xa", t)], out=xa[tok0 + ti * 128:tok0 + (ti + 1) * 128, :], in_=x1[:])
                            I("pool", "memset", [], [smk], ap=sm[:], constant=0.0)
                            yield
                            I("act", "activation", [x1k, smk], ["junk3", smk], out=junk3[:], in_=x1[:], func=AF.Square,
                              accum_out=sm[:, 0:1])
                            yield
                            I("dve", "tensor_scalar", [smk], [smk], out=sm[:, 1:2], in0=sm[:, 0:1], scalar1=1.0 / D,
                              scalar2=EPS, op0=ALU.mult, op1=ALU.add)
                            yield
                            I("act", "activation", [smk], [smk], out=sm[:, 1:2], in_=sm[:, 1:2], func=AF.Sqrt)
                            yield
                            I("dve", "reciprocal", [smk], [smk], out=sm[:, 2:3], in_=sm[:, 1:2])
                            yield
                            I("dve", "scalar_tensor_tensor", [x1k, smk, f"G2_bc{s}"], [h2k], out=h2[:], in0=x1[:],
                              scalar=sm[:, 2:3], in1=G2_bc[s][:], op0=ALU.mult, op1=ALU.mult)
                            yield
                            I("pool", "tensor_tensor", [h2k, f"sh2_bc{s}"], [h2k], out=h2[:], in0=h2[:], in1=sh2_bc[s][:],
                              op=ALU.add)
                            yield
                            I("act", "copy", [h2k], [rowk], out=row[:, 0:D], in_=h2[:])
                            for kc in range(8):
                                pt_ = pta if kc < 4 else ptb
                                I("pe", "transpose", [h2k, "cst_f"], [ptak if kc < 4 else ptbk], out=pt_[:, kc % 4, :],
                                  in_=h2[:, kc * 128:(kc + 1) * 128], identity=ident_f)
                            yield
                            I("act", "copy", [ptak], [h2Tk], out=h2T[:, 0:4, :], in_=pta[:])
                            I("dve", "tensor_copy", [ptbk, h2Tk], [h2Tk], out=h2T[:, 4:8, :], in_=ptb[:])
                            yield
                            for kc in range(8):
                                I("pe", "matmul", [h2Tk, "wr"], [plogk], out=plog, lhsT=h2T[:, kc, :], rhs=wr[:, kc, :],
                                  start=(kc == 0), stop=(kc == 7))
                            yield
                            I("dve", "reduce_max", [plogk, smk], [smk], out=sm[:, 3:4], in_=plog, axis=AX.X)
                            yield
                            I("dve", "tensor_scalar", [smk], [smk], out=sm[:, 3:4], in0=sm[:, 3:4], scalar1=-1.0, scalar2=None,
                              op0=ALU.mult)
                            yield
                            I("act", "activation", [plogk, smk], [exk, smk], out=ex[:], in_=plog, func=AF.Exp,
                              bias=sm[:, 3:4], accum_out=sm[:, 4:5])
                            yield
                            I("dve", "reciprocal", [smk], [smk], out=sm[:, 5:6], in_=sm[:, 4:5])
                            yield
                            I("dve", "tensor_scalar", [exk, smk], [("aff", t)], out=aff_all[:, t, :], in0=ex[:],
                              scalar1=sm[:, 5:6], scalar2=None, op0=ALU.mult)
                            yield
                            I("dve", "tensor_copy", [("aff", t), rowk], [rowk], out=row[:, D:D + 32].bitcast(F32),
                              in_=aff_all[:, t, :])
                            I("pool", "tensor_copy", ["tokid", rowk], [rowk], out=row[:, D + 32:D + 34].bitcast(I32),
                              in_=tokid[:, t:t + 1])
                            DMA("pool", [rowk], [("h2D", t)], out=h2D[t * 128:(t + 1) * 128, :], in_=row[:])
                            yield
                run_streams([p3_stream(k, 2) for k in range(2)])
            S.barrier()
            if stop_after == f"p3_{l}":
                if debug:
                    DMA("sp", [], ["dbg"], out=dbg[:, 0:NT * NE], in_=aff_all[:].rearrange("p t e -> p (t e)"))
                return finish(nc, S, top)

            groups = [(2, NT, CAP, 0)]
            if need_ctx:
                groups.append((0, 2, CAPC, NE * CAP))
            ng = len(groups)
            idxi = sb(LS, "idxi", [128, NE, NT], I32)
            with contextlib.ExitStack() as es:
                lo_t = sb(es, "lo_t", [128, 2, NE], F32)
                hi_t = sb(es, "hi_t", [128, 2, NE], F32)
                mid = sb(es, "mid", [128, 2, NE], F32)
                kk = sb(es, "kk", [128, 2, NE], F32)
                cmp = sb(es, "cmp", [128, NT, NE], F32)
                cntb = sb(es, "cntb", [128, 2, NE], BF)
                cntf = sb(es, "cntf", [128, 2, NE], F32)
                geu = sb(es, "geu", [128, 2, NE], U32)
                ltu = sb(es, "ltu", [128, 2, NE], U32)
                ptot_t = ps(es, "ptot", [128, 512], F32)
                ptot = ptot_t[:, 0:2 * NE]
                I("dve", "memset", [], ["lo_t"], ap=lo_t[:], constant=0.0)
                I("dve", "memset", [], ["hi_t"], ap=hi_t[:], constant=2.0)
                I("dve", "memset", [], ["kk"], ap=kk[:, 0, :], constant=float(CAP))
                I("dve", "memset", ["kk"], ["kk"], ap=kk[:, 1, :], constant=float(CAPC))
                I("dve", "memset", [], ["cntf"], ap=cntf[:], constant=0.0)

                def count_ge(thr, thrk):
                    for g, (tl, th, cap, rb) in enumerate(groups):
                        I("dve", "tensor_tensor", ["aff", thrk], ["cmp"], out=cmp[:, tl:th, :], in0=aff_all[:, tl:th, :],
                          in1=thr[:, g, :].unsqueeze(1).to_broadcast([128, th - tl, NE]), op=ALU.is_ge)
                        I("dve", "reduce_sum", ["cmp"], ["cntf"], out=cntf[:, g, :],
                          in_=cmp[:, tl:th, :].rearrange("p t e -> p e t"), axis=AX.X)
                    I("dve", "tensor_copy", ["cntf"], ["cntb"], out=cntb[:], in_=cntf[:])
                    I("pe", "matmul", ["cntb", "ones_b"], ["ptot"], out=ptot,
                      lhsT=ones_b[:], rhs=cntb[:].rearrange("p g e -> p (g e)"), start=True, stop=True)

                NIT = 34
                for it in range(NIT):
                    I("dve", "tensor_tensor", ["lo_t", "hi_t"], ["mid"], out=mid[:], in0=lo_t[:], in1=hi_t[:], op=ALU.add)
                    I("dve", "tensor_scalar", ["mid"], ["mid"], out=mid[:], in0=mid[:], scalar1=0.5, scalar2=None,
                      op0=ALU.mult)
                    count_ge(mid, "mid")
                    ptv = ptot.rearrange("p (g e) -> p g e", g=2)
                    I("dve", "tensor_tensor", ["ptot", "kk"], ["geu"], out=geu[:], in0=ptv, in1=kk[:], op=ALU.is_ge)
                    I("dve", "tensor_tensor", ["ptot", "kk"], ["ltu"], out=ltu[:], in0=ptv, in1=kk[:], op=ALU.is_lt)
                    I("dve", "copy_predicated", ["geu", "mid", "lo_t"], ["lo_t"], out=lo_t[:], mask=geu[:], data=mid[:])
                    I("dve", "copy_predicated", ["ltu", "mid", "hi_t"], ["hi_t"], out=hi_t[:], mask=ltu[:], data=mid[:])
                count_ge(lo_t, "lo_t")
                offp = sb(es, "offp", [128, 2, NE], F32)
                poff_t = ps(es, "poff", [128, 512], F32)
                poff = poff_t[:, 0:2 * NE]
                I("pe", "matmul", ["cntb", "ltri_b"], ["poff"], out=poff, lhsT=ltri_b[:],
                  rhs=cntb[:].rearrange("p g e -> p (g e)"), start=True, stop=True)
                I("act", "copy", ["poff"], ["offp"], out=offp[:].rearrange("p g e -> p (g e)"), in_=poff)
                Mt = sb(es, "Mt", [128, NE, NT], F32)
                cs_ = sb(es, "cs_", [128, NE, NT], F32)
                onesr = sb(es, "onesr", [128, NE * NT], F32)
                idxf = sb(es, "idxf", [128, NE, NT], F32)
                ebase = sb(es, "ebase", [128, 2, NE], F32)
                I("pool", "memset", [], ["onesr"], ap=onesr[:], constant=1.0)
                I("pool", "iota", [], ["ebase0"], out=idxi[:, :, 0], pattern=[[1, NE]], base=0, channel_multiplier=0)
                I("dve", "tensor_copy", ["ebase0"], ["ebase"], out=ebase[:, 0, :], in_=idxi[:, :, 0])
                I("dve", "tensor_scalar", ["ebase"], ["ebase"], out=ebase[:, 1, :], in0=ebase[:, 0, :], scalar1=float(CAPC),
                  scalar2=float(NE * CAP), op0=ALU.mult, op1=ALU.add)
                I("dve", "tensor_scalar", ["ebase"], ["ebase"], out=ebase[:, 0, :], in0=ebase[:, 0, :], scalar1=float(CAP),
                  scalar2=None, op0=ALU.mult)
                I("dve", "tensor_copy", ["cmp"], ["Mt"], out=Mt[:], in_=cmp[:].rearrange("p t e -> p e t"))
                for g, (tl, th, cap, rb) in enumerate(groups):
                    ntl_ = th - tl
                    for e in range(NE):
                        I("dve", "tensor_tensor_scan", ["Mt", "onesr", "zcol"], ["cs_"], out=cs_[:, e, tl:th],
                          data0=onesr[:, 0:ntl_], data1=Mt[:, e, tl:th], initial=zcol[:, 0:1], op0=ALU.mult, op1=ALU.add)
                    I("dve", "tensor_tensor", ["cs_", "Mt"], ["cs_"], out=cs_[:, :, tl:th], in0=cs_[:, :, tl:th],
                      in1=Mt[:, :, tl:th], op=ALU.subtract)
                    I("dve", "tensor_tensor", ["cs_", "offp"], ["cs_"], out=cs_[:, :, tl:th], in0=cs_[:, :, tl:th],
                      in1=offp[:, g, :].unsqueeze(2).to_broadcast([128, NE, ntl_]), op=ALU.add)
                    I("dve", "tensor_scalar", ["cs_"], ["idxf"], out=idxf[:, :, tl:th], in0=cs_[:, :, tl:th],
                      scalar1=float(cap), scalar2=None, op0=ALU.is_lt)
                    I("dve", "tensor_tensor", ["idxf", "Mt"], ["idxf"], out=idxf[:, :, tl:th], in0=idxf[:, :, tl:th],
                      in1=Mt[:, :, tl:th], op=ALU.mult)
                    I("dve", "tensor_tensor", ["cs_", "ebase"], ["cs_"], out=cs_[:, :, tl:th], in0=cs_[:, :, tl:th],
                      in1=ebase[:, g, :].unsqueeze(2).to_broadcast([128, NE, ntl_]), op=ALU.add)
                    I("dve", "tensor_scalar", ["cs_"], ["cs_"], out=cs_[:, :, tl:th], in0=cs_[:, :, tl:th],
                      scalar1=-BIGIDX, scalar2=None, op0=ALU.add)
                    I("dve", "tensor_tensor", ["idxf", "cs_"], ["idxf"], out=idxf[:, :, tl:th], in0=idxf[:, :, tl:th],
                      in1=cs_[:, :, tl:th], op=ALU.mult)
                    I("dve", "tensor_scalar", ["idxf"], ["idxf"], out=idxf[:, :, tl:th], in0=idxf[:, :, tl:th],
                      scalar1=BIGIDX, scalar2=None, op0=ALU.add)
                I("dve", "tensor_copy", ["idxf", "ebase0"], ["idxi"], out=idxi[:], in_=idxf[:])
                if debug:
                    DMA("sp", ["lo_t"], ["dbg"], out=dbg[:, 0:2 * NE], in_=lo_t[:].rearrange("p g e -> p (g e)"))
                    DMA("sp", ["idxf"], ["dbg"], out=dbg[:, 64:64 + NE * NT], in_=idxf[:].rearrange("p e t -> p (e t)"))
            S.barrier()
            if stop_after == f"p4c_{l}":
                return finish(nc, S, top)

            with contextlib.ExitStack() as es:
                mod5_bc = [sb(es, f"mod5_bc{s}", [128, D], F32) for s in range(ng)]
                with contextlib.ExitStack() as es2:
                    tmpD = sb(es2, "tmpD4", [128, 128], F32)
                    pbc = ps(es2, "pbc4", [128, 512], F32)
                    for s in range(ng):
                        make_bc(es, mod5_bc[s], f"mod5_bc{s}", lambda kc, s=s: modsl(5)[:, kc, s:s + 1], ("modT", l), tmpD,
                                "tmpD4", pbc, "pbc4")
                    S.barrier()
                wg_t = [sb(es, f"wg{i}", [128, 8, D], BF) for i in range(2)]
                wu_t = [sb(es, f"wu{i}", [128, 8, D], BF) for i in range(2)]
                wd_t = [sb(es, "wd0", [128, 8, D], BF)]
                xsb = sb(es, "xsb", [128, 8, ROWW], BF)
                xsc = sb(es, "xsc", [CAPC, ROWW], BF)
                meta_t = [sb(es, f"meta{i}", [128, 9, 40], BF) for i in range(2)]
                xsT_t = [sb(es, f"xsT{i}", [128, 8, CAP + CAPC], BF) for i in range(2)]
                actT = sb(es, "actT", [128, 8, CAP + CAPC], BF)
                sg_r = ring(es, "sg", 2, [128, 512], F32)
                yt_r = ring(es, "yt", 4, [128, D], F32)
                pxt_r = ring(es, "pxt", 2, [128, 8, 128], BF, psum=True)
                pg_r = ring(es, "pg", 2, [128, 512], F32, psum=True)
                pu_r = ring(es, "pu", 2, [128, 512], F32, psum=True)
                pyy_r = ring(es, "pyy", 2, [128, 512], F32, psum=True)
                rw_r = ring(es, "rw", 4, [128, ROWW], BF)
                prog = {"w_g": 0, "w_d": 0, "c_gu": 0, "c_dn": 0, "s_done": 0, "c_start": 0}
                NPASS = 4
                EPP = NE // NPASS

                def s_stream():
                    for g_ in range(NPASS):
                        while prog["c_start"] < EPP * (g_ - 1):
                            yield
                        for (tl, th, cap, rb) in groups:
                            for t in range(tl, th):
                                rw, rwk = rw_r.next()
                                DMA("sp", [], [rwk], out=rw[:], in_=h2D[t * 128:(t + 1) * 128, :])
                                for e in range(EPP * g_, EPP * (g_ + 1)):
                                    S.dma("pool", lambda en, rw=rw, e=e, t=t: en.indirect_dma_start(
                                        out=xs, out_offset=bass.IndirectOffsetOnAxis(ap=idxi[:, e, t:t + 1], axis=0),
                                        in_=rw[:], in_offset=None, bounds_check=preg(en, XS_ROWS - 1), oob_is_err=False),
                                        [rwk, "idxi"], [("xs", e, t)])
                                    yield
                        prog["s_done"] = EPP * (g_ + 1)


                def load_mat(m_, e, w, wk):
                    for h_ in range(2):
                        DMA("sp", [], [wk], out=w[:, 4 * h_:4 * h_ + 4, :],
                            in_=wbf[m_, e, 512 * h_:512 * (h_ + 1), :].rearrange("(k p) n -> p k n", p=128))
                        yield

                def w_stream():
                    for e in range(NE):
                        while prog["c_gu"] < e - 1:
                            yield
                        yield from load_mat(0, e, wg_t[e % 2], ("wg", e % 2))
                        yield from load_mat(1, e, wu_t[e % 2], ("wu", e % 2))
                        prog["w_g"] = e + 1
                        while prog["c_dn"] < e:
                            yield
                        yield from load_mat(2, e, wd_t[0], ("wd", 0))
                        prog["w_d"] = e + 1

                def load_x(e):
                    DMA("sp", [("xs", e, t) for t in range(2, NT)], ["xsb"], out=xsb[:],
                        in_=xs[e * CAP:(e + 1) * CAP, :].rearrange("(t p) w -> p t w", p=128))
                    if need_ctx:
                        DMA("sp", [("xs", e, t) for t in range(0, 2)], ["xsc"], out=xsc[:],
                            in_=xs[NE * CAP + e * CAPC:NE * CAP + (e + 1) * CAPC, :])

                def transposes(e):
                    xsT = xsT_t[e % 2]
                    xk = ("xsT", e % 2)
                    meta = meta_t[e % 2]
                    mk_ = ("meta", e % 2)
                    I("dve", "tensor_copy", ["xsb"], [mk_], out=meta[:, 0:8, :], in_=xsb[:, :, D:D + 40])
                    if need_ctx:
                        I("dve", "tensor_copy", ["xsc", mk_], [mk_], out=meta[0:CAPC, 8, :], in_=xsc[:, D:D + 40])
                    for st in range(8):
                        pxt, pxtk = pxt_r.next()
                        for kc in range(8):
                            I("pe", "transpose", ["xsb", "ident_b"], [pxtk], out=pxt[:, kc, :],
                              in_=xsb[:, st, kc * 128:(kc + 1) * 128], identity=ident_b[:])
                        if st % 2 == 0:
                            I("act", "copy", [pxtk], [xk], out=xsT[:, :, st * 128:(st + 1) * 128], in_=pxt[:])
                        else:
                            I("dve", "tensor_copy", [pxtk], [xk], out=xsT[:, :, st * 128:(st + 1) * 128], in_=pxt[:])
                        yield
                    if need_ctx:
                        pxt, pxtk = pxt_r.next()
                        for kc in range(8):
                            I("pe", "transpose", ["xsc", "ident_b"], [pxtk], out=pxt[:, kc, 0:CAPC],
                              in_=xsc[:, kc * 128:(kc + 1) * 128], identity=ident_b[0:CAPC, 0:CAPC])
                        I("act", "copy", [pxtk], [xk], out=xsT[:, :, CAP:CAP + CAPC], in_=pxt[:, :, 0:CAPC])
                        yield

                def c_stream():
                    while prog["s_done"] <= 0:
                        yield
                    load_x(0)
                    yield from transposes(0)
                    slabs = [(0, 512), (512, 512)] + ([(CAP, CAPC)] if need_ctx else [])
                    for e in range(NE):
                        xsT = xsT_t[e % 2]
                        xk = ("xsT", e % 2)
                        meta = meta_t[e % 2]
                        mk_ = ("meta", e % 2)
                        prog["c_start"] = e + 1
                        if e + 1 < NE:
                            while prog["s_done"] <= e + 1:
                                yield
                            load_x(e + 1)
                        while prog["w_g"] <= e:
                            yield
                        wg, wgk = wg_t[e % 2], ("wg", e % 2)
                        wu, wuk = wu_t[e % 2], ("wu", e % 2)
                        wd, wdk = wd_t[0], ("wd", 0)
                        for fc in range(8):
                            for (c0, w_) in slabs:
                                pg, pgk = pg_r.next()
                                pu, puk = pu_r.next()
                                for kc in range(8):
                                    I("pe", "matmul", [xk, wgk], [pgk], out=pg[:, 0:w_], lhsT=wg[:, kc, fc * 128:(fc + 1) * 128],
                                      rhs=xsT[:, kc, c0:c0 + w_], start=(kc == 0), stop=(kc == 7))
                                for kc in range(8):
                                    I("pe", "matmul", [xk, wuk], [puk], out=pu[:, 0:w_], lhsT=wu[:, kc, fc * 128:(fc + 1) * 128],
                                      rhs=xsT[:, kc, c0:c0 + w_], start=(kc == 0), stop=(kc == 7))
                                sg, sgk = sg_r.next()
                                I("act", "activation", [pgk], [sgk], out=sg[:, 0:w_], in_=pg[:, 0:w_], func=AF.Silu)
                                I("dve", "tensor_tensor", [sgk, puk], [("actT", fc)], out=actT[:, fc, c0:c0 + w_],
                                  in0=sg[:, 0:w_], in1=pu[:, 0:w_], op=ALU.mult)
                                yield
                        prog["c_gu"] = e + 1
                        if e + 1 < NE:
                            yield from transposes(e + 1)
                        while prog["w_d"] <= e:
                            yield
                        tiles = [(st * 128, 128, st, 0) for st in range(8)]
                        if need_ctx:
                            tiles.append((CAP, CAPC, 8, 1))
                        for (c0, m_, mi, g) in tiles:
                            yt, ytk = yt_r.next()
                            gate_ap = meta[0:m_, mi, 2 * e:2 * e + 2].bitcast(F32)
                            for nh in range(2):
                                cs = slice(nh * 512, (nh + 1) * 512)
                                pyy, pyk = pyy_r.next()
                                for fc in range(8):
                                    I("pe", "matmul", [("actT", fc), wdk], [pyk], out=pyy[0:m_, :],
                                      lhsT=actT[:, fc, c0:c0 + m_], rhs=wd[:, fc, cs], start=(fc == 0), stop=(fc == 7))
                                I("dve", "scalar_tensor_tensor", [pyk, mk_, f"mod5_bc{g}"], [ytk], out=yt[0:m_, cs],
                                  in0=pyy[0:m_, :], scalar=gate_ap, in1=mod5_bc[g][0:m_, cs], op0=ALU.mult, op1=ALU.mult)
                                yield
                            idx_ap = meta[0:m_, mi, 32:34].bitcast(I32)
                            S.dma("pool", lambda en, yt=yt, m_=m_, idx_ap=idx_ap: en.indirect_dma_start(
                                out=xa, out_offset=bass.IndirectOffsetOnAxis(ap=idx_ap, axis=0), in_=yt[0:m_, :],
                                in_offset=None, bounds_check=preg(en, NTOK - 1), oob_is_err=True, compute_op=ALU.add),
                                [ytk, mk_] + [("xa_p", (e - 1) % 2, i_) for i_ in range(9)], [("xa_p", e % 2, mi)])
                            yield
                        prog["c_dn"] = e + 1
                run_streams([s_stream(), w_stream(), c_stream()])
            S.barrier()
            if stop_after == f"p4_{l}":
                return finish(nc, S, top)

    with contextlib.ExitStack() as es:
        fg_bc = sb(es, "fg_bc", [128, D], F32)
        DMA("sp", [], ["fg_bc"], out=fg_bc[:], in_=fg_in.to_broadcast([128, D]))
        xt_r = ring(es, "xt5", 3, [128, D], F32)
        ot_r = ring(es, "ot5", 3, [128, D], F32)
        junk = sb(es, "junk5", [128, D], BF)
        sm_r = ring(es, "sm5", 3, [128, 4], F32)
        for t in range(2, NT):
            xt, xtk = xt_r.next()
            DMA("sp", [], [xtk], out=xt[:], in_=xa[t * 128:(t + 1) * 128, :])
            sm, smk = sm_r.next()
            I("pool", "memset", [], [smk], ap=sm[:, 0:1], constant=0.0)
            I("act", "activation", [xtk, smk], ["junk5", smk], out=junk[:], in_=xt[:], func=AF.Square, accum_out=sm[:, 0:1])
            rstd_chain(sm[:, 0:1], smk, sm[:, 1:2], smk, sm[:, 2:3], smk, 1, 1.0 / D)
            ot, otk = ot_r.next()
            I("dve", "scalar_tensor_tensor", [xtk, smk, "fg_bc"], [otk], out=ot[:], in0=xt[:], scalar=sm[:, 2:3],
              in1=fg_bc[:], op0=ALU.mult, op1=ALU.mult)
            DMA("pool", [otk], [("out", t)], out=out[(t - 2) * 128:(t - 1) * 128, :], in_=ot[:])
    return finish(nc, S, top, True)


def finish(nc, S, top, close_top=False):
    S.barrier()
    S.emit()
    S.close()
    if close_top:
        top.close()
    return nc


Q_PERM = np.concatenate([np.arange(64) + 64 * (j + 4 * half) for j in range(4) for half in range(2)])


def rope_table():
    rows = SEQ // 64
    row = np.repeat(np.arange(rows, dtype=np.float32), 64)
    col = np.tile(np.arange(64, dtype=np.float32), rows)
    n_freq = 16
    inv = (np.float32(10000.0) ** (-np.arange(n_freq, dtype=np.float32) / np.float32(n_freq))).astype(np.float32)
    ang = np.concatenate([row[:, None] * inv, col[:, None] * inv], axis=-1).astype(np.float32)
    return np.concatenate([np.cos(ang), np.sin(ang)], axis=-1).astype(np.float32)


def fm(v):
    v = np.asarray(v)
    return np.ascontiguousarray(np.swapaxes(v.reshape(v.shape[:-1] + (v.shape[-1] // 128, 128)), -1, -2))


def prep_inputs(inp):
    f = lambda a: np.ascontiguousarray(np.asarray(a, dtype=np.float32))
    shared = {}
    shared["w_mod"] = f(inp["w_mod"])
    shared["b_modT"] = f(fm(inp["b_mod"]))
    shared["g1T"] = f(fm(inp["norm1_g"]))
    shared["g2T"] = f(fm(inp["norm2_g"]))
    w_in = np.asarray(inp["w_in"], dtype=np.float32).copy()
    w_in[:, :, 0:512] = w_in[:, :, Q_PERM]
    shared["w_in"] = f(w_in)
    shared["qkg"] = f(np.stack([inp["q_norm_g"], inp["k_norm_g"]], axis=1))
    cw = np.asarray(inp["conv_w"], dtype=np.float32)
    shared["convw"] = f(cw.reshape(2, 3, 2, 128).transpose(0, 3, 2, 1))
    lw = np.asarray(inp["lru_conv_w"], dtype=np.float32)
    lb = np.asarray(inp["lru_conv_b"], dtype=np.float32)
    lcw = np.concatenate([lw, lb[:, None, :]], axis=1)
    shared["lcw"] = f(lcw.reshape(2, 5, 2, 128).transpose(0, 3, 2, 1))
    lv = np.stack([inp["lru_ba"], inp["lru_bi"], inp["lru_lam"]], axis=-1)
    shared["lvec"] = f(np.asarray(lv, dtype=np.float32).reshape(2, 2, 2, 128, 3).transpose(0, 3, 1, 2, 4))
    shared["lwa"] = f(inp["lru_wa"])
    shared["lwi"] = f(inp["lru_wi"])
    w_out = np.asarray(inp["w_out"], dtype=np.float32).copy()
    w_out[:, 0:512, :] = w_out[:, Q_PERM, :]
    shared["w_out"] = f(w_out)
    shared["w_r"] = f(inp["w_router"])
    shared["w_gate"] = f(inp["w_gate"])
    shared["w_up"] = f(inp["w_up"])
    shared["w_down"] = f(inp["w_down"])
    shared["fg"] = f(np.asarray(inp["final_g"]).reshape(1, D))
    shared["rope"] = rope_table()
    cst = np.zeros((128, 3, 128), np.float32)
    cst[:, 0, :] = np.eye(128, dtype=np.float32)
    cst[:, 1, :] = np.triu(np.ones((128, 128), np.float32), 1)
    cst[:, 2, :] = 1.0
    shared["cst"] = cst
    x = np.asarray(inp["x"], dtype=np.float32)
    ctx = np.asarray(inp["ctx"], dtype=np.float32)
    c = np.asarray(inp["c"], dtype=np.float32)
    cc = fm(np.asarray(inp["c_ctx"], dtype=np.float32))
    maps = []
    for b in range(x.shape[0]):
        m = dict(shared)
        m["x"] = np.ascontiguousarray(x[b])
        m["ctx"] = np.ascontiguousarray(ctx[b])
        m["cT"] = f(np.stack([fm(c[b]), cc], axis=-1))
        maps.append(m)
    return maps


_NC_CACHE = {}
DBG = {"units": None, "skip": set()}


def kernel(**inputs):
    maps = prep_inputs(inputs)
    if "nc" not in _NC_CACHE:
        _NC_CACHE["nc"] = build()
    nc = _NC_CACHE["nc"]
    res = run_bass_kernel_spmd(nc, maps, core_ids=list(range(8)))
    return np.stack([np.asarray(r["out"]) for r in res.results], axis=0).astype(np.float32)
```

```python
import contextlib
import numpy as np
import concourse.bass as bass
import concourse.mybir as mybir
from concourse.bass_utils import run_bass_kernel_spmd

F32 = mybir.dt.float32
BF = mybir.dt.bfloat16
I32 = mybir.dt.int32
U32 = mybir.dt.uint32
ALU = mybir.AluOpType
AF = mybir.ActivationFunctionType
AX = mybir.AxisListType

D = 1024
SEQ = 8192
CTX = 256
NTOK = SEQ + CTX
NT = NTOK // 128
NE = 16
CAP = 1024
CAPC = 32
EPS = 1e-6
ROWW = 1064
XS_ROWS = NE * CAP + NE * CAPC
BIGIDX = 1.0e6
ENGS = ("pe", "act", "dve", "pool", "sp")


class Sched:
    def __init__(self, nc, nd=None):
        self.nc = nc
        self.q = {e: [] for e in ENGS}
        self.nd = nd or {"sp": 16, "pool": 16, "act": 4}
        self._stack = []
        self.esem = {}
        self.cnt = {}
        self.dsem = {}
        self.dcnt = {}
        self.dnext = {}
        self.waited = {e: {} for e in ENGS}
        self.last_w = {}
        self.readers = {}
        self.nsem = 0
        self.n_instr = 0
        self.excl = set()

    def _new_sem(self, name):
        cm = self.nc.semaphore(name)
        s = cm.__enter__()
        self._stack.append(cm)
        self.nsem += 1
        return s

    def start(self):
        for e in ENGS:
            self.esem[e] = (self._new_sem(f"es_{e}_0"), 0)
            self.cnt[e] = 0
        for e, n in self.nd.items():
            self.dsem[e] = [self._new_sem(f"ds_{e}_{i}") for i in range(n)]
            self.dcnt[e] = [0] * n
            self.dnext[e] = 0

    def close(self):
        for cm in reversed(self._stack):
            cm.__exit__(None, None, None)
        self._stack = []

    def _need(self, eng, tok):
        kind = tok[0]
        if kind == "e":
            a, gen, n, sem = tok[1], tok[2], tok[3], tok[4]
            if a == eng and eng == "pe":
                return
            key = ("e", a, gen)
        else:
            _, a, i, n = tok
            key = ("d", a, i)
            sem = self.dsem[a][i]
        if self.waited[eng].get(key, 0) >= n:
            return
        self.waited[eng][key] = n
        self.q[eng].append(("wait", sem, n))

    def _deps(self, eng, reads, writes):
        for r in reads:
            t = self.last_w.get(r)
            if t is not None:
                self._need(eng, t)
        for w in writes:
            t = self.last_w.get(w)
            if t is not None:
                self._need(eng, t)
            for t in self.readers.get(w, ()):
                self._need(eng, t)

    def _record(self, tok, reads, writes):
        for r in reads:
            self.readers.setdefault(r, []).append(tok)
        for w in writes:
            self.last_w[w] = tok
            self.readers[w] = []

    def op(self, eng, fn, reads=(), writes=()):
        if self.excl:
            ex = [r for r in reads if r in self.excl]
            if ex:
                reads = [r for r in reads if r not in self.excl]
                writes = list(writes) + ex
        self._deps(eng, reads, writes)
        self.cnt[eng] += 1
        sem, gen = self.esem[eng]
        self.q[eng].append(("op", fn, sem))
        tok = ("e", eng, gen, self.cnt[eng], sem)
        self._record(tok, reads, writes)
        self.n_instr += 1
        return tok

    def dma(self, eng, fn, reads=(), writes=()):
        idx = self.dnext[eng]
        self.dnext[eng] = (idx + 1) % self.nd[eng]
        if self.dcnt[eng][idx] > 0:
            self._need(eng, ("d", eng, idx, self.dcnt[eng][idx] * 16))
        self._deps(eng, reads, writes)
        self.dcnt[eng][idx] += 1
        tok = ("d", eng, idx, self.dcnt[eng][idx] * 16)
        self.q[eng].append(("dma", fn, self.dsem[eng][idx]))
        self._record(tok, reads, writes)
        self.n_instr += 1
        return tok

    def barrier(self):
        for e in ENGS:
            for e2 in ENGS:
                if e2 != e and self.cnt[e2] > 0:
                    sem, gen = self.esem[e2]
                    self._need(e, ("e", e2, gen, self.cnt[e2], sem))
            for e2 in self.nd:
                for i in range(self.nd[e2]):
                    if self.dcnt[e2][i] > 0:
                        self._need(e, ("d", e2, i, self.dcnt[e2][i] * 16))
        self.last_w = {}
        self.readers = {}
        for e in ENGS:
            if self.cnt[e] > 20000:
                gen = self.esem[e][1] + 1
                self.esem[e] = (self._new_sem(f"es_{e}_{gen}"), gen)
                self.cnt[e] = 0

    def emit(self):
        with self.nc.Block() as block:
            def play(engname):
                def body(engine):
                    for item in self.q[engname]:
                        if item[0] == "wait":
                            engine.wait_ge(item[1], item[2])
                        elif item[0] == "op":
                            item[1](engine).then_inc(item[2], 1)
                        else:
                            item[1](engine).then_inc(item[2], 16)
                return body
            block.tensor(play("pe"))
            block.scalar(play("act"))
            block.vector(play("dve"))
            block.gpsimd(play("pool"))
            block.sync(play("sp"))


def run_streams(gens):
    gens = list(gens)
    while gens:
        for g in list(gens):
            try:
                next(g)
            except StopIteration:
                gens.remove(g)


class Ring:
    def __init__(self, tiles, name):
        self.tiles = tiles
        self.name = name
        self.i = -1

    def next(self):
        self.i = (self.i + 1) % len(self.tiles)
        return self.tiles[self.i], (self.name, self.i)


def build(stop_after=None, debug=False, nlayers=2):
    nc = bass.Bass("TRN2", target_bir_lowering=False)
    S = Sched(nc)

    def din(name, shape, dt=F32):
        return nc.dram_tensor(name, list(shape), dt, kind="ExternalInput").ap()

    def dscratch(name, shape, dt):
        kind = "ExternalOutput" if debug else "Internal"
        return nc.dram_tensor(name, list(shape), dt, kind=kind).ap()

    x_in = din("x", [SEQ, D])
    ctx_in = din("ctx", [CTX, D])
    cT_in = din("cT", [128, 8, 2])
    wmod_in = din("w_mod", [2, D, 6 * D])
    bmodT_in = din("b_modT", [2, 128, 48])
    g1T_in = din("g1T", [2, 128, 8])
    g2T_in = din("g2T", [2, 128, 8])
    win_in = din("w_in", [2, D, 2048])
    qkg_in = din("qkg", [2, 2, 64])
    convw_in = din("convw", [2, 128, 2, 3])
    lcw_in = din("lcw", [2, 128, 2, 5])
    lvec_in = din("lvec", [2, 128, 2, 2, 3])
    lwa_in = din("lwa", [2, 2, 4, 64, 64])
    lwi_in = din("lwi", [2, 2, 4, 64, 64])
    wout_in = din("w_out", [2, D, D])
    wr_in = din("w_r", [2, D, NE])
    ew = [2, NE, D, D] if not DBG.get("small_w") else [2, NE, 8, 8]
    wg_in = din("w_gate", ew)
    wu_in = din("w_up", ew)
    wd_in = din("w_down", ew)
    fg_in = din("fg", [1, D])
    rope_in = din("rope", [SEQ, 64])
    cst_in = din("cst", [128, 3, 128])
    out = nc.dram_tensor("out", [SEQ, D], F32, kind="ExternalOutput").ap()

    xa = dscratch("xa", [NTOK, D], F32)
    qT = dscratch("qT", [4, 128, NTOK], BF)
    featT = dscratch("featT", [10, 128, NTOK], F32)
    mixT = dscratch("mixT", [8, 128, NTOK], BF)
    h2D = dscratch("h2D", [NTOK, ROWW], BF)
    xs = dscratch("xs", [XS_ROWS, ROWW], BF)
    hD = dscratch("hD", [2, 2, 128, NTOK], F32)
    wbf = nc.dram_tensor("wbf", [3, NE, D, D], BF, kind="Internal").ap() if not DBG.get("small_w") else None
    woutbf = nc.dram_tensor("woutbf", [D, D], BF, kind="Internal").ap()
    winbf = nc.dram_tensor("winbf", [D, 2048], BF, kind="Internal").ap()
    dbg = dscratch("dbg", [128, 4096], F32) if debug else None

    top = contextlib.ExitStack()

    uniq = [0]

    def sb(es, name, shape, dt):
        uniq[0] += 1
        return es.enter_context(nc.sbuf_tensor(f"s{uniq[0]}_{name}", list(shape), dt))

    def ps(es, name, shape, dt, key=None):
        nbytes = int(np.prod(shape[1:])) * (4 if dt in (F32, I32, U32) else 2)
        assert nbytes in (2048, 4096), (name, shape, nbytes)
        S.excl.add(key if key is not None else name)
        uniq[0] += 1
        return es.enter_context(nc.psum_tensor(f"p{uniq[0]}_{name}", list(shape), dt))

    def ring(es, name, n, shape, dt, psum=False):
        if psum:
            return Ring([ps(es, f"{name}{i}", shape, dt, key=(name, i)) for i in range(n)], name)
        return Ring([sb(es, f"{name}{i}", shape, dt) for i in range(n)], name)

    pool_regs = {}

    def preg(en, val):
        if val not in pool_regs:
            pool_regs[val] = en.to_reg(val)
        return pool_regs[val]

    def I(eng, name, reads, writes, **kw):
        return S.op(eng, lambda e, kw=kw, name=name: getattr(e, name)(**kw), reads, writes)

    def DMA(eng, reads, writes, **kw):
        return S.dma(eng, lambda e, kw=kw: e.dma_start(**kw), reads, writes)

    S.start()

    cst_f = sb(top, "cst_f", [128, 3, 128], F32)
    ident_b = sb(top, "ident_b", [128, 128], BF)
    ltri_b = sb(top, "ltri_b", [128, 128], BF)
    ones_b = sb(top, "ones_b", [128, 128], BF)
    modT = sb(top, "modT", [128, 2, 48, 2], F32)
    aff_all = sb(top, "aff_all", [128, NT, NE], F32)
    tokid = sb(top, "tokid", [128, NT], I32)
    zcol = sb(top, "zcol", [128, 1], F32)
    ident_f = cst_f[:, 0, :]
    ltri_f = cst_f[:, 1, :]
    ones_f = cst_f[:, 2, :]

    DMA("sp", [], ["cst_f"], out=cst_f[:], in_=cst_in)
    I("dve", "tensor_copy", ["cst_f"], ["ident_b"], out=ident_b[:], in_=ident_f)
    I("dve", "tensor_copy", ["cst_f"], ["ltri_b"], out=ltri_b[:], in_=ltri_f)
    I("dve", "tensor_copy", ["cst_f"], ["ones_b"], out=ones_b[:], in_=ones_f)
    I("pool", "iota", [], ["tokid"], out=tokid[:], pattern=[[128, NT]], base=0, channel_multiplier=1)
    I("pool", "memset", [], ["zcol"], ap=zcol[:], constant=0.0)

    def rstd_chain(ss, ssk, tmp, tmpk, rs, rsk, n, inv_n):
        I("dve", "tensor_scalar", [ssk], [tmpk], out=tmp, in0=ss, scalar1=inv_n, scalar2=EPS,
          op0=ALU.mult, op1=ALU.add)
        I("act", "activation", [tmpk], [tmpk], out=tmp, in_=tmp, func=AF.Sqrt)
        I("dve", "reciprocal", [tmpk], [rsk], out=rs, in_=tmp)

    with contextlib.ExitStack() as es:
        cT_sb = sb(es, "cT_sb", [128, 8, 2], F32)
        sc = sb(es, "sc", [128, 8, 2], F32)
        bT = sb(es, "bT", [128, 2, 48], F32)
        wm = ring(es, "wm", 2, [128, 6 * D], F32)
        pm = ring(es, "pm", 2, [128, 512], F32, psum=True)
        DMA("sp", [], ["cT_sb"], out=cT_sb[:], in_=cT_in)
        DMA("sp", [], ["bT"], out=bT[:], in_=bmodT_in.rearrange("l p n -> p l n"))
        I("act", "activation", ["cT_sb"], ["sc"], out=sc[:], in_=cT_sb[:], func=AF.Silu)
        for l in range(nlayers):
            acc = modT[:, l].rearrange("p n s -> p (n s)")
            for kc in range(8):
                w, wk = wm.next()
                DMA("sp", [], [wk], out=w[:], in_=wmod_in[l, kc * 128:(kc + 1) * 128, :])
                p, pk = pm.next()
                for n in range(48):
                    I("pe", "matmul", [wk, "sc"], [pk], out=p[:, 2 * n:2 * n + 2],
                      lhsT=w[:, n * 128:(n + 1) * 128], rhs=sc[:, kc, :], start=True, stop=True)
                if kc == 0:
                    I("dve", "tensor_copy", [pk], [("modT", l)], out=acc, in_=p[:, 0:96])
                else:
                    I("dve", "tensor_tensor", [pk, ("modT", l)], [("modT", l)], out=acc, in0=acc, in1=p[:, 0:96],
                      op=ALU.add)
            I("dve", "tensor_tensor", ["bT", ("modT", l)], [("modT", l)], out=modT[:, l], in0=modT[:, l],
              in1=bT[:, l, :].unsqueeze(2).to_broadcast([128, 48, 2]), op=ALU.add)
        if debug:
            DMA("sp", [("modT", 0), ("modT", 1)], ["dbg"], out=dbg[:, 0:192],
                in_=modT[:].rearrange("p l n s -> p (l n s)"))
    S.barrier()
    if stop_after == "p0":
        return finish(nc, S, top)

    def src_rows(l, r0, n):
        if l == 0:
            if r0 < CTX:
                return ctx_in[r0:r0 + n, :]
            return x_in[r0 - CTX:r0 - CTX + n, :]
        return xa[r0:r0 + n, :]

    def make_bc(es_unused, dst, dstk, src_col, srck, tmpD, tmpDk, pbc, pbck, eng_toggle=[0]):
        for h in range(2):
            for j in range(4):
                kc = h * 4 + j
                I("dve", "tensor_scalar", ["cst_f", srck], [tmpDk], out=tmpD[:], in0=ident_f,
                  scalar1=src_col(kc), scalar2=None, op0=ALU.mult)
                I("pe", "matmul", [tmpDk, "cst_f"], [pbck], out=pbc[:, j * 128:(j + 1) * 128], lhsT=ones_f,
                  rhs=tmpD[:], start=True, stop=True)
            I("act", "copy", [pbck], [dstk], out=dst[:, h * 512:(h + 1) * 512], in_=pbc[:])

    units = [(0, 2, True)] + [(CTX + 512 * i, 4, False) for i in range(16)]

    for l in range(nlayers):
        need_ctx = l < nlayers - 1 or (nlayers == 1 and debug)
        last_layer = (l == 1)
        with contextlib.ExitStack() as LS:
            G1T = sb(LS, "G1T", [128, 8, 2], F32)
            G2T = sb(LS, "G2T", [128, 8, 2], F32)
            gT = sb(LS, "gT", [128, 2, 8], F32)
            DMA("sp", [], ["gT"], out=gT[:, 0, :], in_=g1T_in[l])
            DMA("sp", [], ["gT"], out=gT[:, 1, :], in_=g2T_in[l])

            def modsl(i):
                return modT[:, l, i * 8:(i + 1) * 8, :]
            I("dve", "tensor_scalar", [], ["G1T"], out=G1T[:], in0=modsl(1), scalar1=1.0, scalar2=None, op0=ALU.add)
            I("dve", "tensor_tensor", ["G1T", "gT"], ["G1T"], out=G1T[:], in0=G1T[:],
              in1=gT[:, 0, :].unsqueeze(2).to_broadcast([128, 8, 2]), op=ALU.mult)
            I("dve", "tensor_scalar", [], ["G2T"], out=G2T[:], in0=modsl(4), scalar1=1.0, scalar2=None, op0=ALU.add)
            I("dve", "tensor_tensor", ["G2T", "gT"], ["G2T"], out=G2T[:], in0=G2T[:],
              in1=gT[:, 1, :].unsqueeze(2).to_broadcast([128, 8, 2]), op=ALU.mult)
            sh1T = modsl(0)
            sh2T = modsl(3)

            with contextlib.ExitStack() as KV:
                KTz = [sb(KV, f"KTz{i}", [128, NTOK], BF) for i in range(2)]
                Vp = sb(KV, "Vp", [128, NT, 2, 128], BF)
                if "memsets" not in DBG["skip"]:
                    I("pool", "memset", [], ["KTz0z"], ap=KTz[0][64:128, :], constant=0.0)
                    I("pool", "memset", [], ["KTz1z"], ap=KTz[1][0:64, :], constant=0.0)
                    I("pool", "memset", [], ["Vp1"], ap=Vp[:, :, 0, 64:128], constant=1.0)
                    I("pool", "memset", [], ["Vp1"], ap=Vp[:, :, 1, 0:64], constant=1.0)

                with contextlib.ExitStack() as es:
                    win = sb(es, "win", [128, 8, 2048], BF)
                    if l == 0:
                        for kc in range(8):
                            for hh in range(2):
                                DMA("pool", [], ["win"], out=win[:, kc, hh * 1024:(hh + 1) * 1024],
                                    in_=win_in[l, kc * 128:(kc + 1) * 128, hh * 1024:(hh + 1) * 1024])
                    else:
                        for h_ in range(2):
                            DMA("sp", [], ["win"], out=win[:, 4 * h_:4 * h_ + 4, :],
                                in_=winbf[512 * h_:512 * (h_ + 1), :].rearrange("(k p) n -> p k n", p=128))
                    gqk = sb(es, "gqk", [128, 2, 64], F32)
                    if "gqk" not in DBG["skip"]:
                        DMA("sp", [], ["gqk"], out=gqk[:].rearrange("p a b -> p (a b)"),
                            in_=qkg_in[l:l + 1].rearrange("o a b -> o (a b)").to_broadcast([128, 128]))
                    I("dve", "tensor_scalar", ["gqk"], ["gqk"], out=gqk[:, 0, :], in0=gqk[:, 0, :], scalar1=0.125,
                      scalar2=None, op0=ALU.mult)
                    xt_r = ring(es, "xt", 3, [128, D], F32)
                    xn_r = ring(es, "xn", 2, [128, D], BF)
                    junk = sb(es, "junk", [128, D], BF)
                    sm_r = ring(es, "sm", 3, [128, 4], F32)
                    NHX = 3
                    hx_t = [sb(es, f"hxT{i}", [128, 8, 512], BF) for i in range(NHX)]
                    qTs_t = [sb(es, f"qTs{i}", [128, 4, 512], BF) for i in range(2)]
                    fs_r = ring(es, "fs", 3, [128, 512], F32)
                    pT_r = ring(es, "pT", 1, [128, 8, 128], BF, psum=True)
                    pf_r = ring(es, "pf", 1, [128, 512], F32, psum=True)
                    gq_bc = gqk[:, 0, :].unsqueeze(1).to_broadcast([128, 8, 64])
                    gk_bc = gqk[:, 1, :].unsqueeze(1).to_broadcast([128, 2, 64])
                    ulist = units if DBG["units"] is None else units[:DBG["units"]]
                    prog1 = {"a": 0, "b1": 0, "b2": 0, "b1n": {}}

                    def a_stream():
                        for ui, (tok0, ntl, is_ctx) in enumerate(ulist):
                            while min(prog1["b1"], prog1["b2"]) < ui - (NHX - 1):
                                yield
                            s = 1 if is_ctx else 0
                            hx, hxk = hx_t[ui % NHX], ("hx", ui % NHX)
                            for ti in range(ntl):
                                t = tok0 // 128 + ti
                                xt, xtk = xt_r.next()
                                DMA("sp", [], [xtk], out=xt[:], in_=src_rows(l, tok0 + ti * 128, 128))
                                sm, smk = sm_r.next()
                                I("pool", "memset", [], [smk], ap=sm[:, 0:1], constant=0.0)
                                yield
                                I("act", "activation", [xtk, smk], ["junk", smk], out=junk[:], in_=xt[:], func=AF.Square,
                                  accum_out=sm[:, 0:1])
                                yield
                                I("dve", "tensor_scalar", [smk], [smk], out=sm[:, 1:2], in0=sm[:, 0:1], scalar1=1.0 / D,
                                  scalar2=EPS, op0=ALU.mult, op1=ALU.add)
                                yield
                                I("act", "activation", [smk], [smk], out=sm[:, 1:2], in_=sm[:, 1:2], func=AF.Sqrt)
                                yield
                                I("dve", "reciprocal", [smk], [smk], out=sm[:, 2:3], in_=sm[:, 1:2])
                                yield
                                xn, xnk = xn_r.next()
                                I("dve", "tensor_scalar", [xtk, smk], [xnk], out=xn[:], in0=xt[:], scalar1=sm[:, 2:3],
                                  scalar2=None, op0=ALU.mult)
                                yield
                                pT, pTk = pT_r.next()
                                for kc in range(8):
                                    I("pe", "transpose", [xnk, "ident_b"], [pTk], out=pT[:, kc, :],
                                      in_=xn[:, kc * 128:(kc + 1) * 128], identity=ident_b[:])
                                yield
                                for kc in range(8):
                                    dst = hx[:, kc, ti * 128:(ti + 1) * 128]
                                    if t % 2 == 0:
                                        I("act", "activation", [pTk, "G1T"], [hxk], out=dst, in_=pT[:, kc, :],
                                          func=AF.Identity, scale=G1T[:, kc, s:s + 1], bias=sh1T[:, kc, s:s + 1])
                                    else:
                                        I("dve", "tensor_scalar", [pTk, "G1T"], [hxk], out=dst, in0=pT[:, kc, :],
                                          scalar1=G1T[:, kc, s:s + 1], scalar2=sh1T[:, kc, s:s + 1],
                                          op0=ALU.mult, op1=ALU.add)
                                    if kc % 2 == 1:
                                        yield
                            prog1["a"] = ui + 1

                    def b1_stream(par):
                        P = f"b1{par}_"
                        pq = ps(es, P + "pq", [128, 512], F32)
                        pkv = ps(es, P + "pkv", [128, 512], F32)
                        pqT = ps(es, P + "pqT", [128, 8, 128], BF)
                        pqk, pkvk, pqTk = P + "pq", P + "pkv", P + "pqT"
                        sq_r = ring(es, P + "sq", 1, [128, 640], F32)
                        qs_r = ring(es, P + "qs", 1, [128, 32], F32)
                        qn_r = ring(es, P + "qn", 1, [128, 10, 64], F32)
                        ra_r = ring(es, P + "ra", 1, [128, 10, 32], F32)
                        rb_r = ring(es, P + "rb", 1, [128, 10, 32], F32)
                        qr_r = ring(es, P + "qr", 1, [128, 10, 64], BF)
                        rope_r = ring(es, P + "rope", 1, [128, 64], F32)
                        for ui, (tok0, ntl, is_ctx) in enumerate(ulist):
                            while prog1["a"] <= ui:
                                yield
                            NTu = ntl * 128
                            hx, hxk = hx_t[ui % NHX], ("hx", ui % NHX)
                            qTs, qTsk = qTs_t[ui % 2], ("qTs", ui % 2)
                            for ti in range(par, ntl, 2):
                                t = tok0 // 128 + ti
                                for kc in range(8):
                                    I("pe", "matmul", [hxk, "win"], [pqk], out=pq[:],
                                      lhsT=hx[:, kc, ti * 128:(ti + 1) * 128], rhs=win[:, kc, 0:512],
                                      start=(kc == 0), stop=(kc == 7))
                                yield
                                for kc in range(8):
                                    I("pe", "matmul", [hxk, "win"], [pkvk], out=pkv[:, 0:256],
                                      lhsT=hx[:, kc, ti * 128:(ti + 1) * 128], rhs=win[:, kc, 512:768],
                                      start=(kc == 0), stop=(kc == 7))
                                yield
                                sq, sqk = sq_r.next()
                                qs, qsk = qs_r.next()
                                qn, qnk = qn_r.next()
                                qr, qrk = qr_r.next()
                                I("act", "activation", [pqk], [sqk], out=sq[:, 0:512], in_=pq[:], func=AF.Square)
                                I("act", "activation", [pkvk], [sqk], out=sq[:, 512:640], in_=pkv[:, 0:128],
                                  func=AF.Square)
                                yield
                                I("dve", "reduce_sum", [sqk], [qsk], out=qs[:, 0:10],
                                  in_=sq[:].rearrange("p (h d) -> p h d", d=64), axis=AX.X)
                                yield
                                I("dve", "tensor_scalar", [qsk], [qsk], out=qs[:, 10:20], in0=qs[:, 0:10], scalar1=1.0 / 64,
                                  scalar2=EPS, op0=ALU.mult, op1=ALU.add)
                                yield
                                I("act", "activation", [qsk], [qsk], out=qs[:, 10:20], in_=qs[:, 10:20], func=AF.Sqrt)
                                yield
                                I("dve", "reciprocal", [qsk], [qsk], out=qs[:, 20:30], in_=qs[:, 10:20])
                                yield
                                I("dve", "tensor_tensor", [pqk, qsk], [qnk], out=qn[:, 0:8, :],
                                  in0=pq[:].rearrange("p (h d) -> p h d", d=64),
                                  in1=qs[:, 20:28].unsqueeze(2).to_broadcast([128, 8, 64]), op=ALU.mult)
                                I("dve", "tensor_tensor", [pkvk, qsk, qnk], [qnk], out=qn[:, 8:10, :],
                                  in0=pkv[:, 0:128].rearrange("p (h d) -> p h d", d=64),
                                  in1=qs[:, 28:30].unsqueeze(2).to_broadcast([128, 2, 64]), op=ALU.mult)
                                I("act", "copy", [pkvk], [("Vp", t)], out=Vp[:, t, 0, 0:64], in_=pkv[:, 128:192])
                                I("act", "copy", [pkvk, ("Vp", t)], [("Vp", t)], out=Vp[:, t, 1, 64:128],
                                  in_=pkv[:, 192:256])
                                yield
                                if is_ctx:
                                    I("pool", "tensor_tensor", [qnk, "gqk"], [qrk], out=qr[:, 0:8, :], in0=qn[:, 0:8, :],
                                      in1=gq_bc, op=ALU.mult)
                                    I("pool", "tensor_tensor", [qnk, "gqk", qrk], [qrk], out=qr[:, 8:10, :],
                                      in0=qn[:, 8:10, :], in1=gk_bc, op=ALU.mult)
                                    yield
                                else:
                                    I("pool", "tensor_tensor", [qnk, "gqk"], [qnk], out=qn[:, 0:8, :], in0=qn[:, 0:8, :],
                                      in1=gq_bc, op=ALU.mult)
                                    I("pool", "tensor_tensor", [qnk, "gqk"], [qnk], out=qn[:, 8:10, :], in0=qn[:, 8:10, :],
                                      in1=gk_bc, op=ALU.mult)
                                    rp, rpk = rope_r.next()
                                    DMA("sp", [], [rpk], out=rp[:], in_=rope_in[(t - 2) * 128:(t - 1) * 128, :])
                                    yield
                                    ra, rak = ra_r.next()
                                    rb, rbk = rb_r.next()
                                    cosb = rp[:, 0:32].unsqueeze(1).to_broadcast([128, 10, 32])
                                    sinb = rp[:, 32:64].unsqueeze(1).to_broadcast([128, 10, 32])
                                    t1 = qn[:, :, 0:32]
                                    t2 = qn[:, :, 32:64]
                                    I("dve", "tensor_tensor", [qnk, rpk], [rak], out=ra[:], in0=t1, in1=cosb, op=ALU.mult)
                                    I("pool", "tensor_tensor", [qnk, rpk], [rbk], out=rb[:], in0=t2, in1=sinb, op=ALU.mult)
                                    yield
                                    I("dve", "tensor_tensor", [rak, rbk], [qrk], out=qr[:, :, 0:32], in0=ra[:], in1=rb[:],
                                      op=ALU.subtract)
                                    yield
                                    I("dve", "tensor_tensor", [qnk, rpk, rak], [rak], out=ra[:], in0=t1, in1=sinb,
                                      op=ALU.mult)
                                    I("pool", "tensor_tensor", [qnk, rpk, rbk], [rbk], out=rb[:], in0=t2, in1=cosb,
                                      op=ALU.mult)
                                    yield
                                    I("dve", "tensor_tensor", [rak, rbk, qrk], [qrk], out=qr[:, :, 32:64], in0=ra[:],
                                      in1=rb[:], op=ALU.add)
                                    yield
                                qrf = qr[:].rearrange("p h d -> p (h d)")
                                for j in range(5):
                                    I("pe", "transpose", [qrk, "ident_b"], [pqTk], out=pqT[:, j, :],
                                      in_=qrf[:, j * 128:(j + 1) * 128], identity=ident_b[:])
                                yield
                                I("act", "copy", [pqTk], [(qTsk, ti)], out=qTs[:, :, ti * 128:(ti + 1) * 128],
                                  in_=pqT[:, 0:4, :])
                                I("act", "copy", [pqTk], [("KT", t)], out=KTz[0][0:64, t * 128:(t + 1) * 128],
                                  in_=pqT[0:64, 4, :])
                                I("act", "copy", [pqTk, ("KT", t)], [("KT", t)],
                                  out=KTz[1][64:128, t * 128:(t + 1) * 128], in_=pqT[64:128, 4, :])
                                yield
                            prog1["b1n"][ui] = prog1["b1n"].get(ui, 0) + 1
                            if prog1["b1n"][ui] == 2:
                                DMA("pool", [(qTsk, ti) for ti in range(ntl)], [("qT", tok0)],
                                    out=qT[:, :, tok0:tok0 + NTu].rearrange("j p n -> p j n"), in_=qTs[:, :, 0:NTu])
                                prog1["b1"] = ui + 1

                    def b2_stream():
                        for ui, (tok0, ntl, is_ctx) in enumerate(ulist):
                            while prog1["a"] <= ui:
                                yield
                            NTu = ntl * 128
                            hx, hxk = hx_t[ui % NHX], ("hx", ui % NHX)
                            for n in range(10):
                                pf, pfk = pf_r.next()
                                for kc in range(8):
                                    I("pe", "matmul", [hxk, "win"], [pfk], out=pf[:, 0:NTu],
                                      lhsT=win[:, kc, 768 + n * 128:768 + (n + 1) * 128], rhs=hx[:, kc, 0:NTu],
                                      start=(kc == 0), stop=(kc == 7))
                                    if kc % 4 == 3:
                                        yield
                                fs, fsk = fs_r.next()
                                if n % 2 == 0:
                                    I("act", "copy", [pfk], [fsk], out=fs[:, 0:NTu], in_=pf[:, 0:NTu])
                                else:
                                    I("dve", "tensor_copy", [pfk], [fsk], out=fs[:, 0:NTu], in_=pf[:, 0:NTu])
                                DMA("pool", [fsk], [("featT", n, tok0)], out=featT[n, :, tok0:tok0 + NTu],
                                    in_=fs[:, 0:NTu])
                                yield
                            prog1["b2"] = ui + 1
                    run_streams([a_stream(), b1_stream(0), b1_stream(1), b2_stream()])
                S.barrier()
                if stop_after == f"p1_{l}":
                    if debug:
                        with contextlib.ExitStack() as es:
                            kd = sb(es, "kd", [128, 1024], F32)
                            I("dve", "tensor_copy", [], ["kd"], out=kd[:, 0:512], in_=KTz[0][:, 0:512])
                            I("dve", "tensor_copy", ["kd"], ["kd"], out=kd[:, 512:1024], in_=KTz[1][:, 0:512])
                            DMA("sp", ["kd"], ["dbg"], out=dbg[:, 0:1024], in_=kd[:])
                            vd = sb(es, "vd", [128, 1024], F32)
                            I("dve", "tensor_copy", [], ["vd"], out=vd[:],
                              in_=Vp[:, 0:4].rearrange("p t h d -> p (t h d)"))
                            DMA("sp", ["vd"], ["dbg"], out=dbg[:, 1024:2048], in_=vd[:])
                    return finish(nc, S, top)

                def seq_bounds(t0):
                    return (0, CTX) if t0 < CTX else (CTX, NTOK)

                CSEG = 512
                segs_c = [(0, CTX)] + [(CTX + CSEG * i, CSEG) for i in range(SEQ // CSEG)]
                SEG = 256
                segs_l = [(0, CTX)] + [(CTX + SEG * i, SEG) for i in range(SEQ // SEG)]
                GSEG = 512
                segs_g = [(0, CTX)] + [(CTX + GSEG * i, GSEG) for i in range(SEQ // GSEG)]
                with contextlib.ExitStack() as es:
                    cw = sb(es, "cw", [128, 2, 3], F32)
                    DMA("sp", [], ["cw"], out=cw[:], in_=convw_in[l])
                    lcw = sb(es, "lcw", [128, 2, 5], F32)
                    lvec = sb(es, "lvec", [128, 2, 2, 3], F32)
                    nlv = sb(es, "nlv", [128, 2, 2, 2], F32)
                    cneg = sb(es, "cneg", [128, 2, 2], F32)
                    DMA("sp", [], ["lcw"], out=lcw[:], in_=lcw_in[l])
                    DMA("sp", [], ["lvec"], out=lvec[:], in_=lvec_in[l])
                    I("dve", "tensor_scalar", ["lvec"], ["nlv"], out=nlv[:], in0=lvec[:, :, :, 0:2], scalar1=-1.0,
                      scalar2=None, op0=ALU.mult)
                    I("act", "activation", ["lvec"], ["cneg"], out=cneg[:], in_=lvec[:, :, :, 2], func=AF.Exp, scale=-1.0)
                    I("dve", "tensor_scalar", ["cneg"], ["cneg"], out=cneg[:], in0=cneg[:], scalar1=1.0, scalar2=None,
                      op0=ALU.add)
                    I("act", "activation", ["cneg"], ["cneg"], out=cneg[:], in_=cneg[:], func=AF.Ln)
                    I("dve", "tensor_scalar", ["cneg"], ["cneg"], out=cneg[:], in0=cneg[:], scalar1=-8.0, scalar2=None,
                      op0=ALU.mult)
                    Wblk = sb(es, "Wblk", [128, 2, 2, 2, 128], F32)
                    I("pool", "memset", [], ["Wblk"], ap=Wblk[:], constant=0.0)
                    for c in range(2):
                        for d in range(2):
                            for bi_ in range(2):
                                blk = 2 * c + bi_
                                DMA("sp", [], ["Wblk"],
                                    out=Wblk[bi_ * 64:(bi_ + 1) * 64, c, d, 0, bi_ * 64:(bi_ + 1) * 64], in_=lwa_in[l, d, blk])
                                DMA("sp", [], ["Wblk"],
                                    out=Wblk[bi_ * 64:(bi_ + 1) * 64, c, d, 1, bi_ * 64:(bi_ + 1) * 64], in_=lwi_in[l, d, blk])
                    prog2 = {"scan": [0, 0]}

                    def conv_stream(c):
                        P = f"cv{c}_"
                        ccs = sb(es, P + "ccs", [128, CSEG + 2], F32)
                        chs = sb(es, P + "chs", [128, CSEG + 2], F32)
                        cbs = sb(es, P + "cbs", [128, CSEG], F32)
                        uu = sb(es, P + "uu", [128, CSEG + 2], F32)
                        yy = sb(es, P + "yy", [128, CSEG], F32)
                        oo = sb(es, P + "oo", [128, CSEG], BF)
                        for (t0, n) in segs_c:
                            if t0 < CTX and not need_ctx:
                                continue
                            lo_s, hi_s = seq_bounds(t0)
                            lo = max(t0 - 1, lo_s)
                            hi = min(t0 + n + 1, hi_s)
                            off = lo - (t0 - 1)
                            DMA("sp", [], [P + "ccs"], out=ccs[:, off:off + hi - lo], in_=featT[2 + c, :, lo:hi])
                            DMA("sp", [], [P + "chs"], out=chs[:, off:off + hi - lo], in_=featT[4 + c, :, lo:hi])
                            DMA("sp", [], [P + "cbs"], out=cbs[:, 0:n], in_=featT[0 + c, :, t0:t0 + n])
                            yield
                            I("pool", "tensor_tensor", [P + "ccs", P + "chs"], [P + "uu"], out=uu[:, off:off + hi - lo],
                              in0=ccs[:, off:off + hi - lo], in1=chs[:, off:off + hi - lo], op=ALU.mult)
                            if off == 1:
                                I("pool", "memset", [P + "uu"], [P + "uu"], ap=uu[:, 0:1], constant=0.0)
                            if hi < t0 + n + 1:
                                I("pool", "memset", [P + "uu"], [P + "uu"], ap=uu[:, n + 1:n + 2], constant=0.0)
                            yield
                            I("dve", "tensor_scalar", [P + "uu", "cw"], [P + "yy"], out=yy[:, 0:n], in0=uu[:, 1:n + 1],
                              scalar1=cw[:, c, 1:2], scalar2=None, op0=ALU.mult)
                            yield
                            I("dve", "scalar_tensor_tensor", [P + "uu", "cw", P + "yy"], [P + "yy"], out=yy[:, 0:n],
                              in0=uu[:, 0:n], scalar=cw[:, c, 0:1], in1=yy[:, 0:n], op0=ALU.mult, op1=ALU.add)
                            yield
                            I("dve", "scalar_tensor_tensor", [P + "uu", "cw", P + "yy"], [P + "yy"], out=yy[:, 0:n],
                              in0=uu[:, 2:n + 2], scalar=cw[:, c, 2:3], in1=yy[:, 0:n], op0=ALU.mult, op1=ALU.add)
                            yield
                            I("dve", "tensor_tensor", [P + "yy", P + "cbs"], [P + "oo"], out=oo[:, 0:n], in0=yy[:, 0:n],
                              in1=cbs[:, 0:n], op=ALU.mult)
                            DMA("pool", [P + "oo"], [("mixT", 4 + c, t0)], out=mixT[4 + c, :, t0:t0 + n], in_=oo[:, 0:n])
                            yield

                    def scan_lane(d):
                        P = f"ls{d}_"
                        lus = sb(es, P + "lus", [128, SEG + 3], F32)
                        uc = sb(es, P + "uc", [128, SEG], F32)
                        rr = sb(es, P + "rr", [128, SEG], F32)
                        ii = sb(es, P + "ii", [128, SEG], F32)
                        tt = sb(es, P + "tt", [128, SEG], F32)
                        hh = sb(es, P + "hh", [128, SEG], F32)
                        carry = sb(es, P + "carry", [128, 1], F32)
                        pp = ps(es, P + "pp", [128, 512], F32)
                        order = segs_l if d == 0 else [segs_l[0]] + segs_l[:0:-1]
                        for c in range(2):
                            I("dve", "tensor_copy", ["zcol"], [P + "carry"], out=carry[:], in_=zcol[:])
                            for (t0, n) in order:
                                lo_s, hi_s = seq_bounds(t0)
                                lo = max(t0 - 2, lo_s)
                                hi = min(t0 + n + 1, hi_s)
                                off = lo - (t0 - 2)
                                if off > 0:
                                    I("pool", "memset", [], [P + "lus"], ap=lus[:, 0:2], constant=0.0)
                                if hi < t0 + n + 1:
                                    I("pool", "memset", [], [P + "lus"], ap=lus[:, n + 2:n + 3], constant=0.0)
                                DMA("sp", [], [P + "lus"], out=lus[:, off:off + hi - lo], in_=featT[6 + c, :, lo:hi])
                                yield
                                I("dve", "tensor_scalar", [P + "lus", "lcw"], [P + "uc"], out=uc[:, 0:n], in0=lus[:, 2:n + 2],
                                  scalar1=lcw[:, c, 2:3], scalar2=lcw[:, c, 4:5], op0=ALU.mult, op1=ALU.add)
                                yield
                                for k_, o_ in ((0, 0), (1, 1), (3, 3)):
                                    I("dve", "scalar_tensor_tensor", [P + "lus", "lcw", P + "uc"], [P + "uc"], out=uc[:, 0:n],
                                      in0=lus[:, o_:o_ + n], scalar=lcw[:, c, k_:k_ + 1], in1=uc[:, 0:n],
                                      op0=ALU.mult, op1=ALU.add)
                                    yield
                                I("pe", "matmul", ["Wblk", P + "uc"], [P + "pp"], out=pp[:, 0:n], lhsT=Wblk[:, c, d, 0, :],
                                  rhs=uc[:, 0:n], start=True, stop=True)
                                I("pe", "matmul", ["Wblk", P + "uc"], [P + "pp"], out=pp[:, 256:256 + n],
                                  lhsT=Wblk[:, c, d, 1, :], rhs=uc[:, 0:n], start=True, stop=True)
                                yield
                                I("act", "activation", [P + "pp", "nlv"], [P + "rr"], out=rr[:, 0:n], in_=pp[:, 0:n],
                                  func=AF.Exp, scale=-1.0, bias=nlv[:, d, c, 0:1])
                                I("act", "activation", [P + "pp", "nlv"], [P + "ii"], out=ii[:, 0:n], in_=pp[:, 256:256 + n],
                                  func=AF.Exp, scale=-1.0, bias=nlv[:, d, c, 1:2])
                                yield
                                I("dve", "tensor_scalar", [P + "rr"], [P + "rr"], out=rr[:, 0:n], in0=rr[:, 0:n], scalar1=1.0,
                                  scalar2=None, op0=ALU.add)
                                yield
                                I("dve", "reciprocal", [P + "rr"], [P + "rr"], out=rr[:, 0:n], in_=rr[:, 0:n])
                                yield
                                I("pool", "tensor_scalar", [P + "ii"], [P + "ii"], out=ii[:, 0:n], in0=ii[:, 0:n], scalar1=1.0,
                                  scalar2=None, op0=ALU.add)
                                yield
                                I("dve", "reciprocal", [P + "ii"], [P + "ii"], out=ii[:, 0:n], in_=ii[:, 0:n])
                                yield
                                I("act", "activation", [P + "rr", "cneg"], [P + "rr"], out=rr[:, 0:n], in_=rr[:, 0:n],
                                  func=AF.Exp, scale=cneg[:, d, c:c + 1])
                                I("pool", "tensor_tensor", [P + "ii", P + "uc"], [P + "ii"], out=ii[:, 0:n], in0=ii[:, 0:n],
                                  in1=uc[:, 0:n], op=ALU.mult)
                                yield
                                I("pool", "tensor_tensor", [P + "rr"], [P + "tt"], out=tt[:, 0:n], in0=rr[:, 0:n],
                                  in1=rr[:, 0:n], op=ALU.mult)
                                yield
                                I("act", "activation", [P + "tt"], [P + "tt"], out=tt[:, 0:n], in_=tt[:, 0:n], func=AF.Ln,
                                  scale=-1.0, bias=1.0)
                                I("act", "activation", [P + "tt"], [P + "tt"], out=tt[:, 0:n], in_=tt[:, 0:n], func=AF.Exp,
                                  scale=0.5)
                                yield
                                I("dve", "tensor_tensor", [P + "ii", P + "tt"], [P + "ii"], out=ii[:, 0:n], in0=ii[:, 0:n],
                                  in1=tt[:, 0:n], op=ALU.mult)
                                yield
                                if d == 0:
                                    I("dve", "tensor_tensor_scan", [P + "rr", P + "ii", P + "carry"], [P + "hh"],
                                      out=hh[:, 0:n], data0=rr[:, 0:n], data1=ii[:, 0:n], initial=carry[:, 0:1],
                                      op0=ALU.mult, op1=ALU.add)
                                    I("dve", "tensor_copy", [P + "hh", P + "carry"], [P + "carry"], out=carry[:],
                                      in_=hh[:, n - 1:n])
                                else:
                                    I("dve", "tensor_tensor_scan", [P + "rr", P + "ii", P + "carry"], [P + "hh"],
                                      out=hh[:, 0:n][:, ::-1], data0=rr[:, 0:n][:, ::-1], data1=ii[:, 0:n][:, ::-1],
                                      initial=carry[:, 0:1], op0=ALU.mult, op1=ALU.add)
                                    I("dve", "tensor_copy", [P + "hh", P + "carry"], [P + "carry"], out=carry[:],
                                      in_=hh[:, 0:1])
                                if not (t0 < CTX and not need_ctx):
                                    DMA("pool", [P + "hh"], [("hD", d, c, t0)], out=hD[d, c, :, t0:t0 + n], in_=hh[:, 0:n])
                                yield
                            prog2["scan"][c] += 1

                    def comb_stream(c):
                        P = f"cb{c}_"
                        h0 = sb(es, P + "h0", [128, GSEG], F32)
                        h1 = sb(es, P + "h1", [128, GSEG], F32)
                        lgs = sb(es, P + "lgs", [128, GSEG], F32)
                        tt = sb(es, P + "tt", [128, GSEG], F32)
                        ob = sb(es, P + "ob", [128, GSEG], BF)
                        while prog2["scan"][c] < 2:
                            yield
                        for si, (t0, n) in enumerate(segs_g):
                            if t0 < CTX and not need_ctx:
                                continue
                            rk = [("hD", d_, c, t0 + o_) for d_ in range(2) for o_ in range(0, n, SEG)]
                            DMA("sp", rk, [P + "h0"], out=h0[:, 0:n], in_=hD[0, c, :, t0:t0 + n])
                            DMA("sp", rk, [P + "h1"], out=h1[:, 0:n], in_=hD[1, c, :, t0:t0 + n])
                            DMA("sp", [], [P + "lgs"], out=lgs[:, 0:n], in_=featT[8 + c, :, t0:t0 + n])
                            yield
                            I("pool", "tensor_tensor", [P + "h0", P + "h1"], [P + "h0"], out=h0[:, 0:n], in0=h0[:, 0:n],
                              in1=h1[:, 0:n], op=ALU.add)
                            I("dve", "tensor_tensor", [P + "lgs"], [P + "tt"], out=tt[:, 0:n], in0=lgs[:, 0:n],
                              in1=lgs[:, 0:n], op=ALU.mult)
                            yield
                            I("dve", "tensor_scalar", [P + "tt"], [P + "tt"], out=tt[:, 0:n], in0=tt[:, 0:n],
                              scalar1=0.044715, scalar2=1.0, op0=ALU.mult, op1=ALU.add)
                            yield
                            I("pool", "tensor_tensor", [P + "tt", P + "lgs"], [P + "tt"], out=tt[:, 0:n], in0=tt[:, 0:n],
                              in1=lgs[:, 0:n], op=ALU.mult)
                            yield
                            I("act", "activation", [P + "tt"], [P + "tt"], out=tt[:, 0:n], in_=tt[:, 0:n],
                              func=AF.Exp, scale=-1.5957691216057308)
                            yield
                            I("dve", "tensor_scalar", [P + "tt"], [P + "tt"], out=tt[:, 0:n], in0=tt[:, 0:n], scalar1=1.0,
                              scalar2=None, op0=ALU.add)
                            yield
                            I("dve", "reciprocal", [P + "tt"], [P + "tt"], out=tt[:, 0:n], in_=tt[:, 0:n])
                            yield
                            I("pool", "tensor_tensor", [P + "tt", P + "lgs"], [P + "tt"], out=tt[:, 0:n], in0=tt[:, 0:n],
                              in1=lgs[:, 0:n], op=ALU.mult)
                            yield
                            I("dve", "tensor_tensor", [P + "tt", P + "h0"], [P + "ob"], out=ob[:, 0:n], in0=tt[:, 0:n],
                              in1=h0[:, 0:n], op=ALU.mult)
                            DMA("pool", [P + "ob"], [("mixT", 6 + c, t0)], out=mixT[6 + c, :, t0:t0 + n], in_=ob[:, 0:n])
                            yield

                    def att_stream():
                        qt_r = ring(es, "qt", 2, [128, 512], BF)
                        mo_r = ring(es, "mo", 2, [128, 512], BF)
                        pe_r = ring(es, "pex", 2, [128, 2, 512], BF)
                        rec_r = ring(es, "rec", 2, [128, 512], F32)
                        ps_r = ring(es, "pss", 2, [128, 2, 512], F32, psum=True)
                        po_r = ring(es, "po", 2, [128, 512], F32, psum=True)
                        qblocks = []
                        if need_ctx:
                            qblocks.append((0, CTX, [0, 1]))
                        for i in range(16):
                            qblocks.append((CTX + 512 * i, 512, list(range(NT))))
                        for (q0, N, kts) in qblocks:
                            for j in range(4):
                                qt, qtk = qt_r.next()
                                DMA("sp", [], [qtk], out=qt[:, 0:N], in_=qT[j, :, q0:q0 + N])
                                mo, mok = mo_r.next()
                                for half in range(2):
                                    po, pok = po_r.next()
                                    npair = len(kts) // 2

                                    def emit_s(pi_):
                                        p_, pk_ = ps_r.next()
                                        for u_ in range(2):
                                            kt = kts[2 * pi_ + u_]
                                            I("pe", "matmul", [qtk, ("KTz", half)], [pk_], out=p_[:, u_, 0:N],
                                              lhsT=KTz[half][:, kt * 128:(kt + 1) * 128], rhs=qt[:, 0:N], start=True, stop=True)
                                        return p_, pk_
                                    cur = emit_s(0)
                                    for pi_ in range(npair):
                                        nxt = emit_s(pi_ + 1) if pi_ + 1 < npair else None
                                        p_, pk_ = cur
                                        ex, exk = pe_r.next()
                                        I("act", "activation", [pk_], [exk], out=ex[:, :, 0:N], in_=p_[:, :, 0:N], func=AF.Exp)
                                        for u_ in range(2):
                                            i = 2 * pi_ + u_
                                            I("pe", "matmul", [exk, "Vp"], [pok], out=po[:, 0:N], lhsT=Vp[:, kts[i], half, :],
                                              rhs=ex[:, u_, 0:N], start=(i == 0), stop=(i == len(kts) - 1))
                                        cur = nxt
                                        yield
                                    rec, reck = rec_r.next()
                                    o_sl = slice(0, 64) if half == 0 else slice(64, 128)
                                    s_sl = slice(64, 128) if half == 0 else slice(0, 64)
                                    I("dve", "reciprocal", [pok], [reck], out=rec[o_sl, 0:N], in_=po[s_sl, 0:N])
                                    I("dve", "tensor_tensor", [pok, reck], [mok], out=mo[o_sl, 0:N], in0=po[o_sl, 0:N],
                                      in1=rec[o_sl, 0:N], op=ALU.mult)
                                DMA("pool", [mok], [("mixT", j, q0)], out=mixT[j, :, q0:q0 + N], in_=mo[:, 0:N])
                                yield

                    def wconv_stream():
                        for q4 in range(4):
                            DMA("pool", [], [("woutbf", q4)], out=woutbf[q4 * 256:(q4 + 1) * 256, :],
                                in_=wout_in[l, q4 * 256:(q4 + 1) * 256, :])
                            yield
                        if l + 1 < nlayers:
                            for kc in range(8):
                                DMA("pool", [], [("winbf", kc)], out=winbf[kc * 128:(kc + 1) * 128, :],
                                    in_=win_in[l + 1, kc * 128:(kc + 1) * 128, :])
                                yield
                        if wbf is None:
                            return
                        for e in range(NE):
                            for m_, src in enumerate((wg_in, wu_in, wd_in)):
                                for q4 in range(4):
                                    for _ in range(18):
                                        yield
                                    DMA("pool", [], [("wbf", m_, e, q4)], out=wbf[m_, e, q4 * 256:(q4 + 1) * 256, :],
                                        in_=src[l, e, q4 * 256:(q4 + 1) * 256, :])

                    run_streams([att_stream(), conv_stream(0), conv_stream(1), scan_lane(0), scan_lane(1),
                                 comb_stream(0), comb_stream(1), wconv_stream()])
            S.barrier()
            if stop_after == f"p2c_{l}":
                return finish(nc, S, top)

            with contextlib.ExitStack() as es:
                wr = sb(es, "wr", [128, 8, NE], F32)
                DMA("sp", [], ["wr"], out=wr[:], in_=wr_in[l].rearrange("(k p) e -> p k e", p=128))
                nstream = 2 if need_ctx else 1
                wout_s = [sb(es, f"wout_s{s}", [128, 8, D], BF) for s in range(nstream)]
                G2_bc = [sb(es, f"G2_bc{s}", [128, D], F32) for s in range(nstream)]
                sh2_bc = [sb(es, f"sh2_bc{s}", [128, D], F32) for s in range(nstream)]
                with contextlib.ExitStack() as es2:
                    wout = sb(es2, "wout", [128, 8, D], BF)
                    for h_ in range(2):
                        DMA("sp", [], ["wout"], out=wout[:, 4 * h_:4 * h_ + 4, :],
                            in_=woutbf[512 * h_:512 * (h_ + 1), :].rearrange("(k p) n -> p k n", p=128))
                    gate_bc = [sb(es2, f"gate_bc{s}", [128, D], F32) for s in range(nstream)]
                    tmpD = sb(es2, "tmpD", [128, 128], F32)
                    pbc = ps(es2, "pbc", [128, 512], F32)
                    for s in range(nstream):
                        make_bc(es, gate_bc[s], f"gate_bc{s}", lambda kc, s=s: modsl(2)[:, kc, s:s + 1], ("modT", l), tmpD,
                                "tmpD", pbc, "pbc")
                        make_bc(es, G2_bc[s], f"G2_bc{s}", lambda kc, s=s: G2T[:, kc, s:s + 1], "G2T", tmpD, "tmpD", pbc,
                                "pbc")
                        make_bc(es, sh2_bc[s], f"sh2_bc{s}", lambda kc, s=s: sh2T[:, kc, s:s + 1], ("modT", l), tmpD,
                                "tmpD", pbc, "pbc")
                        for kc in range(8):
                            I("dve" if kc % 2 == 0 else "pool", "tensor_tensor", ["wout", f"gate_bc{s}"], [f"wout_s{s}"],
                              out=wout_s[s][:, kc, :], in0=wout[:, kc, :], in1=gate_bc[s][:], op=ALU.mult)
                    S.barrier()
                junk3 = sb(es, "junk3", [128, D], BF)

                def p3_stream(k, nk):
                    P = f"p3{k}_"
                    mx = sb(es, P + "mx", [128, 8, 512], BF)
                    xt = sb(es, P + "xt", [128, D], F32)
                    x1 = sb(es, P + "x1", [128, D], F32)
                    h2 = sb(es, P + "h2", [128, D], F32)
                    row = sb(es, P + "row", [128, ROWW], BF)
                    h2T = sb(es, P + "h2T", [128, 8, 128], F32)
                    sm = sb(es, P + "sm", [128, 8], F32)
                    ex = sb(es, P + "ex", [128, NE], F32)
                    py = ps(es, P + "py", [128, 512], F32)
                    pta = ps(es, P + "pta", [128, 4, 128], F32)
                    ptb = ps(es, P + "ptb", [128, 4, 128], F32)
                    plog_t = ps(es, P + "plog", [128, 512], F32)
                    plog = plog_t[:, 0:NE]
                    mxk, xtk, x1k, h2k, rowk, h2Tk, smk, exk = (P + n_ for n_ in ("mx", "xt", "x1", "h2", "row", "h2T", "sm", "ex"))
                    pyk, ptak, ptbk, plogk = P + "py", P + "pta", P + "ptb", P + "plog"
                    ulist = [u_ for u_ in units if not (u_[2] and not need_ctx)]
                    for ui, (tok0, ntl, is_ctx) in enumerate(ulist):
                        if ui % nk != k:
                            continue
                        s = 1 if is_ctx else 0
                        NTu = ntl * 128
                        DMA("sp", [], [mxk], out=mx[:, :, 0:NTu], in_=mixT[:, :, tok0:tok0 + NTu].rearrange("c p n -> p c n"))
                        for ti in range(ntl):
                            t = tok0 // 128 + ti
                            DMA("sp", [], [xtk], out=xt[:], in_=src_rows(l, tok0 + ti * 128, 128))
                            yield
                            for nh in range(2):
                                cs = slice(nh * 512, (nh + 1) * 512)
                                for kc in range(8):
                                    I("pe", "matmul", [mxk, f"wout_s{s}"], [pyk], out=py[:],
                                      lhsT=mx[:, kc, ti * 128:(ti + 1) * 128], rhs=wout_s[s][:, kc, cs],
                                      start=(kc == 0), stop=(kc == 7))
                                yield
                                I("dve", "tensor_tensor", [pyk, xtk], [x1k], out=x1[:, cs], in0=py[:], in1=xt[:, cs],
                                  op=ALU.add)
                                yield
                            DMA("pool", [x1k], [("xa", t)], out=xa[tok0 + ti * 128:tok0 + (ti + 1) * 128, :], in_=x1[:])
                            I("pool", "memset", [], [smk], ap=sm[:], constant=0.0)
                            yield
                            I("act", "activation", [x1k, smk], ["junk3", smk], out=junk3[:], in_=x1[:], func=AF.Square,
                              accum_out=sm[:, 0:1])
                            yield
                            I("dve", "tensor_scalar", [smk], [smk], out=sm[:, 1:2], in0=sm[:, 0:1], scalar1=1.0 / D,
                              scalar2=EPS, op0=ALU.mult, op1=ALU.add)
                            yield
                            I("act", "activation", [smk], [smk], out=sm[:, 1:2], in_=sm[:, 1:2], func=AF.Sqrt)
                            yield
                            I("dve", "reciprocal", [smk], [smk], out=sm[:, 2:3], in_=sm[:, 1:2])
                            yield
                            I("dve", "scalar_tensor_tensor", [x1k, smk, f"G2_bc{s}"], [h2k], out=h2[:], in0=x1[:],
                              scalar=sm[:, 2:3], in1=G2_bc[s][:], op0=ALU.mult, op1=ALU.mult)
                            yield
                            I("pool", "tensor_tensor", [h2k, f"sh2_bc{s}"], [h2k], out=h2[:], in0=h2[:], in1=sh2_bc[s][:],
                              op=ALU.add)
                            yield
                            I("act", "copy", [h2k], [rowk], out=row[:, 0:D], in_=h2[:])
                            for kc in range(8):
                                pt_ = pta if kc < 4 else ptb
                                I("pe", "transpose", [h2k, "cst_f"], [ptak if kc < 4 else ptbk], out=pt_[:, kc % 4, :],
                                  in_=h2[:, kc * 128:(kc + 1) * 128], identity=ident_f)
                            yield
                            I("act", "copy", [ptak], [h2Tk], out=h2T[:, 0:4, :], in_=pta[:])
                            I("dve", "tensor_copy", [ptbk, h2Tk], [h2Tk], out=h2T[:, 4:8, :], in_=ptb[:])
                            yield
                            for kc in range(8):
                                I("pe", "matmul", [h2Tk, "wr"], [plogk], out=plog, lhsT=h2T[:, kc, :], rhs=wr[:, kc, :],
                                  start=(kc == 0), stop=(kc == 7))
                            yield
                            I("dve", "reduce_max", [plogk, smk], [smk], out=sm[:, 3:4], in_=plog, axis=AX.X)
                            yield
                            I("dve", "tensor_scalar", [smk], [smk], out=sm[:, 3:4], in0=sm[:, 3:4], scalar1=-1.0, scalar2=None,
                              op0=ALU.mult)
                            yield
                            I("act", "activation", [plogk, smk], [exk, smk], out=ex[:], in_=plog, func=AF.Exp,
                              bias=sm[:, 3:4], accum_out=sm[:, 4:5])
                            yield
                            I("dve", "reciprocal", [smk], [smk], out=sm[:, 5:6], in_=sm[:, 4:5])
                            yield
                            I("dve", "tensor_scalar", [exk, smk], [("aff", t)], out=aff_all[:, t, :], in0=ex[:],
                              scalar1=sm[:, 5:6], scalar2=None, op0=ALU.mult)
                            yield
                            I("dve", "tensor_copy", [("aff", t), rowk], [rowk], out=row[:, D:D + 32].bitcast(F32),
                              in_=aff_all[:, t, :])
                            I("pool", "tensor_copy", ["tokid", rowk], [rowk], out=row[:, D + 32:D + 34].bitcast(I32),
                              in_=tokid[:, t:t + 1])
                            DMA("pool", [rowk], [("h2D", t)], out=h2D[t * 128:(t + 1) * 128, :], in_=row[:])
                            yield
                run_streams([p3_stream(k, 2) for k in range(2)])
            S.barrier()
            if stop_after == f"p3_{l}":
                if debug:
                    DMA("sp", [], ["dbg"], out=dbg[:, 0:NT * NE], in_=aff_all[:].rearrange("p t e -> p (t e)"))
                return finish(nc, S, top)

            groups = [(2, NT, CAP, 0)]
            if need_ctx:
                groups.append((0, 2, CAPC, NE * CAP))
            ng = len(groups)
            idxi = sb(LS, "idxi", [128, NE, NT], I32)
            with contextlib.ExitStack() as es:
                lo_t = sb(es, "lo_t", [128, 2, NE], F32)
                hi_t = sb(es, "hi_t", [128, 2, NE], F32)
                mid = sb(es, "mid", [128, 2, NE], F32)
                kk = sb(es, "kk", [128, 2, NE], F32)
                cmp = sb(es, "cmp", [128, NT, NE], F32)
                cntb = sb(es, "cntb", [128, 2, NE], BF)
                cntf = sb(es, "cntf", [128, 2, NE], F32)
                geu = sb(es, "geu", [128, 2, NE], U32)
                ltu = sb(es, "ltu", [128, 2, NE], U32)
                ptot_t = ps(es, "ptot", [128, 512], F32)
                ptot = ptot_t[:, 0:2 * NE]
                I("dve", "memset", [], ["lo_t"], ap=lo_t[:], constant=0.0)
                I("dve", "memset", [], ["hi_t"], ap=hi_t[:], constant=2.0)
                I("dve", "memset", [], ["kk"], ap=kk[:, 0, :], constant=float(CAP))
                I("dve", "memset", ["kk"], ["kk"], ap=kk[:, 1, :], constant=float(CAPC))
                I("dve", "memset", [], ["cntf"], ap=cntf[:], constant=0.0)

                def count_ge(thr, thrk):
                    for g, (tl, th, cap, rb) in enumerate(groups):
                        I("dve", "tensor_tensor", ["aff", thrk], ["cmp"], out=cmp[:, tl:th, :], in0=aff_all[:, tl:th, :],
                          in1=thr[:, g, :].unsqueeze(1).to_broadcast([128, th - tl, NE]), op=ALU.is_ge)
                        I("dve", "reduce_sum", ["cmp"], ["cntf"], out=cntf[:, g, :],
                          in_=cmp[:, tl:th, :].rearrange("p t e -> p e t"), axis=AX.X)
                    I("dve", "tensor_copy", ["cntf"], ["cntb"], out=cntb[:], in_=cntf[:])
                    I("pe", "matmul", ["cntb", "ones_b"], ["ptot"], out=ptot,
                      lhsT=ones_b[:], rhs=cntb[:].rearrange("p g e -> p (g e)"), start=True, stop=True)

                NIT = 34
                for it in range(NIT):
                    I("dve", "tensor_tensor", ["lo_t", "hi_t"], ["mid"], out=mid[:], in0=lo_t[:], in1=hi_t[:], op=ALU.add)
                    I("dve", "tensor_scalar", ["mid"], ["mid"], out=mid[:], in0=mid[:], scalar1=0.5, scalar2=None,
                      op0=ALU.mult)
                    count_ge(mid, "mid")
                    ptv = ptot.rearrange("p (g e) -> p g e", g=2)
                    I("dve", "tensor_tensor", ["ptot", "kk"], ["geu"], out=geu[:], in0=ptv, in1=kk[:], op=ALU.is_ge)
                    I("dve", "tensor_tensor", ["ptot", "kk"], ["ltu"], out=ltu[:], in0=ptv, in1=kk[:], op=ALU.is_lt)
                    I("dve", "copy_predicated", ["geu", "mid", "lo_t"], ["lo_t"], out=lo_t[:], mask=geu[:], data=mid[:])
                    I("dve", "copy_predicated", ["ltu", "mid", "hi_t"], ["hi_t"], out=hi_t[:], mask=ltu[:], data=mid[:])
                count_ge(lo_t, "lo_t")
                offp = sb(es, "offp", [128, 2, NE], F32)
                poff_t = ps(es, "poff", [128, 512], F32)
                poff = poff_t[:, 0:2 * NE]
                I("pe", "matmul", ["cntb", "ltri_b"], ["poff"], out=poff, lhsT=ltri_b[:],
                  rhs=cntb[:].rearrange("p g e -> p (g e)"), start=True, stop=True)
                I("act", "copy", ["poff"], ["offp"], out=offp[:].rearrange("p g e -> p (g e)"), in_=poff)
                Mt = sb(es, "Mt", [128, NE, NT], F32)
                cs_ = sb(es, "cs_", [128, NE, NT], F32)
                onesr = sb(es, "onesr", [128, NE * NT], F32)
                idxf = sb(es, "idxf", [128, NE, NT], F32)
                ebase = sb(es, "ebase", [128, 2, NE], F32)
                I("pool", "memset", [], ["onesr"], ap=onesr[:], constant=1.0)
                I("pool", "iota", [], ["ebase0"], out=idxi[:, :, 0], pattern=[[1, NE]], base=0, channel_multiplier=0)
                I("dve", "tensor_copy", ["ebase0"], ["ebase"], out=ebase[:, 0, :], in_=idxi[:, :, 0])
                I("dve", "tensor_scalar", ["ebase"], ["ebase"], out=ebase[:, 1, :], in0=ebase[:, 0, :], scalar1=float(CAPC),
                  scalar2=float(NE * CAP), op0=ALU.mult, op1=ALU.add)
                I("dve", "tensor_scalar", ["ebase"], ["ebase"], out=ebase[:, 0, :], in0=ebase[:, 0, :], scalar1=float(CAP),
                  scalar2=None, op0=ALU.mult)
                I("dve", "tensor_copy", ["cmp"], ["Mt"], out=Mt[:], in_=cmp[:].rearrange("p t e -> p e t"))
                for g, (tl, th, cap, rb) in enumerate(groups):
                    ntl_ = th - tl
                    for e in range(NE):
                        I("dve", "tensor_tensor_scan", ["Mt", "onesr", "zcol"], ["cs_"], out=cs_[:, e, tl:th],
                          data0=onesr[:, 0:ntl_], data1=Mt[:, e, tl:th], initial=zcol[:, 0:1], op0=ALU.mult, op1=ALU.add)
                    I("dve", "tensor_tensor", ["cs_", "Mt"], ["cs_"], out=cs_[:, :, tl:th], in0=cs_[:, :, tl:th],
                      in1=Mt[:, :, tl:th], op=ALU.subtract)
                    I("dve", "tensor_tensor", ["cs_", "offp"], ["cs_"], out=cs_[:, :, tl:th], in0=cs_[:, :, tl:th],
                      in1=offp[:, g, :].unsqueeze(2).to_broadcast([128, NE, ntl_]), op=ALU.add)
                    I("dve", "tensor_scalar", ["cs_"], ["idxf"], out=idxf[:, :, tl:th], in0=cs_[:, :, tl:th],
                      scalar1=float(cap), scalar2=None, op0=ALU.is_lt)
                    I("dve", "tensor_tensor", ["idxf", "Mt"], ["idxf"], out=idxf[:, :, tl:th], in0=idxf[:, :, tl:th],
                      in1=Mt[:, :, tl:th], op=ALU.mult)
                    I("dve", "tensor_tensor", ["cs_", "ebase"], ["cs_"], out=cs_[:, :, tl:th], in0=cs_[:, :, tl:th],
                      in1=ebase[:, g, :].unsqueeze(2).to_broadcast([128, NE, ntl_]), op=ALU.add)
                    I("dve", "tensor_scalar", ["cs_"], ["cs_"], out=cs_[:, :, tl:th], in0=cs_[:, :, tl:th],
                      scalar1=-BIGIDX, scalar2=None, op0=ALU.add)
                    I("dve", "tensor_tensor", ["idxf", "cs_"], ["idxf"], out=idxf[:, :, tl:th], in0=idxf[:, :, tl:th],
                      in1=cs_[:, :, tl:th], op=ALU.mult)
                    I("dve", "tensor_scalar", ["idxf"], ["idxf"], out=idxf[:, :, tl:th], in0=idxf[:, :, tl:th],
                      scalar1=BIGIDX, scalar2=None, op0=ALU.add)
                I("dve", "tensor_copy", ["idxf", "ebase0"], ["idxi"], out=idxi[:], in_=idxf[:])
                if debug:
                    DMA("sp", ["lo_t"], ["dbg"], out=dbg[:, 0:2 * NE], in_=lo_t[:].rearrange("p g e -> p (g e)"))
                    DMA("sp", ["idxf"], ["dbg"], out=dbg[:, 64:64 + NE * NT], in_=idxf[:].rearrange("p e t -> p (e t)"))
            S.barrier()
            if stop_after == f"p4c_{l}":
                return finish(nc, S, top)

            with contextlib.ExitStack() as es:
                mod5_bc = [sb(es, f"mod5_bc{s}", [128, D], F32) for s in range(ng)]
                with contextlib.ExitStack() as es2:
                    tmpD = sb(es2, "tmpD4", [128, 128], F32)
                    pbc = ps(es2, "pbc4", [128, 512], F32)
                    for s in range(ng):
                        make_bc(es, mod5_bc[s], f"mod5_bc{s}", lambda kc, s=s: modsl(5)[:, kc, s:s + 1], ("modT", l), tmpD,
                                "tmpD4", pbc, "pbc4")
                    S.barrier()
                wg_t = [sb(es, f"wg{i}", [128, 8, D], BF) for i in range(2)]
                wu_t = [sb(es, f"wu{i}", [128, 8, D], BF) for i in range(2)]
                wd_t = [sb(es, "wd0", [128, 8, D], BF)]
                xsb = sb(es, "xsb", [128, 8, ROWW], BF)
                xsc = sb(es, "xsc", [CAPC, ROWW], BF)
                meta_t = [sb(es, f"meta{i}", [128, 9, 40], BF) for i in range(2)]
                xsT_t = [sb(es, f"xsT{i}", [128, 8, CAP + CAPC], BF) for i in range(2)]
                actT = sb(es, "actT", [128, 8, CAP + CAPC], BF)
                sg_r = ring(es, "sg", 2, [128, 512], F32)
                yt_r = ring(es, "yt", 4, [128, D], F32)
                pxt_r = ring(es, "pxt", 2, [128, 8, 128], BF, psum=True)
                pg_r = ring(es, "pg", 2, [128, 512], F32, psum=True)
                pu_r = ring(es, "pu", 2, [128, 512], F32, psum=True)
                pyy_r = ring(es, "pyy", 2, [128, 512], F32, psum=True)
                rw_r = ring(es, "rw", 4, [128, ROWW], BF)
                prog = {"w_g": 0, "w_d": 0, "c_gu": 0, "c_dn": 0, "s_done": 0, "c_start": 0}
                NPASS = 8
                EPP = NE // NPASS

                def s_stream():
                    for g_ in range(NPASS):
                        while prog["c_start"] < EPP * (g_ - 1):
                            yield
                        for (tl, th, cap, rb) in groups:
                            for t in range(tl, th):
                                rw, rwk = rw_r.next()
                                DMA("sp", [], [rwk], out=rw[:], in_=h2D[t * 128:(t + 1) * 128, :])
                                for e in range(EPP * g_, EPP * (g_ + 1)):
                                    S.dma("pool", lambda en, rw=rw, e=e, t=t: en.indirect_dma_start(
                                        out=xs, out_offset=bass.IndirectOffsetOnAxis(ap=idxi[:, e, t:t + 1], axis=0),
                                        in_=rw[:], in_offset=None, bounds_check=preg(en, XS_ROWS - 1), oob_is_err=False),
                                        [rwk, "idxi"], [("xs", e, t)])
                                    yield
                        prog["s_done"] = EPP * (g_ + 1)


                def load_mat(m_, e, w, wk):
                    for h_ in range(2):
                        DMA("sp", [], [wk], out=w[:, 4 * h_:4 * h_ + 4, :],
                            in_=wbf[m_, e, 512 * h_:512 * (h_ + 1), :].rearrange("(k p) n -> p k n", p=128))
                        yield

                def w_stream():
                    for e in range(NE):
                        while prog["c_gu"] < e - 1:
                            yield
                        yield from load_mat(0, e, wg_t[e % 2], ("wg", e % 2))
                        yield from load_mat(1, e, wu_t[e % 2], ("wu", e % 2))
                        prog["w_g"] = e + 1
                        while prog["c_dn"] < e:
                            yield
                        yield from load_mat(2, e, wd_t[0], ("wd", 0))
                        prog["w_d"] = e + 1

                def load_x(e):
                    DMA("sp", [("xs", e, t) for t in range(2, NT)], ["xsb"], out=xsb[:],
                        in_=xs[e * CAP:(e + 1) * CAP, :].rearrange("(t p) w -> p t w", p=128))
                    if need_ctx:
                        DMA("sp", [("xs", e, t) for t in range(0, 2)], ["xsc"], out=xsc[:],
                            in_=xs[NE * CAP + e * CAPC:NE * CAP + (e + 1) * CAPC, :])

                def transposes(e):
                    xsT = xsT_t[e % 2]
                    xk = ("xsT", e % 2)
                    meta = meta_t[e % 2]
                    mk_ = ("meta", e % 2)
                    I("dve", "tensor_copy", ["xsb"], [mk_], out=meta[:, 0:8, :], in_=xsb[:, :, D:D + 40])
                    if need_ctx:
                        I("dve", "tensor_copy", ["xsc", mk_], [mk_], out=meta[0:CAPC, 8, :], in_=xsc[:, D:D + 40])
                    for st in range(8):
                        pxt, pxtk = pxt_r.next()
                        for kc in range(8):
                            I("pe", "transpose", ["xsb", "ident_b"], [pxtk], out=pxt[:, kc, :],
                              in_=xsb[:, st, kc * 128:(kc + 1) * 128], identity=ident_b[:])
                        if st % 2 == 0:
                            I("act", "copy", [pxtk], [xk], out=xsT[:, :, st * 128:(st + 1) * 128], in_=pxt[:])
                        else:
                            I("dve", "tensor_copy", [pxtk], [xk], out=xsT[:, :, st * 128:(st + 1) * 128], in_=pxt[:])
                        yield
                    if need_ctx:
                        pxt, pxtk = pxt_r.next()
                        for kc in range(8):
                            I("pe", "transpose", ["xsc", "ident_b"], [pxtk], out=pxt[:, kc, 0:CAPC],
                              in_=xsc[:, kc * 128:(kc + 1) * 128], identity=ident_b[0:CAPC, 0:CAPC])
                        I("act", "copy", [pxtk], [xk], out=xsT[:, :, CAP:CAP + CAPC], in_=pxt[:, :, 0:CAPC])
                        yield

                def c_stream():
                    while prog["s_done"] <= 0:
                        yield
                    load_x(0)
                    yield from transposes(0)
                    slabs = [(0, 512), (512, 512)] + ([(CAP, CAPC)] if need_ctx else [])
                    for e in range(NE):
                        xsT = xsT_t[e % 2]
                        xk = ("xsT", e % 2)
                        meta = meta_t[e % 2]
                        mk_ = ("meta", e % 2)
                        prog["c_start"] = e + 1
                        if e + 1 < NE:
                            while prog["s_done"] <= e + 1:
                                yield
                            load_x(e + 1)
                        while prog["w_g"] <= e:
                            yield
                        wg, wgk = wg_t[e % 2], ("wg", e % 2)
                        wu, wuk = wu_t[e % 2], ("wu", e % 2)
                        wd, wdk = wd_t[0], ("wd", 0)
                        for fc in range(8):
                            for (c0, w_) in slabs:
                                pg, pgk = pg_r.next()
                                pu, puk = pu_r.next()
                                for kc in range(8):
                                    I("pe", "matmul", [xk, wgk], [pgk], out=pg[:, 0:w_], lhsT=wg[:, kc, fc * 128:(fc + 1) * 128],
                                      rhs=xsT[:, kc, c0:c0 + w_], start=(kc == 0), stop=(kc == 7))
                                for kc in range(8):
                                    I("pe", "matmul", [xk, wuk], [puk], out=pu[:, 0:w_], lhsT=wu[:, kc, fc * 128:(fc + 1) * 128],
                                      rhs=xsT[:, kc, c0:c0 + w_], start=(kc == 0), stop=(kc == 7))
                                sg, sgk = sg_r.next()
                                I("act", "activation", [pgk], [sgk], out=sg[:, 0:w_], in_=pg[:, 0:w_], func=AF.Silu)
                                I("dve", "tensor_tensor", [sgk, puk], [("actT", fc)], out=actT[:, fc, c0:c0 + w_],
                                  in0=sg[:, 0:w_], in1=pu[:, 0:w_], op=ALU.mult)
                                yield
                        prog["c_gu"] = e + 1
                        if e + 1 < NE:
                            yield from transposes(e + 1)
                        while prog["w_d"] <= e:
                            yield
                        tiles = [(st * 128, 128, st, 0) for st in range(8)]
                        if need_ctx:
                            tiles.append((CAP, CAPC, 8, 1))
                        for (c0, m_, mi, g) in tiles:
                            yt, ytk = yt_r.next()
                            gate_ap = meta[0:m_, mi, 2 * e:2 * e + 2].bitcast(F32)
                            for nh in range(2):
                                cs = slice(nh * 512, (nh + 1) * 512)
                                pyy, pyk = pyy_r.next()
                                for fc in range(8):
                                    I("pe", "matmul", [("actT", fc), wdk], [pyk], out=pyy[0:m_, :],
                                      lhsT=actT[:, fc, c0:c0 + m_], rhs=wd[:, fc, cs], start=(fc == 0), stop=(fc == 7))
                                I("dve", "scalar_tensor_tensor", [pyk, mk_, f"mod5_bc{g}"], [ytk], out=yt[0:m_, cs],
                                  in0=pyy[0:m_, :], scalar=gate_ap, in1=mod5_bc[g][0:m_, cs], op0=ALU.mult, op1=ALU.mult)
                                yield
                            idx_ap = meta[0:m_, mi, 32:34].bitcast(I32)
                            S.dma("pool", lambda en, yt=yt, m_=m_, idx_ap=idx_ap: en.indirect_dma_start(
                                out=xa, out_offset=bass.IndirectOffsetOnAxis(ap=idx_ap, axis=0), in_=yt[0:m_, :],
                                in_offset=None, bounds_check=preg(en, NTOK - 1), oob_is_err=True, compute_op=ALU.add),
                                [ytk, mk_] + [("xa_p", (e - 1) % 2, i_) for i_ in range(9)], [("xa_p", e % 2, mi)])
                            yield
                        prog["c_dn"] = e + 1
                run_streams([s_stream(), w_stream(), c_stream()])
            S.barrier()
            if stop_after == f"p4_{l}":
                return finish(nc, S, top)

    with contextlib.ExitStack() as es:
        fg_bc = sb(es, "fg_bc", [128, D], F32)
        DMA("sp", [], ["fg_bc"], out=fg_bc[:], in_=fg_in.to_broadcast([128, D]))
        xt_r = ring(es, "xt5", 3, [128, D], F32)
        ot_r = ring(es, "ot5", 3, [128, D], F32)
        junk = sb(es, "junk5", [128, D], BF)
        sm_r = ring(es, "sm5", 3, [128, 4], F32)
        for t in range(2, NT):
            xt, xtk = xt_r.next()
            DMA("sp", [], [xtk], out=xt[:], in_=xa[t * 128:(t + 1) * 128, :])
            sm, smk = sm_r.next()
            I("pool", "memset", [], [smk], ap=sm[:, 0:1], constant=0.0)
            I("act", "activation", [xtk, smk], ["junk5", smk], out=junk[:], in_=xt[:], func=AF.Square, accum_out=sm[:, 0:1])
            rstd_chain(sm[:, 0:1], smk, sm[:, 1:2], smk, sm[:, 2:3], smk, 1, 1.0 / D)
            ot, otk = ot_r.next()
            I("dve", "scalar_tensor_tensor", [xtk, smk, "fg_bc"], [otk], out=ot[:], in0=xt[:], scalar=sm[:, 2:3],
              in1=fg_bc[:], op0=ALU.mult, op1=ALU.mult)
            DMA("pool", [otk], [("out", t)], out=out[(t - 2) * 128:(t - 1) * 128, :], in_=ot[:])
    return finish(nc, S, top, True)


def finish(nc, S, top, close_top=False):
    S.barrier()
    S.emit()
    S.close()
    if close_top:
        top.close()
    return nc


Q_PERM = np.concatenate([np.arange(64) + 64 * (j + 4 * half) for j in range(4) for half in range(2)])


def rope_table():
    rows = SEQ // 64
    row = np.repeat(np.arange(rows, dtype=np.float32), 64)
    col = np.tile(np.arange(64, dtype=np.float32), rows)
    n_freq = 16
    inv = (np.float32(10000.0) ** (-np.arange(n_freq, dtype=np.float32) / np.float32(n_freq))).astype(np.float32)
    ang = np.concatenate([row[:, None] * inv, col[:, None] * inv], axis=-1).astype(np.float32)
    return np.concatenate([np.cos(ang), np.sin(ang)], axis=-1).astype(np.float32)


def fm(v):
    v = np.asarray(v)
    return np.ascontiguousarray(np.swapaxes(v.reshape(v.shape[:-1] + (v.shape[-1] // 128, 128)), -1, -2))


def prep_inputs(inp):
    f = lambda a: np.ascontiguousarray(np.asarray(a, dtype=np.float32))
    shared = {}
    shared["w_mod"] = f(inp["w_mod"])
    shared["b_modT"] = f(fm(inp["b_mod"]))
    shared["g1T"] = f(fm(inp["norm1_g"]))
    shared["g2T"] = f(fm(inp["norm2_g"]))
    w_in = np.asarray(inp["w_in"], dtype=np.float32).copy()
    w_in[:, :, 0:512] = w_in[:, :, Q_PERM]
    shared["w_in"] = f(w_in)
    shared["qkg"] = f(np.stack([inp["q_norm_g"], inp["k_norm_g"]], axis=1))
    cw = np.asarray(inp["conv_w"], dtype=np.float32)
    shared["convw"] = f(cw.reshape(2, 3, 2, 128).transpose(0, 3, 2, 1))
    lw = np.asarray(inp["lru_conv_w"], dtype=np.float32)
    lb = np.asarray(inp["lru_conv_b"], dtype=np.float32)
    lcw = np.concatenate([lw, lb[:, None, :]], axis=1)
    shared["lcw"] = f(lcw.reshape(2, 5, 2, 128).transpose(0, 3, 2, 1))
    lv = np.stack([inp["lru_ba"], inp["lru_bi"], inp["lru_lam"]], axis=-1)
    shared["lvec"] = f(np.asarray(lv, dtype=np.float32).reshape(2, 2, 2, 128, 3).transpose(0, 3, 1, 2, 4))
    shared["lwa"] = f(inp["lru_wa"])
    shared["lwi"] = f(inp["lru_wi"])
    w_out = np.asarray(inp["w_out"], dtype=np.float32).copy()
    w_out[:, 0:512, :] = w_out[:, Q_PERM, :]
    shared["w_out"] = f(w_out)
    shared["w_r"] = f(inp["w_router"])
    shared["w_gate"] = f(inp["w_gate"])
    shared["w_up"] = f(inp["w_up"])
    shared["w_down"] = f(inp["w_down"])
    shared["fg"] = f(np.asarray(inp["final_g"]).reshape(1, D))
    shared["rope"] = rope_table()
    cst = np.zeros((128, 3, 128), np.float32)
    cst[:, 0, :] = np.eye(128, dtype=np.float32)
    cst[:, 1, :] = np.triu(np.ones((128, 128), np.float32), 1)
    cst[:, 2, :] = 1.0
    shared["cst"] = cst
    x = np.asarray(inp["x"], dtype=np.float32)
    ctx = np.asarray(inp["ctx"], dtype=np.float32)
    c = np.asarray(inp["c"], dtype=np.float32)
    cc = fm(np.asarray(inp["c_ctx"], dtype=np.float32))
    maps = []
    for b in range(x.shape[0]):
        m = dict(shared)
        m["x"] = np.ascontiguousarray(x[b])
        m["ctx"] = np.ascontiguousarray(ctx[b])
        m["cT"] = f(np.stack([fm(c[b]), cc], axis=-1))
        maps.append(m)
    return maps


_NC_CACHE = {}
DBG = {"units": None, "skip": set()}


def kernel(**inputs):
    maps = prep_inputs(inputs)
    if "nc" not in _NC_CACHE:
        _NC_CACHE["nc"] = build()
    nc = _NC_CACHE["nc"]
    res = run_bass_kernel_spmd(nc, maps, core_ids=list(range(8)))
    return np.stack([np.asarray(r["out"]) for r in res.results], axis=0).astype(np.float32)
```

```python
import contextlib
import numpy as np
import concourse.bass as bass
import concourse.mybir as mybir
from concourse.bass_utils import run_bass_kernel_spmd

F32 = mybir.dt.float32
BF = mybir.dt.bfloat16
I32 = mybir.dt.int32
U32 = mybir.dt.uint32
ALU = mybir.AluOpType
AF = mybir.ActivationFunctionType
AX = mybir.AxisListType

D = 1024
SEQ = 8192
CTX = 256
NTOK = SEQ + CTX
NT = NTOK // 128
NE = 16
CAP = 1024
CAPC = 32
EPS = 1e-6
ROWW = 1064
XS_ROWS = NE * CAP + NE * CAPC
BIGIDX = 1.0e6
ENGS = ("pe", "act", "dve", "pool", "sp")


class Sched:
    def __init__(self, nc, nd=None):
        self.nc = nc
        self.q = {e: [] for e in ENGS}
        self.nd = nd or {"sp": 16, "pool": 16, "act": 4}
        self._stack = []
        self.esem = {}
        self.cnt = {}
        self.dsem = {}
        self.dcnt = {}
        self.dnext = {}
        self.waited = {e: {} for e in ENGS}
        self.last_w = {}
        self.readers = {}
        self.nsem = 0
        self.n_instr = 0
        self.excl = set()

    def _new_sem(self, name):
        cm = self.nc.semaphore(name)
        s = cm.__enter__()
        self._stack.append(cm)
        self.nsem += 1
        return s

    def start(self):
        for e in ENGS:
            self.esem[e] = (self._new_sem(f"es_{e}_0"), 0)
            self.cnt[e] = 0
        for e, n in self.nd.items():
            self.dsem[e] = [self._new_sem(f"ds_{e}_{i}") for i in range(n)]
            self.dcnt[e] = [0] * n
            self.dnext[e] = 0

    def close(self):
        for cm in reversed(self._stack):
            cm.__exit__(None, None, None)
        self._stack = []

    def _need(self, eng, tok):
        kind = tok[0]
        if kind == "e":
            a, gen, n, sem = tok[1], tok[2], tok[3], tok[4]
            if a == eng and eng == "pe":
                return
            key = ("e", a, gen)
        else:
            _, a, i, n = tok
            key = ("d", a, i)
            sem = self.dsem[a][i]
        if self.waited[eng].get(key, 0) >= n:
            return
        self.waited[eng][key] = n
        self.q[eng].append(("wait", sem, n))

    def _deps(self, eng, reads, writes):
        for r in reads:
            t = self.last_w.get(r)
            if t is not None:
                self._need(eng, t)
        for w in writes:
            t = self.last_w.get(w)
            if t is not None:
                self._need(eng, t)
            for t in self.readers.get(w, ()):
                self._need(eng, t)

    def _record(self, tok, reads, writes):
        for r in reads:
            self.readers.setdefault(r, []).append(tok)
        for w in writes:
            self.last_w[w] = tok
            self.readers[w] = []

    def op(self, eng, fn, reads=(), writes=()):
        if self.excl:
            ex = [r for r in reads if r in self.excl]
            if ex:
                reads = [r for r in reads if r not in self.excl]
                writes = list(writes) + ex
        self._deps(eng, reads, writes)
        self.cnt[eng] += 1
        sem, gen = self.esem[eng]
        self.q[eng].append(("op", fn, sem))
        tok = ("e", eng, gen, self.cnt[eng], sem)
        self._record(tok, reads, writes)
        self.n_instr += 1
        return tok

    def dma(self, eng, fn, reads=(), writes=()):
        idx = self.dnext[eng]
        self.dnext[eng] = (idx + 1) % self.nd[eng]
        if self.dcnt[eng][idx] > 0:
            self._need(eng, ("d", eng, idx, self.dcnt[eng][idx] * 16))
        self._deps(eng, reads, writes)
        self.dcnt[eng][idx] += 1
        tok = ("d", eng, idx, self.dcnt[eng][idx] * 16)
        self.q[eng].append(("dma", fn, self.dsem[eng][idx]))
        self._record(tok, reads, writes)
        self.n_instr += 1
        return tok

    def barrier(self):
        for e in ENGS:
            for e2 in ENGS:
                if e2 != e and self.cnt[e2] > 0:
                    sem, gen = self.esem[e2]
                    self._need(e, ("e", e2, gen, self.cnt[e2], sem))
            for e2 in self.nd:
                for i in range(self.nd[e2]):
                    if self.dcnt[e2][i] > 0:
                        self._need(e, ("d", e2, i, self.dcnt[e2][i] * 16))
        self.last_w = {}
        self.readers = {}
        for e in ENGS:
            if self.cnt[e] > 20000:
                gen = self.esem[e][1] + 1
                self.esem[e] = (self._new_sem(f"es_{e}_{gen}"), gen)
                self.cnt[e] = 0

    def emit(self):
        with self.nc.Block() as block:
            def play(engname):
                def body(engine):
                    for item in self.q[engname]:
                        if item[0] == "wait":
                            engine.wait_ge(item[1], item[2])
                        elif item[0] == "op":
                            item[1](engine).then_inc(item[2], 1)
                        else:
                            item[1](engine).then_inc(item[2], 16)
                return body
            block.tensor(play("pe"))
            block.scalar(play("act"))
            block.vector(play("dve"))
            block.gpsimd(play("pool"))
            block.sync(play("sp"))


def run_streams(gens):
    gens = list(gens)
    while gens:
        for g in list(gens):
            try:
                next(g)
            except StopIteration:
                gens.remove(g)


class Ring:
    def __init__(self, tiles, name):
        self.tiles = tiles
        self.name = name
        self.i = -1

    def next(self):
        self.i = (self.i + 1) % len(self.tiles)
        return self.tiles[self.i], (self.name, self.i)


def build(stop_after=None, debug=False, nlayers=2):
    nc = bass.Bass("TRN2", target_bir_lowering=False)
    S = Sched(nc)

    def din(name, shape, dt=F32):
        return nc.dram_tensor(name, list(shape), dt, kind="ExternalInput").ap()

    def dscratch(name, shape, dt):
        kind = "ExternalOutput" if debug else "Internal"
        return nc.dram_tensor(name, list(shape), dt, kind=kind).ap()

    x_in = din("x", [SEQ, D])
    ctx_in = din("ctx", [CTX, D])
    cT_in = din("cT", [128, 8, 2])
    wmod_in = din("w_mod", [2, D, 6 * D])
    bmodT_in = din("b_modT", [2, 128, 48])
    g1T_in = din("g1T", [2, 128, 8])
    g2T_in = din("g2T", [2, 128, 8])
    win_in = din("w_in", [2, D, 2048])
    qkg_in = din("qkg", [2, 2, 64])
    convw_in = din("convw", [2, 128, 2, 3])
    lcw_in = din("lcw", [2, 128, 2, 5])
    lvec_in = din("lvec", [2, 128, 2, 2, 3])
    lwa_in = din("lwa", [2, 2, 4, 64, 64])
    lwi_in = din("lwi", [2, 2, 4, 64, 64])
    wout_in = din("w_out", [2, D, D])
    wr_in = din("w_r", [2, D, NE])
    ew = [2, NE, D, D] if not DBG.get("small_w") else [2, NE, 8, 8]
    wg_in = din("w_gate", ew)
    wu_in = din("w_up", ew)
    wd_in = din("w_down", ew)
    fg_in = din("fg", [1, D])
    rope_in = din("rope", [SEQ, 64])
    cst_in = din("cst", [128, 3, 128])
    out = nc.dram_tensor("out", [SEQ, D], F32, kind="ExternalOutput").ap()

    xa = dscratch("xa", [NTOK, D], F32)
    qT = dscratch("qT", [4, 128, NTOK], BF)
    featT = dscratch("featT", [10, 128, NTOK], F32)
    mixT = dscratch("mixT", [8, 128, NTOK], BF)
    h2D = dscratch("h2D", [NTOK, ROWW], BF)
    xs = dscratch("xs", [XS_ROWS, ROWW], BF)
    hD = dscratch("hD", [2, 2, 128, NTOK], F32)
    wbf = nc.dram_tensor("wbf", [3, NE, D, D], BF, kind="Internal").ap() if not DBG.get("small_w") else None
    woutbf = nc.dram_tensor("woutbf", [D, D], BF, kind="Internal").ap()
    winbf = nc.dram_tensor("winbf", [D, 2048], BF, kind="Internal").ap()
    dbg = dscratch("dbg", [128, 4096], F32) if debug else None

    top = contextlib.ExitStack()

    uniq = [0]

    def sb(es, name, shape, dt):
        uniq[0] += 1
        return es.enter_context(nc.sbuf_tensor(f"s{uniq[0]}_{name}", list(shape), dt))

    def ps(es, name, shape, dt, key=None):
        nbytes = int(np.prod(shape[1:])) * (4 if dt in (F32, I32, U32) else 2)
        assert nbytes in (2048, 4096), (name, shape, nbytes)
        S.excl.add(key if key is not None else name)
        uniq[0] += 1
        return es.enter_context(nc.psum_tensor(f"p{uniq[0]}_{name}", list(shape), dt))

    def ring(es, name, n, shape, dt, psum=False):
        if psum:
            return Ring([ps(es, f"{name}{i}", shape, dt, key=(name, i)) for i in range(n)], name)
        return Ring([sb(es, f"{name}{i}", shape, dt) for i in range(n)], name)

    pool_regs = {}

    def preg(en, val):
        if val not in pool_regs:
            pool_regs[val] = en.to_reg(val)
        return pool_regs[val]

    def I(eng, name, reads, writes, **kw):
        return S.op(eng, lambda e, kw=kw, name=name: getattr(e, name)(**kw), reads, writes)

    def DMA(eng, reads, writes, **kw):
        return S.dma(eng, lambda e, kw=kw: e.dma_start(**kw), reads, writes)

    S.start()

    cst_f = sb(top, "cst_f", [128, 3, 128], F32)
    ident_b = sb(top, "ident_b", [128, 128], BF)
    ltri_b = sb(top, "ltri_b", [128, 128], BF)
    ones_b = sb(top, "ones_b", [128, 128], BF)
    modT = sb(top, "modT", [128, 2, 48, 2], F32)
    aff_all = sb(top, "aff_all", [128, NT, NE], F32)
    tokid = sb(top, "tokid", [128, NT], I32)
    zcol = sb(top, "zcol", [128, 1], F32)
    ident_f = cst_f[:, 0, :]
    ltri_f = cst_f[:, 1, :]
    ones_f = cst_f[:, 2, :]

    DMA("sp", [], ["cst_f"], out=cst_f[:], in_=cst_in)
    I("dve", "tensor_copy", ["cst_f"], ["ident_b"], out=ident_b[:], in_=ident_f)
    I("dve", "tensor_copy", ["cst_f"], ["ltri_b"], out=ltri_b[:], in_=ltri_f)
    I("dve", "tensor_copy", ["cst_f"], ["ones_b"], out=ones_b[:], in_=ones_f)
    I("pool", "iota", [], ["tokid"], out=tokid[:], pattern=[[128, NT]], base=0, channel_multiplier=1)
    I("pool", "memset", [], ["zcol"], ap=zcol[:], constant=0.0)

    def rstd_chain(ss, ssk, tmp, tmpk, rs, rsk, n, inv_n):
        I("dve", "tensor_scalar", [ssk], [tmpk], out=tmp, in0=ss, scalar1=inv_n, scalar2=EPS,
          op0=ALU.mult, op1=ALU.add)
        I("act", "activation", [tmpk], [tmpk], out=tmp, in_=tmp, func=AF.Sqrt)
        I("dve", "reciprocal", [tmpk], [rsk], out=rs, in_=tmp)

    with contextlib.ExitStack() as es:
        cT_sb = sb(es, "cT_sb", [128, 8, 2], F32)
        sc = sb(es, "sc", [128, 8, 2], F32)
        bT = sb(es, "bT", [128, 2, 48], F32)
        wm = ring(es, "wm", 2, [128, 6 * D], F32)
        pm = ring(es, "pm", 2, [128, 512], F32, psum=True)
        DMA("sp", [], ["cT_sb"], out=cT_sb[:], in_=cT_in)
        DMA("sp", [], ["bT"], out=bT[:], in_=bmodT_in.rearrange("l p n -> p l n"))
        I("act", "activation", ["cT_sb"], ["sc"], out=sc[:], in_=cT_sb[:], func=AF.Silu)
        for l in range(nlayers):
            acc = modT[:, l].rearrange("p n s -> p (n s)")
            for kc in range(8):
                w, wk = wm.next()
                DMA("sp", [], [wk], out=w[:], in_=wmod_in[l, kc * 128:(kc + 1) * 128, :])
                p, pk = pm.next()
                for n in range(48):
                    I("pe", "matmul", [wk, "sc"], [pk], out=p[:, 2 * n:2 * n + 2],
                      lhsT=w[:, n * 128:(n + 1) * 128], rhs=sc[:, kc, :], start=True, stop=True)
                if kc == 0:
                    I("dve", "tensor_copy", [pk], [("modT", l)], out=acc, in_=p[:, 0:96])
                else:
                    I("dve", "tensor_tensor", [pk, ("modT", l)], [("modT", l)], out=acc, in0=acc, in1=p[:, 0:96],
                      op=ALU.add)
            I("dve", "tensor_tensor", ["bT", ("modT", l)], [("modT", l)], out=modT[:, l], in0=modT[:, l],
              in1=bT[:, l, :].unsqueeze(2).to_broadcast([128, 48, 2]), op=ALU.add)
        if debug:
            DMA("sp", [("modT", 0), ("modT", 1)], ["dbg"], out=dbg[:, 0:192],
                in_=modT[:].rearrange("p l n s -> p (l n s)"))
    S.barrier()
    if stop_after == "p0":
        return finish(nc, S, top)

    def src_rows(l, r0, n):
        if l == 0:
            if r0 < CTX:
                return ctx_in[r0:r0 + n, :]
            return x_in[r0 - CTX:r0 - CTX + n, :]
        return xa[r0:r0 + n, :]

    def make_bc(es_unused, dst, dstk, src_col, srck, tmpD, tmpDk, pbc, pbck, eng_toggle=[0]):
        for h in range(2):
            for j in range(4):
                kc = h * 4 + j
                I("dve", "tensor_scalar", ["cst_f", srck], [tmpDk], out=tmpD[:], in0=ident_f,
                  scalar1=src_col(kc), scalar2=None, op0=ALU.mult)
                I("pe", "matmul", [tmpDk, "cst_f"], [pbck], out=pbc[:, j * 128:(j + 1) * 128], lhsT=ones_f,
                  rhs=tmpD[:], start=True, stop=True)
            I("act", "copy", [pbck], [dstk], out=dst[:, h * 512:(h + 1) * 512], in_=pbc[:])

    units = [(0, 2, True)] + [(CTX + 512 * i, 4, False) for i in range(16)]

    for l in range(nlayers):
        need_ctx = l < nlayers - 1 or (nlayers == 1 and debug)
        last_layer = (l == 1)
        with contextlib.ExitStack() as LS:
            G1T = sb(LS, "G1T", [128, 8, 2], F32)
            G2T = sb(LS, "G2T", [128, 8, 2], F32)
            gT = sb(LS, "gT", [128, 2, 8], F32)
            DMA("sp", [], ["gT"], out=gT[:, 0, :], in_=g1T_in[l])
            DMA("sp", [], ["gT"], out=gT[:, 1, :], in_=g2T_in[l])

            def modsl(i):
                return modT[:, l, i * 8:(i + 1) * 8, :]
            I("dve", "tensor_scalar", [], ["G1T"], out=G1T[:], in0=modsl(1), scalar1=1.0, scalar2=None, op0=ALU.add)
            I("dve", "tensor_tensor", ["G1T", "gT"], ["G1T"], out=G1T[:], in0=G1T[:],
              in1=gT[:, 0, :].unsqueeze(2).to_broadcast([128, 8, 2]), op=ALU.mult)
            I("dve", "tensor_scalar", [], ["G2T"], out=G2T[:], in0=modsl(4), scalar1=1.0, scalar2=None, op0=ALU.add)
            I("dve", "tensor_tensor", ["G2T", "gT"], ["G2T"], out=G2T[:], in0=G2T[:],
              in1=gT[:, 1, :].unsqueeze(2).to_broadcast([128, 8, 2]), op=ALU.mult)
            sh1T = modsl(0)
            sh2T = modsl(3)

            with contextlib.ExitStack() as KV:
                KTz = [sb(KV, f"KTz{i}", [128, NTOK], BF) for i in range(2)]
                Vp = sb(KV, "Vp", [128, NT, 2, 128], BF)
                if "memsets" not in DBG["skip"]:
                    I("pool", "memset", [], ["KTz0z"], ap=KTz[0][64:128, :], constant=0.0)
                    I("pool", "memset", [], ["KTz1z"], ap=KTz[1][0:64, :], constant=0.0)
                    I("pool", "memset", [], ["Vp1"], ap=Vp[:, :, 0, 64:128], constant=1.0)
                    I("pool", "memset", [], ["Vp1"], ap=Vp[:, :, 1, 0:64], constant=1.0)

                with contextlib.ExitStack() as es:
                    win = sb(es, "win", [128, 8, 2048], BF)
                    if l == 0:
                        for kc in range(8):
                            for hh in range(2):
                                DMA("pool", [], ["win"], out=win[:, kc, hh * 1024:(hh + 1) * 1024],
                                    in_=win_in[l, kc * 128:(kc + 1) * 128, hh * 1024:(hh + 1) * 1024])
                    else:
                        for h_ in range(2):
                            DMA("sp", [], ["win"], out=win[:, 4 * h_:4 * h_ + 4, :],
                                in_=winbf[512 * h_:512 * (h_ + 1), :].rearrange("(k p) n -> p k n", p=128))
                    gqk = sb(es, "gqk", [128, 2, 64], F32)
                    if "gqk" not in DBG["skip"]:
                        DMA("sp", [], ["gqk"], out=gqk[:].rearrange("p a b -> p (a b)"),
                            in_=qkg_in[l:l + 1].rearrange("o a b -> o (a b)").to_broadcast([128, 128]))
                    I("dve", "tensor_scalar", ["gqk"], ["gqk"], out=gqk[:, 0, :], in0=gqk[:, 0, :], scalar1=0.125,
                      scalar2=None, op0=ALU.mult)
                    xt_r = ring(es, "xt", 3, [128, D], F32)
                    xn_r = ring(es, "xn", 2, [128, D], BF)
                    junk = sb(es, "junk", [128, D], BF)
                    sm_r = ring(es, "sm", 3, [128, 4], F32)
                    NHX = 3
                    hx_t = [sb(es, f"hxT{i}", [128, 8, 512], BF) for i in range(NHX)]
                    qTs_t = [sb(es, f"qTs{i}", [128, 4, 512], BF) for i in range(2)]
                    fs_r = ring(es, "fs", 3, [128, 512], F32)
                    pT_r = ring(es, "pT", 1, [128, 8, 128], BF, psum=True)
                    pf_r = ring(es, "pf", 1, [128, 512], F32, psum=True)
                    gq_bc = gqk[:, 0, :].unsqueeze(1).to_broadcast([128, 8, 64])
                    gk_bc = gqk[:, 1, :].unsqueeze(1).to_broadcast([128, 2, 64])
                    ulist = units if DBG["units"] is None else units[:DBG["units"]]
                    prog1 = {"a": 0, "b1": 0, "b2": 0, "b1n": {}}

                    def a_stream():
                        for ui, (tok0, ntl, is_ctx) in enumerate(ulist):
                            while min(prog1["b1"], prog1["b2"]) < ui - (NHX - 1):
                                yield
                            s = 1 if is_ctx else 0
                            hx, hxk = hx_t[ui % NHX], ("hx", ui % NHX)
                            for ti in range(ntl):
                                t = tok0 // 128 + ti
                                xt, xtk = xt_r.next()
                                DMA("sp", [], [xtk], out=xt[:], in_=src_rows(l, tok0 + ti * 128, 128))
                                sm, smk = sm_r.next()
                                I("pool", "memset", [], [smk], ap=sm[:, 0:1], constant=0.0)
                                yield
                                I("act", "activation", [xtk, smk], ["junk", smk], out=junk[:], in_=xt[:], func=AF.Square,
                                  accum_out=sm[:, 0:1])
                                yield
                                I("dve", "tensor_scalar", [smk], [smk], out=sm[:, 1:2], in0=sm[:, 0:1], scalar1=1.0 / D,
                                  scalar2=EPS, op0=ALU.mult, op1=ALU.add)
                                yield
                                I("act", "activation", [smk], [smk], out=sm[:, 1:2], in_=sm[:, 1:2], func=AF.Sqrt)
                                yield
                                I("dve", "reciprocal", [smk], [smk], out=sm[:, 2:3], in_=sm[:, 1:2])
                                yield
                                xn, xnk = xn_r.next()
                                I("dve", "tensor_scalar", [xtk, smk], [xnk], out=xn[:], in0=xt[:], scalar1=sm[:, 2:3],
                                  scalar2=None, op0=ALU.mult)
                                yield
                                pT, pTk = pT_r.next()
                                for kc in range(8):
                                    I("pe", "transpose", [xnk, "ident_b"], [pTk], out=pT[:, kc, :],
                                      in_=xn[:, kc * 128:(kc + 1) * 128], identity=ident_b[:])
                                yield
                                for kc in range(8):
                                    dst = hx[:, kc, ti * 128:(ti + 1) * 128]
                                    if t % 2 == 0:
                                        I("act", "activation", [pTk, "G1T"], [hxk], out=dst, in_=pT[:, kc, :],
                                          func=AF.Identity, scale=G1T[:, kc, s:s + 1], bias=sh1T[:, kc, s:s + 1])
                                    else:
                                        I("dve", "tensor_scalar", [pTk, "G1T"], [hxk], out=dst, in0=pT[:, kc, :],
                                          scalar1=G1T[:, kc, s:s + 1], scalar2=sh1T[:, kc, s:s + 1],
                                          op0=ALU.mult, op1=ALU.add)
                                    if kc % 2 == 1:
                                        yield
                            prog1["a"] = ui + 1

                    def b1_stream(par):
                        P = f"b1{par}_"
                        pq = ps(es, P + "pq", [128, 512], F32)
                        pkv = ps(es, P + "pkv", [128, 512], F32)
                        pqT = ps(es, P + "pqT", [128, 8, 128], BF)
                        pqk, pkvk, pqTk = P + "pq", P + "pkv", P + "pqT"
                        sq_r = ring(es, P + "sq", 1, [128, 640], F32)
                        qs_r = ring(es, P + "qs", 1, [128, 32], F32)
                        qn_r = ring(es, P + "qn", 1, [128, 10, 64], F32)
                        ra_r = ring(es, P + "ra", 1, [128, 10, 32], F32)
                        rb_r = ring(es, P + "rb", 1, [128, 10, 32], F32)
                        qr_r = ring(es, P + "qr", 1, [128, 10, 64], BF)
                        rope_r = ring(es, P + "rope", 1, [128, 64], F32)
                        for ui, (tok0, ntl, is_ctx) in enumerate(ulist):
                            while prog1["a"] <= ui:
                                yield
                            NTu = ntl * 128
                            hx, hxk = hx_t[ui % NHX], ("hx", ui % NHX)
                            qTs, qTsk = qTs_t[ui % 2], ("qTs", ui % 2)
                            for ti in range(par, ntl, 2):
                                t = tok0 // 128 + ti
                                for kc in range(8):
                                    I("pe", "matmul", [hxk, "win"], [pqk], out=pq[:],
                                      lhsT=hx[:, kc, ti * 128:(ti + 1) * 128], rhs=win[:, kc, 0:512],
                                      start=(kc == 0), stop=(kc == 7))
                                yield
                                for kc in range(8):
                                    I("pe", "matmul", [hxk, "win"], [pkvk], out=pkv[:, 0:256],
                                      lhsT=hx[:, kc, ti * 128:(ti + 1) * 128], rhs=win[:, kc, 512:768],
                                      start=(kc == 0), stop=(kc == 7))
                                yield
                                sq, sqk = sq_r.next()
                                qs, qsk = qs_r.next()
                                qn, qnk = qn_r.next()
                                qr, qrk = qr_r.next()
                                I("act", "activation", [pqk], [sqk], out=sq[:, 0:512], in_=pq[:], func=AF.Square)
                                I("act", "activation", [pkvk], [sqk], out=sq[:, 512:640], in_=pkv[:, 0:128],
                                  func=AF.Square)
                                yield
                                I("dve", "reduce_sum", [sqk], [qsk], out=qs[:, 0:10],
                                  in_=sq[:].rearrange("p (h d) -> p h d", d=64), axis=AX.X)
                                yield
                                I("dve", "tensor_scalar", [qsk], [qsk], out=qs[:, 10:20], in0=qs[:, 0:10], scalar1=1.0 / 64,
                                  scalar2=EPS, op0=ALU.mult, op1=ALU.add)
                                yield
                                I("act", "activation", [qsk], [qsk], out=qs[:, 10:20], in_=qs[:, 10:20], func=AF.Sqrt)
                                yield
                                I("dve", "reciprocal", [qsk], [qsk], out=qs[:, 20:30], in_=qs[:, 10:20])
                                yield
                                I("dve", "tensor_tensor", [pqk, qsk], [qnk], out=qn[:, 0:8, :],
                                  in0=pq[:].rearrange("p (h d) -> p h d", d=64),
                                  in1=qs[:, 20:28].unsqueeze(2).to_broadcast([128, 8, 64]), op=ALU.mult)
                                I("dve", "tensor_tensor", [pkvk, qsk, qnk], [qnk], out=qn[:, 8:10, :],
                                  in0=pkv[:, 0:128].rearrange("p (h d) -> p h d", d=64),
                                  in1=qs[:, 28:30].unsqueeze(2).to_broadcast([128, 2, 64]), op=ALU.mult)
                                I("act", "copy", [pkvk], [("Vp", t)], out=Vp[:, t, 0, 0:64], in_=pkv[:, 128:192])
                                I("act", "copy", [pkvk, ("Vp", t)], [("Vp", t)], out=Vp[:, t, 1, 64:128],
                                  in_=pkv[:, 192:256])
                                yield
                                if is_ctx:
                                    I("pool", "tensor_tensor", [qnk, "gqk"], [qrk], out=qr[:, 0:8, :], in0=qn[:, 0:8, :],
                                      in1=gq_bc, op=ALU.mult)
                                    I("pool", "tensor_tensor", [qnk, "gqk", qrk], [qrk], out=qr[:, 8:10, :],
                                      in0=qn[:, 8:10, :], in1=gk_bc, op=ALU.mult)
                                    yield
                                else:
                                    I("pool", "tensor_tensor", [qnk, "gqk"], [qnk], out=qn[:, 0:8, :], in0=qn[:, 0:8, :],
                                      in1=gq_bc, op=ALU.mult)
                                    I("pool", "tensor_tensor", [qnk, "gqk"], [qnk], out=qn[:, 8:10, :], in0=qn[:, 8:10, :],
                                      in1=gk_bc, op=ALU.mult)
                                    rp, rpk = rope_r.next()
                                    DMA("sp", [], [rpk], out=rp[:], in_=rope_in[(t - 2) * 128:(t - 1) * 128, :])
                                    yield
                                    ra, rak = ra_r.next()
                                    rb, rbk = rb_r.next()
                                    cosb = rp[:, 0:32].unsqueeze(1).to_broadcast([128, 10, 32])
                                    sinb = rp[:, 32:64].unsqueeze(1).to_broadcast([128, 10, 32])
                                    t1 = qn[:, :, 0:32]
                                    t2 = qn[:, :, 32:64]
                                    I("dve", "tensor_tensor", [qnk, rpk], [rak], out=ra[:], in0=t1, in1=cosb, op=ALU.mult)
                                    I("pool", "tensor_tensor", [qnk, rpk], [rbk], out=rb[:], in0=t2, in1=sinb, op=ALU.mult)
                                    yield
                                    I("dve", "tensor_tensor", [rak, rbk], [qrk], out=qr[:, :, 0:32], in0=ra[:], in1=rb[:],
                                      op=ALU.subtract)
                                    yield
                                    I("dve", "tensor_tensor", [qnk, rpk, rak], [rak], out=ra[:], in0=t1, in1=sinb,
                                      op=ALU.mult)
                                    I("pool", "tensor_tensor", [qnk, rpk, rbk], [rbk], out=rb[:], in0=t2, in1=cosb,
                                      op=ALU.mult)
                                    yield
                                    I("dve", "tensor_tensor", [rak, rbk, qrk], [qrk], out=qr[:, :, 32:64], in0=ra[:],
                                      in1=rb[:], op=ALU.add)
                                    yield
                                qrf = qr[:].rearrange("p h d -> p (h d)")
                                for j in range(5):
                                    I("pe", "transpose", [qrk, "ident_b"], [pqTk], out=pqT[:, j, :],
                                      in_=qrf[:, j * 128:(j + 1) * 128], identity=ident_b[:])
                                yield
                                I("act", "copy", [pqTk], [(qTsk, ti)], out=qTs[:, :, ti * 128:(ti + 1) * 128],
                                  in_=pqT[:, 0:4, :])
                                I("act", "copy", [pqTk], [("KT", t)], out=KTz[0][0:64, t * 128:(t + 1) * 128],
                                  in_=pqT[0:64, 4, :])
                                I("act", "copy", [pqTk, ("KT", t)], [("KT", t)],
                                  out=KTz[1][64:128, t * 128:(t + 1) * 128], in_=pqT[64:128, 4, :])
                                yield
                            prog1["b1n"][ui] = prog1["b1n"].get(ui, 0) + 1
                            if prog1["b1n"][ui] == 2:
                                DMA("pool", [(qTsk, ti) for ti in range(ntl)], [("qT", tok0)],
                                    out=qT[:, :, tok0:tok0 + NTu].rearrange("j p n -> p j n"), in_=qTs[:, :, 0:NTu])
                                prog1["b1"] = ui + 1

                    def b2_stream():
                        for ui, (tok0, ntl, is_ctx) in enumerate(ulist):
                            while prog1["a"] <= ui:
                                yield
                            NTu = ntl * 128
                            hx, hxk = hx_t[ui % NHX], ("hx", ui % NHX)
                            for n in range(10):
                                pf, pfk = pf_r.next()
                                for kc in range(8):
                                    I("pe", "matmul", [hxk, "win"], [pfk], out=pf[:, 0:NTu],
                                      lhsT=win[:, kc, 768 + n * 128:768 + (n + 1) * 128], rhs=hx[:, kc, 0:NTu],
                                      start=(kc == 0), stop=(kc == 7))
                                    if kc % 4 == 3:
                                        yield
                                fs, fsk = fs_r.next()
                                if n % 2 == 0:
                                    I("act", "copy", [pfk], [fsk], out=fs[:, 0:NTu], in_=pf[:, 0:NTu])
                                else:
                                    I("dve", "tensor_copy", [pfk], [fsk], out=fs[:, 0:NTu], in_=pf[:, 0:NTu])
                                DMA("pool", [fsk], [("featT", n, tok0)], out=featT[n, :, tok0:tok0 + NTu],
                                    in_=fs[:, 0:NTu])
                                yield
                            prog1["b2"] = ui + 1
                    run_streams([a_stream(), b1_stream(0), b1_stream(1), b2_stream()])
                S.barrier()
                if stop_after == f"p1_{l}":
                    if debug:
                        with contextlib.ExitStack() as es:
                            kd = sb(es, "kd", [128, 1024], F32)
                            I("dve", "tensor_copy", [], ["kd"], out=kd[:, 0:512], in_=KTz[0][:, 0:512])
                            I("dve", "tensor_copy", ["kd"], ["kd"], out=kd[:, 512:1024], in_=KTz[1][:, 0:512])
                            DMA("sp", ["kd"], ["dbg"], out=dbg[:, 0:1024], in_=kd[:])
                            vd = sb(es, "vd", [128, 1024], F32)
                            I("dve", "tensor_copy", [], ["vd"], out=vd[:],
                              in_=Vp[:, 0:4].rearrange("p t h d -> p (t h d)"))
                            DMA("sp", ["vd"], ["dbg"], out=dbg[:, 1024:2048], in_=vd[:])
                    return finish(nc, S, top)

                def seq_bounds(t0):
                    return (0, CTX) if t0 < CTX else (CTX, NTOK)

                CSEG = 512
                segs_c = [(0, CTX)] + [(CTX + CSEG * i, CSEG) for i in range(SEQ // CSEG)]
                SEG = 256
                segs_l = [(0, CTX)] + [(CTX + SEG * i, SEG) for i in range(SEQ // SEG)]
                GSEG = 512
                segs_g = [(0, CTX)] + [(CTX + GSEG * i, GSEG) for i in range(SEQ // GSEG)]
                with contextlib.ExitStack() as es:
                    cw = sb(es, "cw", [128, 2, 3], F32)
                    DMA("sp", [], ["cw"], out=cw[:], in_=convw_in[l])
                    lcw = sb(es, "lcw", [128, 2, 5], F32)
                    lvec = sb(es, "lvec", [128, 2, 2, 3], F32)
                    nlv = sb(es, "nlv", [128, 2, 2, 2], F32)
                    cneg = sb(es, "cneg", [128, 2, 2], F32)
                    DMA("sp", [], ["lcw"], out=lcw[:], in_=lcw_in[l])
                    DMA("sp", [], ["lvec"], out=lvec[:], in_=lvec_in[l])
                    I("dve", "tensor_scalar", ["lvec"], ["nlv"], out=nlv[:], in0=lvec[:, :, :, 0:2], scalar1=-1.0,
                      scalar2=None, op0=ALU.mult)
                    I("act", "activation", ["lvec"], ["cneg"], out=cneg[:], in_=lvec[:, :, :, 2], func=AF.Exp, scale=-1.0)
                    I("dve", "tensor_scalar", ["cneg"], ["cneg"], out=cneg[:], in0=cneg[:], scalar1=1.0, scalar2=None,
                      op0=ALU.add)
                    I("act", "activation", ["cneg"], ["cneg"], out=cneg[:], in_=cneg[:], func=AF.Ln)
                    I("dve", "tensor_scalar", ["cneg"], ["cneg"], out=cneg[:], in0=cneg[:], scalar1=-8.0, scalar2=None,
                      op0=ALU.mult)
                    Wblk = sb(es, "Wblk", [128, 2, 2, 2, 128], F32)
                    I("pool", "memset", [], ["Wblk"], ap=Wblk[:], constant=0.0)
                    for c in range(2):
                        for d in range(2):
                            for bi_ in range(2):
                                blk = 2 * c + bi_
                                DMA("sp", [], ["Wblk"],
                                    out=Wblk[bi_ * 64:(bi_ + 1) * 64, c, d, 0, bi_ * 64:(bi_ + 1) * 64], in_=lwa_in[l, d, blk])
                                DMA("sp", [], ["Wblk"],
                                    out=Wblk[bi_ * 64:(bi_ + 1) * 64, c, d, 1, bi_ * 64:(bi_ + 1) * 64], in_=lwi_in[l, d, blk])
                    prog2 = {"scan": [0, 0]}

                    def conv_stream(c):
                        P = f"cv{c}_"
                        ccs = sb(es, P + "ccs", [128, CSEG + 2], F32)
                        chs = sb(es, P + "chs", [128, CSEG + 2], F32)
                        cbs = sb(es, P + "cbs", [128, CSEG], F32)
                        uu = sb(es, P + "uu", [128, CSEG + 2], F32)
                        yy = sb(es, P + "yy", [128, CSEG], F32)
                        oo = sb(es, P + "oo", [128, CSEG], BF)
                        for (t0, n) in segs_c:
                            if t0 < CTX and not need_ctx:
                                continue
                            lo_s, hi_s = seq_bounds(t0)
                            lo = max(t0 - 1, lo_s)
                            hi = min(t0 + n + 1, hi_s)
                            off = lo - (t0 - 1)
                            DMA("sp", [], [P + "ccs"], out=ccs[:, off:off + hi - lo], in_=featT[2 + c, :, lo:hi])
                            DMA("sp", [], [P + "chs"], out=chs[:, off:off + hi - lo], in_=featT[4 + c, :, lo:hi])
                            DMA("sp", [], [P + "cbs"], out=cbs[:, 0:n], in_=featT[0 + c, :, t0:t0 + n])
                            yield
                            I("pool", "tensor_tensor", [P + "ccs", P + "chs"], [P + "uu"], out=uu[:, off:off + hi - lo],
                              in0=ccs[:, off:off + hi - lo], in1=chs[:, off:off + hi - lo], op=ALU.mult)
                            if off == 1:
                                I("pool", "memset", [P + "uu"], [P + "uu"], ap=uu[:, 0:1], constant=0.0)
                            if hi < t0 + n + 1:
                                I("pool", "memset", [P + "uu"], [P + "uu"], ap=uu[:, n + 1:n + 2], constant=0.0)
                            yield
                            I("dve", "tensor_scalar", [P + "uu", "cw"], [P + "yy"], out=yy[:, 0:n], in0=uu[:, 1:n + 1],
                              scalar1=cw[:, c, 1:2], scalar2=None, op0=ALU.mult)
                            yield
                            I("dve", "scalar_tensor_tensor", [P + "uu", "cw", P + "yy"], [P + "yy"], out=yy[:, 0:n],
                              in0=uu[:, 0:n], scalar=cw[:, c, 0:1], in1=yy[:, 0:n], op0=ALU.mult, op1=ALU.add)
                            yield
                            I("dve", "scalar_tensor_tensor", [P + "uu", "cw", P + "yy"], [P + "yy"], out=yy[:, 0:n],
                              in0=uu[:, 2:n + 2], scalar=cw[:, c, 2:3], in1=yy[:, 0:n], op0=ALU.mult, op1=ALU.add)
                            yield
                            I("dve", "tensor_tensor", [P + "yy", P + "cbs"], [P + "oo"], out=oo[:, 0:n], in0=yy[:, 0:n],
                              in1=cbs[:, 0:n], op=ALU.mult)
                            DMA("pool", [P + "oo"], [("mixT", 4 + c, t0)], out=mixT[4 + c, :, t0:t0 + n], in_=oo[:, 0:n])
                            yield

                    def scan_lane(d):
                        P = f"ls{d}_"
                        lus = sb(es, P + "lus", [128, SEG + 3], F32)
                        uc = sb(es, P + "uc", [128, SEG], F32)
                        rr = sb(es, P + "rr", [128, SEG], F32)
                        ii = sb(es, P + "ii", [128, SEG], F32)
                        tt = sb(es, P + "tt", [128, SEG], F32)
                        hh = sb(es, P + "hh", [128, SEG], F32)
                        carry = sb(es, P + "carry", [128, 1], F32)
                        pp = ps(es, P + "pp", [128, 512], F32)
                        order = segs_l if d == 0 else [segs_l[0]] + segs_l[:0:-1]
                        for c in range(2):
                            I("dve", "tensor_copy", ["zcol"], [P + "carry"], out=carry[:], in_=zcol[:])
                            for (t0, n) in order:
                                lo_s, hi_s = seq_bounds(t0)
                                lo = max(t0 - 2, lo_s)
                                hi = min(t0 + n + 1, hi_s)
                                off = lo - (t0 - 2)
                                if off > 0:
                                    I("pool", "memset", [], [P + "lus"], ap=lus[:, 0:2], constant=0.0)
                                if hi < t0 + n + 1:
                                    I("pool", "memset", [], [P + "lus"], ap=lus[:, n + 2:n + 3], constant=0.0)
                                DMA("sp", [], [P + "lus"], out=lus[:, off:off + hi - lo], in_=featT[6 + c, :, lo:hi])
                                yield
                                I("dve", "tensor_scalar", [P + "lus", "lcw"], [P + "uc"], out=uc[:, 0:n], in0=lus[:, 2:n + 2],
                                  scalar1=lcw[:, c, 2:3], scalar2=lcw[:, c, 4:5], op0=ALU.mult, op1=ALU.add)
                                yield
                                for k_, o_ in ((0, 0), (1, 1), (3, 3)):
                                    I("dve", "scalar_tensor_tensor", [P + "lus", "lcw", P + "uc"], [P + "uc"], out=uc[:, 0:n],
                                      in0=lus[:, o_:o_ + n], scalar=lcw[:, c, k_:k_ + 1], in1=uc[:, 0:n],
                                      op0=ALU.mult, op1=ALU.add)
                                    yield
                                I("pe", "matmul", ["Wblk", P + "uc"], [P + "pp"], out=pp[:, 0:n], lhsT=Wblk[:, c, d, 0, :],
                                  rhs=uc[:, 0:n], start=True, stop=True)
                                I("pe", "matmul", ["Wblk", P + "uc"], [P + "pp"], out=pp[:, 256:256 + n],
                                  lhsT=Wblk[:, c, d, 1, :], rhs=uc[:, 0:n], start=True, stop=True)
                                yield
                                I("act", "activation", [P + "pp", "nlv"], [P + "rr"], out=rr[:, 0:n], in_=pp[:, 0:n],
                                  func=AF.Exp, scale=-1.0, bias=nlv[:, d, c, 0:1])
                                I("act", "activation", [P + "pp", "nlv"], [P + "ii"], out=ii[:, 0:n], in_=pp[:, 256:256 + n],
                                  func=AF.Exp, scale=-1.0, bias=nlv[:, d, c, 1:2])
                                yield
                                I("dve", "tensor_scalar", [P + "rr"], [P + "rr"], out=rr[:, 0:n], in0=rr[:, 0:n], scalar1=1.0,
                                  scalar2=None, op0=ALU.add)
                                yield
                                I("dve", "reciprocal", [P + "rr"], [P + "rr"], out=rr[:, 0:n], in_=rr[:, 0:n])
                                yield
                                I("pool", "tensor_scalar", [P + "ii"], [P + "ii"], out=ii[:, 0:n], in0=ii[:, 0:n], scalar1=1.0,
                                  scalar2=None, op0=ALU.add)
                                yield
                                I("dve", "reciprocal", [P + "ii"], [P + "ii"], out=ii[:, 0:n], in_=ii[:, 0:n])
                                yield
                                I("act", "activation", [P + "rr", "cneg"], [P + "rr"], out=rr[:, 0:n], in_=rr[:, 0:n],
                                  func=AF.Exp, scale=cneg[:, d, c:c + 1])
                                I("pool", "tensor_tensor", [P + "ii", P + "uc"], [P + "ii"], out=ii[:, 0:n], in0=ii[:, 0:n],
                                  in1=uc[:, 0:n], op=ALU.mult)
                                yield
                                I("pool", "tensor_tensor", [P + "rr"], [P + "tt"], out=tt[:, 0:n], in0=rr[:, 0:n],
                                  in1=rr[:, 0:n], op=ALU.mult)
                                yield
                                I("act", "activation", [P + "tt"], [P + "tt"], out=tt[:, 0:n], in_=tt[:, 0:n], func=AF.Ln,
                                  scale=-1.0, bias=1.0)
                                I("act", "activation", [P + "tt"], [P + "tt"], out=tt[:, 0:n], in_=tt[:, 0:n], func=AF.Exp,
                                  scale=0.5)
                                yield
                                I("dve", "tensor_tensor", [P + "ii", P + "tt"], [P + "ii"], out=ii[:, 0:n], in0=ii[:, 0:n],
                                  in1=tt[:, 0:n], op=ALU.mult)
                                yield
                                if d == 0:
                                    I("dve", "tensor_tensor_scan", [P + "rr", P + "ii", P + "carry"], [P + "hh"],
                                      out=hh[:, 0:n], data0=rr[:, 0:n], data1=ii[:, 0:n], initial=carry[:, 0:1],
                                      op0=ALU.mult, op1=ALU.add)
                                    I("dve", "tensor_copy", [P + "hh", P + "carry"], [P + "carry"], out=carry[:],
                                      in_=hh[:, n - 1:n])
                                else:
                                    I("dve", "tensor_tensor_scan", [P + "rr", P + "ii", P + "carry"], [P + "hh"],
                                      out=hh[:, 0:n][:, ::-1], data0=rr[:, 0:n][:, ::-1], data1=ii[:, 0:n][:, ::-1],
                                      initial=carry[:, 0:1], op0=ALU.mult, op1=ALU.add)
                                    I("dve", "tensor_copy", [P + "hh", P + "carry"], [P + "carry"], out=carry[:],
                                      in_=hh[:, 0:1])
                                if not (t0 < CTX and not need_ctx):
                                    DMA("pool", [P + "hh"], [("hD", d, c, t0)], out=hD[d, c, :, t0:t0 + n], in_=hh[:, 0:n])
                                yield
                            prog2["scan"][c] += 1

                    def comb_stream(c):
                        P = f"cb{c}_"
                        h0 = sb(es, P + "h0", [128, GSEG], F32)
                        h1 = sb(es, P + "h1", [128, GSEG], F32)
                        lgs = sb(es, P + "lgs", [128, GSEG], F32)
                        tt = sb(es, P + "tt", [128, GSEG], F32)
                        ob = sb(es, P + "ob", [128, GSEG], BF)
                        while prog2["scan"][c] < 2:
                            yield
                        for si, (t0, n) in enumerate(segs_g):
                            if t0 < CTX and not need_ctx:
                                continue
                            rk = [("hD", d_, c, t0 + o_) for d_ in range(2) for o_ in range(0, n, SEG)]
                            DMA("sp", rk, [P + "h0"], out=h0[:, 0:n], in_=hD[0, c, :, t0:t0 + n])
                            DMA("sp", rk, [P + "h1"], out=h1[:, 0:n], in_=hD[1, c, :, t0:t0 + n])
                            DMA("sp", [], [P + "lgs"], out=lgs[:, 0:n], in_=featT[8 + c, :, t0:t0 + n])
                            yield
                            I("pool", "tensor_tensor", [P + "h0", P + "h1"], [P + "h0"], out=h0[:, 0:n], in0=h0[:, 0:n],
                              in1=h1[:, 0:n], op=ALU.add)
                            I("dve", "tensor_tensor", [P + "lgs"], [P + "tt"], out=tt[:, 0:n], in0=lgs[:, 0:n],
                              in1=lgs[:, 0:n], op=ALU.mult)
                            yield
                            I("dve", "tensor_scalar", [P + "tt"], [P + "tt"], out=tt[:, 0:n], in0=tt[:, 0:n],
                              scalar1=0.044715, scalar2=1.0, op0=ALU.mult, op1=ALU.add)
                            yield
                            I("pool", "tensor_tensor", [P + "tt", P + "lgs"], [P + "tt"], out=tt[:, 0:n], in0=tt[:, 0:n],
                              in1=lgs[:, 0:n], op=ALU.mult)
                            yield
                            I("act", "activation", [P + "tt"], [P + "tt"], out=tt[:, 0:n], in_=tt[:, 0:n],
                              func=AF.Exp, scale=-1.5957691216057308)
                            yield
                            I("dve", "tensor_scalar", [P + "tt"], [P + "tt"], out=tt[:, 0:n], in0=tt[:, 0:n], scalar1=1.0,
                              scalar2=None, op0=ALU.add)
                            yield
                            I("dve", "reciprocal", [P + "tt"], [P + "tt"], out=tt[:, 0:n], in_=tt[:, 0:n])
                            yield
                            I("pool", "tensor_tensor", [P + "tt", P + "lgs"], [P + "tt"], out=tt[:, 0:n], in0=tt[:, 0:n],
                              in1=lgs[:, 0:n], op=ALU.mult)
                            yield
                            I("dve", "tensor_tensor", [P + "tt", P + "h0"], [P + "ob"], out=ob[:, 0:n], in0=tt[:, 0:n],
                              in1=h0[:, 0:n], op=ALU.mult)
                            DMA("pool", [P + "ob"], [("mixT", 6 + c, t0)], out=mixT[6 + c, :, t0:t0 + n], in_=ob[:, 0:n])
                            yield

                    def att_stream():
                        qt_r = ring(es, "qt", 2, [128, 512], BF)
                        mo_r = ring(es, "mo", 2, [128, 512], BF)
                        pe_r = ring(es, "pex", 2, [128, 2, 512], BF)
                        rec_r = ring(es, "rec", 2, [128, 512], F32)
                        ps_r = ring(es, "pss", 2, [128, 2, 512], F32, psum=True)
                        po_r = ring(es, "po", 2, [128, 512], F32, psum=True)
                        qblocks = []
                        if need_ctx:
                            qblocks.append((0, CTX, [0, 1]))
                        for i in range(16):
                            qblocks.append((CTX + 512 * i, 512, list(range(NT))))
                        for (q0, N, kts) in qblocks:
                            for j in range(4):
                                qt, qtk = qt_r.next()
                                DMA("sp", [], [qtk], out=qt[:, 0:N], in_=qT[j, :, q0:q0 + N])
                                mo, mok = mo_r.next()
                                for half in range(2):
                                    po, pok = po_r.next()
                                    npair = len(kts) // 2

                                    def emit_s(pi_):
                                        p_, pk_ = ps_r.next()
                                        for u_ in range(2):
                                            kt = kts[2 * pi_ + u_]
                                            I("pe", "matmul", [qtk, ("KTz", half)], [pk_], out=p_[:, u_, 0:N],
                                              lhsT=KTz[half][:, kt * 128:(kt + 1) * 128], rhs=qt[:, 0:N], start=True, stop=True)
                                        return p_, pk_
                                    cur = emit_s(0)
                                    for pi_ in range(npair):
                                        nxt = emit_s(pi_ + 1) if pi_ + 1 < npair else None
                                        p_, pk_ = cur
                                        ex, exk = pe_r.next()
                                        I("act", "activation", [pk_], [exk], out=ex[:, :, 0:N], in_=p_[:, :, 0:N], func=AF.Exp)
                                        for u_ in range(2):
                                            i = 2 * pi_ + u_
                                            I("pe", "matmul", [exk, "Vp"], [pok], out=po[:, 0:N], lhsT=Vp[:, kts[i], half, :],
                                              rhs=ex[:, u_, 0:N], start=(i == 0), stop=(i == len(kts) - 1))
                                        cur = nxt
                                        yield
                                    rec, reck = rec_r.next()
                                    o_sl = slice(0, 64) if half == 0 else slice(64, 128)
                                    s_sl = slice(64, 128) if half == 0 else slice(0, 64)
                                    I("dve", "reciprocal", [pok], [reck], out=rec[o_sl, 0:N], in_=po[s_sl, 0:N])
                                    I("dve", "tensor_tensor", [pok, reck], [mok], out=mo[o_sl, 0:N], in0=po[o_sl, 0:N],
                                      in1=rec[o_sl, 0:N], op=ALU.mult)
                                DMA("pool", [mok], [("mixT", j, q0)], out=mixT[j, :, q0:q0 + N], in_=mo[:, 0:N])
                                yield

                    def wconv_stream():
                        for q4 in range(4):
                            DMA("pool", [], [("woutbf", q4)], out=woutbf[q4 * 256:(q4 + 1) * 256, :],
                                in_=wout_in[l, q4 * 256:(q4 + 1) * 256, :])
                            yield
                        if l + 1 < nlayers:
                            for kc in range(8):
                                DMA("pool", [], [("winbf", kc)], out=winbf[kc * 128:(kc + 1) * 128, :],
                                    in_=win_in[l + 1, kc * 128:(kc + 1) * 128, :])
                                yield
                        if wbf is None:
                            return
                        for e in range(NE):
                            for m_, src in enumerate((wg_in, wu_in, wd_in)):
                                for q4 in range(4):
                                    for _ in range(18):
                                        yield
                                    DMA("pool", [], [("wbf", m_, e, q4)], out=wbf[m_, e, q4 * 256:(q4 + 1) * 256, :],
                                        in_=src[l, e, q4 * 256:(q4 + 1) * 256, :])

                    run_streams([att_stream(), conv_stream(0), conv_stream(1), scan_lane(0), scan_lane(1),
                                 comb_stream(0), comb_stream(1), wconv_stream()])
            S.barrier()
            if stop_after == f"p2c_{l}":
                return finish(nc, S, top)

            with contextlib.ExitStack() as es:
                wr = sb(es, "wr", [128, 8, NE], F32)
                DMA("sp", [], ["wr"], out=wr[:], in_=wr_in[l].rearrange("(k p) e -> p k e", p=128))
                nstream = 2 if need_ctx else 1
                wout_s = [sb(es, f"wout_s{s}", [128, 8, D], BF) for s in range(nstream)]
                G2_bc = [sb(es, f"G2_bc{s}", [128, D], F32) for s in range(nstream)]
                sh2_bc = [sb(es, f"sh2_bc{s}", [128, D], F32) for s in range(nstream)]
                with contextlib.ExitStack() as es2:
                    wout = sb(es2, "wout", [128, 8, D], BF)
                    for h_ in range(2):
                        DMA("sp", [], ["wout"], out=wout[:, 4 * h_:4 * h_ + 4, :],
                            in_=woutbf[512 * h_:512 * (h_ + 1), :].rearrange("(k p) n -> p k n", p=128))
                    gate_bc = [sb(es2, f"gate_bc{s}", [128, D], F32) for s in range(nstream)]
                    tmpD = sb(es2, "tmpD", [128, 128], F32)
                    pbc = ps(es2, "pbc", [128, 512], F32)
                    for s in range(nstream):
                        make_bc(es, gate_bc[s], f"gate_bc{s}", lambda kc, s=s: modsl(2)[:, kc, s:s + 1], ("modT", l), tmpD,
                                "tmpD", pbc, "pbc")
                        make_bc(es, G2_bc[s], f"G2_bc{s}", lambda kc, s=s: G2T[:, kc, s:s + 1], "G2T", tmpD, "tmpD", pbc,
                                "pbc")
                        make_bc(es, sh2_bc[s], f"sh2_bc{s}", lambda kc, s=s: sh2T[:, kc, s:s + 1], ("modT", l), tmpD,
                                "tmpD", pbc, "pbc")
                        for kc in range(8):
                            I("dve" if kc % 2 == 0 else "pool", "tensor_tensor", ["wout", f"gate_bc{s}"], [f"wout_s{s}"],
                              out=wout_s[s][:, kc, :], in0=wout[:, kc, :], in1=gate_bc[s][:], op=ALU.mult)
                    S.barrier()
                junk3 = sb(es, "junk3", [128, D], BF)

                def p3_stream(k, nk):
                    P = f"p3{k}_"
                    mx = sb(es, P + "mx", [128, 8, 512], BF)
                    xt = sb(es, P + "xt", [128, D], F32)
                    x1 = sb(es, P + "x1", [128, D], F32)
                    h2 = sb(es, P + "h2", [128, D], F32)
                    row = sb(es, P + "row", [128, ROWW], BF)
                    h2T = sb(es, P + "h2T", [128, 8, 128], F32)
                    sm = sb(es, P + "sm", [128, 8], F32)
                    ex = sb(es, P + "ex", [128, NE], F32)
                    py = ps(es, P + "py", [128, 512], F32)
                    pta = ps(es, P + "pta", [128, 4, 128], F32)
                    ptb = ps(es, P + "ptb", [128, 4, 128], F32)
                    plog_t = ps(es, P + "plog", [128, 512], F32)
                    plog = plog_t[:, 0:NE]
                    mxk, xtk, x1k, h2k, rowk, h2Tk, smk, exk = (P + n_ for n_ in ("mx", "xt", "x1", "h2", "row", "h2T", "sm", "ex"))
                    pyk, ptak, ptbk, plogk = P + "py", P + "pta", P + "ptb", P + "plog"
                    ulist = [u_ for u_ in units if not (u_[2] and not need_ctx)]
                    for ui, (tok0, ntl, is_ctx) in enumerate(ulist):
                        if ui % nk != k:
                            continue
                        s = 1 if is_ctx else 0
                        NTu = ntl * 128
                        DMA("sp", [], [mxk], out=mx[:, :, 0:NTu], in_=mixT[:, :, tok0:tok0 + NTu].rearrange("c p n -> p c n"))
                        for ti in range(ntl):
                            t = tok0 // 128 + ti
                            DMA("sp", [], [xtk], out=xt[:], in_=src_rows(l, tok0 + ti * 128, 128))
                            yield
                            for nh in range(2):
                                cs = slice(nh * 512, (nh + 1) * 512)
                                for kc in range(8):
                                    I("pe", "matmul", [mxk, f"wout_s{s}"], [pyk], out=py[:],
                                      lhsT=mx[:, kc, ti * 128:(ti + 1) * 128], rhs=wout_s[s][:, kc, cs],
                                      start=(kc == 0), stop=(kc == 7))
                                yield
                                I("dve", "tensor_tensor", [pyk, xtk], [x1k], out=x1[:, cs], in0=py[:], in1=xt[:, cs],
                                  op=ALU.add)
                                yield
                            DMA("pool", [x1k], [("xa", t)], out=xa[tok0 + ti * 128:tok0 + (ti + 1) * 128, :], in_=x1[:])
                            I("pool", "memset", [], [smk], ap=sm[:], constant=0.0)
                            yield
                            I("act", "activation", [x1k, smk], ["junk3", smk], out=junk3[:], in_=x1[:], func=AF.Square,
                              accum_out=sm[:, 0:1])
                            yield
                            I("dve", "tensor_scalar", [smk], [smk], out=sm[:, 1:2], in0=sm[:, 0:1], scalar1=1.0 / D,
                              scalar2=EPS, op0=ALU.mult, op1=ALU.add)
                            yield
                            I("act", "activation", [smk], [smk], out=sm[:, 1:2], in_=sm[:, 1:2], func=AF.Sqrt)
                            yield
                            I("dve", "reciprocal", [smk], [smk], out=sm[:, 2:3], in_=sm[:, 1:2])
                            yield
                            I("dve", "scalar_tensor_tensor", [x1k, smk, f"G2_bc{s}"], [h2k], out=h2[:], in0=x1[:],
                              scalar=sm[:, 2:3], in1=G2_bc[s][:], op0=ALU.mult, op1=ALU.mult)
                            yield
                            I("pool", "tensor_tensor", [h2k, f"sh2_bc{s}"], [h2k], out=h2[:], in0=h2[:], in1=sh2_bc[s][:],
                              op=ALU.add)
                            yield
                            I("act", "copy", [h2k], [rowk], out=row[:, 0:D], in_=h2[:])
                            for kc in range(8):
                                pt_ = pta if kc < 4 else ptb
                                I("pe", "transpose", [h2k, "cst_f"], [ptak if kc < 4 else ptbk], out=pt_[:, kc % 4, :],
                                  in_=h2[:, kc * 128:(kc + 1) * 128], identity=ident_f)
                            yield
                            I("act", "copy", [ptak], [h2Tk], out=h2T[:, 0:4, :], in_=pta[:])
                            I("dve", "tensor_copy", [ptbk, h2Tk], [h2Tk], out=h2T[:, 4:8, :], in_=ptb[:])
                            yield
                            for kc in range(8):
                                I("pe", "matmul", [h2Tk, "wr"], [plogk], out=plog, lhsT=h2T[:, kc, :], rhs=wr[:, kc, :],
                                  start=(kc == 0), stop=(kc == 7))
                            yield
                            I("dve", "reduce_max", [plogk, smk], [smk], out=sm[:, 3:4], in_=plog, axis=AX.X)
                            yield
                            I("dve", "tensor_scalar", [smk], [smk], out=sm[:, 3:4], in0=sm[:, 3:4], scalar1=-1.0, scalar2=None,
                              op0=ALU.mult)
                            yield
                            I("act", "activation", [plogk, smk], [exk, smk], out=ex[:], in_=plog, func=AF.Exp,
                              bias=sm[:, 3:4], accum_out=sm[:, 4:5])
                            yield
                            I("dve", "reciprocal", [smk], [smk], out=sm[:, 5:6], in_=sm[:, 4:5])
                            yield
                            I("dve", "tensor_scalar", [exk, smk], [("aff", t)], out=aff_all[:, t, :], in0=ex[:],
                              scalar1=sm[:, 5:6], scalar2=None, op0=ALU.mult)
                            yield
                            I("dve", "tensor_copy", [("aff", t), rowk], [rowk], out=row[:, D:D + 32].bitcast(F32),
                              in_=aff_all[:, t, :])
                            I("pool", "tensor_copy", ["tokid", rowk], [rowk], out=row[:, D + 32:D + 34].bitcast(I32),
                              in_=tokid[:, t:t + 1])
                            DMA("pool", [rowk], [("h2D", t)], out=h2D[t * 128:(t + 1) * 128, :], in_=row[:])
                            yield
                run_streams([p3_stream(k, 2) for k in range(2)])
            S.barrier()
            if stop_after == f"p3_{l}":
                if debug:
                    DMA("sp", [], ["dbg"], out=dbg[:, 0:NT * NE], in_=aff_all[:].rearrange("p t e -> p (t e)"))
                return finish(nc, S, top)

            groups = [(2, NT, CAP, 0)]
            if need_ctx:
                groups.append((0, 2, CAPC, NE * CAP))
            ng = len(groups)
            idxi = sb(LS, "idxi", [128, NE, NT], I32)
            with contextlib.ExitStack() as es:
                lo_t = sb(es, "lo_t", [128, 2, NE], F32)
                hi_t = sb(es, "hi_t", [128, 2, NE], F32)
                mid = sb(es, "mid", [128, 2, NE], F32)
                kk = sb(es, "kk", [128, 2, NE], F32)
                cmp = sb(es, "cmp", [128, NT, NE], F32)
                cntb = sb(es, "cntb", [128, 2, NE], BF)
                cntf = sb(es, "cntf", [128, 2, NE], F32)
                geu = sb(es, "geu", [128, 2, NE], U32)
                ltu = sb(es, "ltu", [128, 2, NE], U32)
                ptot_t = ps(es, "ptot", [128, 512], F32)
                ptot = ptot_t[:, 0:2 * NE]
                I("dve", "memset", [], ["lo_t"], ap=lo_t[:], constant=0.0)
                I("dve", "memset", [], ["hi_t"], ap=hi_t[:], constant=2.0)
                I("dve", "memset", [], ["kk"], ap=kk[:, 0, :], constant=float(CAP))
                I("dve", "memset", ["kk"], ["kk"], ap=kk[:, 1, :], constant=float(CAPC))
                I("dve", "memset", [], ["cntf"], ap=cntf[:], constant=0.0)

                def count_ge(thr, thrk):
                    for g, (tl, th, cap, rb) in enumerate(groups):
                        I("dve", "tensor_tensor", ["aff", thrk], ["cmp"], out=cmp[:, tl:th, :], in0=aff_all[:, tl:th, :],
                          in1=thr[:, g, :].unsqueeze(1).to_broadcast([128, th - tl, NE]), op=ALU.is_ge)
                        I("dve", "reduce_sum", ["cmp"], ["cntf"], out=cntf[:, g, :],
                          in_=cmp[:, tl:th, :].rearrange("p t e -> p e t"), axis=AX.X)
                    I("dve", "tensor_copy", ["cntf"], ["cntb"], out=cntb[:], in_=cntf[:])
                    I("pe", "matmul", ["cntb", "ones_b"], ["ptot"], out=ptot,
                      lhsT=ones_b[:], rhs=cntb[:].rearrange("p g e -> p (g e)"), start=True, stop=True)

                NIT = 34
                for it in range(NIT):
                    I("dve", "tensor_tensor", ["lo_t", "hi_t"], ["mid"], out=mid[:], in0=lo_t[:], in1=hi_t[:], op=ALU.add)
                    I("dve", "tensor_scalar", ["mid"], ["mid"], out=mid[:], in0=mid[:], scalar1=0.5, scalar2=None,
                      op0=ALU.mult)
                    count_ge(mid, "mid")
                    ptv = ptot.rearrange("p (g e) -> p g e", g=2)
                    I("dve", "tensor_tensor", ["ptot", "kk"], ["geu"], out=geu[:], in0=ptv, in1=kk[:], op=ALU.is_ge)
                    I("dve", "tensor_tensor", ["ptot", "kk"], ["ltu"], out=ltu[:], in0=ptv, in1=kk[:], op=ALU.is_lt)
                    I("dve", "copy_predicated", ["geu", "mid", "lo_t"], ["lo_t"], out=lo_t[:], mask=geu[:], data=mid[:])
                    I("dve", "copy_predicated", ["ltu", "mid", "hi_t"], ["hi_t"], out=hi_t[:], mask=ltu[:], data=mid[:])
                count_ge(lo_t, "lo_t")
                offp = sb(es, "offp", [128, 2, NE], F32)
                poff_t = ps(es, "poff", [128, 512], F32)
                poff = poff_t[:, 0:2 * NE]
                I("pe", "matmul", ["cntb", "ltri_b"], ["poff"], out=poff, lhsT=ltri_b[:],
                  rhs=cntb[:].rearrange("p g e -> p (g e)"), start=True, stop=True)
                I("act", "copy", ["poff"], ["offp"], out=offp[:].rearrange("p g e -> p (g e)"), in_=poff)
                Mt = sb(es, "Mt", [128, NE, NT], F32)
                cs_ = sb(es, "cs_", [128, NE, NT], F32)
                onesr = sb(es, "onesr", [128, NE * NT], F32)
                idxf = sb(es, "idxf", [128, NE, NT], F32)
                ebase = sb(es, "ebase", [128, 2, NE], F32)
                I("pool", "memset", [], ["onesr"], ap=onesr[:], constant=1.0)
                I("pool", "iota", [], ["ebase0"], out=idxi[:, :, 0], pattern=[[1, NE]], base=0, channel_multiplier=0)
                I("dve", "tensor_copy", ["ebase0"], ["ebase"], out=ebase[:, 0, :], in_=idxi[:, :, 0])
                I("dve", "tensor_scalar", ["ebase"], ["ebase"], out=ebase[:, 1, :], in0=ebase[:, 0, :], scalar1=float(CAPC),
                  scalar2=float(NE * CAP), op0=ALU.mult, op1=ALU.add)
                I("dve", "tensor_scalar", ["ebase"], ["ebase"], out=ebase[:, 0, :], in0=ebase[:, 0, :], scalar1=float(CAP),
                  scalar2=None, op0=ALU.mult)
                I("dve", "tensor_copy", ["cmp"], ["Mt"], out=Mt[:], in_=cmp[:].rearrange("p t e -> p e t"))
                for g, (tl, th, cap, rb) in enumerate(groups):
                    ntl_ = th - tl
                    for e in range(NE):
                        I("dve", "tensor_tensor_scan", ["Mt", "onesr", "zcol"], ["cs_"], out=cs_[:, e, tl:th],
                          data0=onesr[:, 0:ntl_], data1=Mt[:, e, tl:th], initial=zcol[:, 0:1], op0=ALU.mult, op1=ALU.add)
                    I("dve", "tensor_tensor", ["cs_", "Mt"], ["cs_"], out=cs_[:, :, tl:th], in0=cs_[:, :, tl:th],
                      in1=Mt[:, :, tl:th], op=ALU.subtract)
                    I("dve", "tensor_tensor", ["cs_", "offp"], ["cs_"], out=cs_[:, :, tl:th], in0=cs_[:, :, tl:th],
                      in1=offp[:, g, :].unsqueeze(2).to_broadcast([128, NE, ntl_]), op=ALU.add)
                    I("dve", "tensor_scalar", ["cs_"], ["idxf"], out=idxf[:, :, tl:th], in0=cs_[:, :, tl:th],
                      scalar1=float(cap), scalar2=None, op0=ALU.is_lt)
                    I("dve", "tensor_tensor", ["idxf", "Mt"], ["idxf"], out=idxf[:, :, tl:th], in0=idxf[:, :, tl:th],
                      in1=Mt[:, :, tl:th], op=ALU.mult)
                    I("dve", "tensor_tensor", ["cs_", "ebase"], ["cs_"], out=cs_[:, :, tl:th], in0=cs_[:, :, tl:th],
                      in1=ebase[:, g, :].unsqueeze(2).to_broadcast([128, NE, ntl_]), op=ALU.add)
                    I("dve", "tensor_scalar", ["cs_"], ["cs_"], out=cs_[:, :, tl:th], in0=cs_[:, :, tl:th],
                      scalar1=-BIGIDX, scalar2=None, op0=ALU.add)
                    I("dve", "tensor_tensor", ["idxf", "cs_"], ["idxf"], out=idxf[:, :, tl:th], in0=idxf[:, :, tl:th],
                      in1=cs_[:, :, tl:th], op=ALU.mult)
                    I("dve", "tensor_scalar", ["idxf"], ["idxf"], out=idxf[:, :, tl:th], in0=idxf[:, :, tl:th],
                      scalar1=BIGIDX, scalar2=None, op0=ALU.add)
                I("dve", "tensor_copy", ["idxf", "ebase0"], ["idxi"], out=idxi[:], in_=idxf[:])
                if debug:
                    DMA("sp", ["lo_t"], ["dbg"], out=dbg[:, 0:2 * NE], in_=lo_t[:].rearrange("p g e -> p (g e)"))
                    DMA("sp", ["idxf"], ["dbg"], out=dbg[:, 64:64 + NE * NT], in_=idxf[:].rearrange("p e t -> p (e t)"))
            S.barrier()
            if stop_after == f"p4c_{l}":
                return finish(nc, S, top)

            with contextlib.ExitStack() as es:
                mod5_bc = [sb(es, f"mod5_bc{s}", [128, D], F32) for s in range(ng)]
                with contextlib.ExitStack() as es2:
                    tmpD = sb(es2, "tmpD4", [128, 128], F32)
                    pbc = ps(es2, "pbc4", [128, 512], F32)
                    for s in range(ng):
                        make_bc(es, mod5_bc[s], f"mod5_bc{s}", lambda kc, s=s: modsl(5)[:, kc, s:s + 1], ("modT", l), tmpD,
                                "tmpD4", pbc, "pbc4")
                    S.barrier()
                wg_t = [sb(es, f"wg{i}", [128, 8, D], BF) for i in range(2)]
                wu_t = [sb(es, f"wu{i}", [128, 8, D], BF) for i in range(2)]
                wd_t = [sb(es, "wd0", [128, 8, D], BF)]
                xsb = sb(es, "xsb", [128, 8, ROWW], BF)
                xsc = sb(es, "xsc", [CAPC, ROWW], BF)
                meta_t = [sb(es, f"meta{i}", [128, 9, 40], BF) for i in range(2)]
                xsT_t = [sb(es, f"xsT{i}", [128, 8, CAP + CAPC], BF) for i in range(2)]
                actT = sb(es, "actT", [128, 8, CAP + CAPC], BF)
                sg_r = ring(es, "sg", 2, [128, 512], F32)
                yt_r = ring(es, "yt", 4, [128, D], F32)
                pxt_r = ring(es, "pxt", 2, [128, 8, 128], BF, psum=True)
                pg_r = ring(es, "pg", 2, [128, 512], F32, psum=True)
                pu_r = ring(es, "pu", 2, [128, 512], F32, psum=True)
                pyy_r = ring(es, "pyy", 2, [128, 512], F32, psum=True)
                rw_r = ring(es, "rw", 4, [128, ROWW], BF)
                prog = {"w_g": 0, "w_d": 0, "c_gu": 0, "c_dn": 0, "s_done": 0, "c_start": 0}
                NPASS = 4
                EPP = NE // NPASS

                def s_stream():
                    for g_ in range(NPASS):
                        while prog["c_start"] < EPP * (g_ - 1):
                            yield
                        for (tl, th, cap, rb) in groups:
                            for t in range(tl, th):
                                rw, rwk = rw_r.next()
                                DMA("sp", [], [rwk], out=rw[:], in_=h2D[t * 128:(t + 1) * 128, :])
                                for e in range(EPP * g_, EPP * (g_ + 1)):
                                    S.dma("pool", lambda en, rw=rw, e=e, t=t: en.indirect_dma_start(
                                        out=xs, out_offset=bass.IndirectOffsetOnAxis(ap=idxi[:, e, t:t + 1], axis=0),
                                        in_=rw[:], in_offset=None, bounds_check=preg(en, XS_ROWS - 1), oob_is_err=False),
                                        [rwk, "idxi"], [("xs", e, t)])
                                    yield
                        prog["s_done"] = EPP * (g_ + 1)


                def load_mat(m_, e, w, wk):
                    for h_ in range(2):
                        DMA("sp", [], [wk], out=w[:, 4 * h_:4 * h_ + 4, :],
                            in_=wbf[m_, e, 512 * h_:512 * (h_ + 1), :].rearrange("(k p) n -> p k n", p=128))
                        yield

                def w_stream():
                    for e in range(NE):
                        while prog["c_gu"] < e - 1:
                            yield
                        yield from load_mat(0, e, wg_t[e % 2], ("wg", e % 2))
                        yield from load_mat(1, e, wu_t[e % 2], ("wu", e % 2))
                        prog["w_g"] = e + 1
                        while prog["c_dn"] < e:
                            yield
                        yield from load_mat(2, e, wd_t[0], ("wd", 0))
                        prog["w_d"] = e + 1

                def load_x(e):
                    DMA("sp", [("xs", e, t) for t in range(2, NT)], ["xsb"], out=xsb[:],
                        in_=xs[e * CAP:(e + 1) * CAP, :].rearrange("(t p) w -> p t w", p=128))
                    if need_ctx:
                        DMA("sp", [("xs", e, t) for t in range(0, 2)], ["xsc"], out=xsc[:],
                            in_=xs[NE * CAP + e * CAPC:NE * CAP + (e + 1) * CAPC, :])

                def transposes(e):
                    xsT = xsT_t[e % 2]
                    xk = ("xsT", e % 2)
                    meta = meta_t[e % 2]
                    mk_ = ("meta", e % 2)
                    I("dve", "tensor_copy", ["xsb"], [mk_], out=meta[:, 0:8, :], in_=xsb[:, :, D:D + 40])
                    if need_ctx:
                        I("dve", "tensor_copy", ["xsc", mk_], [mk_], out=meta[0:CAPC, 8, :], in_=xsc[:, D:D + 40])
                    for st in range(8):
                        pxt, pxtk = pxt_r.next()
                        for kc in range(8):
                            I("pe", "transpose", ["xsb", "ident_b"], [pxtk], out=pxt[:, kc, :],
                              in_=xsb[:, st, kc * 128:(kc + 1) * 128], identity=ident_b[:])
                        if st % 2 == 0:
                            I("act", "copy", [pxtk], [xk], out=xsT[:, :, st * 128:(st + 1) * 128], in_=pxt[:])
                        else:
                            I("dve", "tensor_copy", [pxtk], [xk], out=xsT[:, :, st * 128:(st + 1) * 128], in_=pxt[:])
                        yield
                    if need_ctx:
                        pxt, pxtk = pxt_r.next()
                        for kc in range(8):
                            I("pe", "transpose", ["xsc", "ident_b"], [pxtk], out=pxt[:, kc, 0:CAPC],
                              in_=xsc[:, kc * 128:(kc + 1) * 128], identity=ident_b[0:CAPC, 0:CAPC])
                        I("act", "copy", [pxtk], [xk], out=xsT[:, :, CAP:CAP + CAPC], in_=pxt[:, :, 0:CAPC])
                        yield

                def c_stream():
                    while prog["s_done"] <= 0:
                        yield
                    load_x(0)
                    yield from transposes(0)
                    slabs = [(0, 512), (512, 512)] + ([(CAP, CAPC)] if need_ctx else [])
                    for e in range(NE):
                        xsT = xsT_t[e % 2]
                        xk = ("xsT", e % 2)
                        meta = meta_t[e % 2]
                        mk_ = ("meta", e % 2)
                        prog["c_start"] = e + 1
                        if e + 1 < NE:
                            while prog["s_done"] <= e + 1:
                                yield
                            load_x(e + 1)
                        while prog["w_g"] <= e:
                            yield
                        wg, wgk = wg_t[e % 2], ("wg", e % 2)
                        wu, wuk = wu_t[e % 2], ("wu", e % 2)
                        wd, wdk = wd_t[0], ("wd", 0)
                        for fc in range(8):
                            for (c0, w_) in slabs:
                                pg, pgk = pg_r.next()
                                pu, puk = pu_r.next()
                                for kc in range(8):
                                    I("pe", "matmul", [xk, wgk], [pgk], out=pg[:, 0:w_], lhsT=wg[:, kc, fc * 128:(fc + 1) * 128],
                                      rhs=xsT[:, kc, c0:c0 + w_], start=(kc == 0), stop=(kc == 7))
                                for kc in range(8):
                                    I("pe", "matmul", [xk, wuk], [puk], out=pu[:, 0:w_], lhsT=wu[:, kc, fc * 128:(fc + 1) * 128],
                                      rhs=xsT[:, kc, c0:c0 + w_], start=(kc == 0), stop=(kc == 7))
                                sg, sgk = sg_r.next()
                                I("act", "activation", [pgk], [sgk], out=sg[:, 0:w_], in_=pg[:, 0:w_], func=AF.Silu)
                                I("dve", "tensor_tensor", [sgk, puk], [("actT", fc)], out=actT[:, fc, c0:c0 + w_],
                                  in0=sg[:, 0:w_], in1=pu[:, 0:w_], op=ALU.mult)
                                yield
                        prog["c_gu"] = e + 1
                        if e + 1 < NE:
                            yield from transposes(e + 1)
                        while prog["w_d"] <= e:
                            yield
                        tiles = [(st * 128, 128, st, 0) for st in range(8)]
                        if need_ctx:
                            tiles.append((CAP, CAPC, 8, 1))
                        for (c0, m_, mi, g) in tiles:
                            yt, ytk = yt_r.next()
                            gate_ap = meta[0:m_, mi, 2 * e:2 * e + 2].bitcast(F32)
                            for nh in range(2):
                                cs = slice(nh * 512, (nh + 1) * 512)
                                pyy, pyk = pyy_r.next()
                                for fc in range(8):
                                    I("pe", "matmul", [("actT", fc), wdk], [pyk], out=pyy[0:m_, :],
                                      lhsT=actT[:, fc, c0:c0 + m_], rhs=wd[:, fc, cs], start=(fc == 0), stop=(fc == 7))
                                I("dve", "scalar_tensor_tensor", [pyk, mk_, f"mod5_bc{g}"], [ytk], out=yt[0:m_, cs],
                                  in0=pyy[0:m_, :], scalar=gate_ap, in1=mod5_bc[g][0:m_, cs], op0=ALU.mult, op1=ALU.mult)
                                yield
                            idx_ap = meta[0:m_, mi, 32:34].bitcast(I32)
                            S.dma("pool", lambda en, yt=yt, m_=m_, idx_ap=idx_ap: en.indirect_dma_start(
                                out=xa, out_offset=bass.IndirectOffsetOnAxis(ap=idx_ap, axis=0), in_=yt[0:m_, :],
                                in_offset=None, bounds_check=preg(en, NTOK - 1), oob_is_err=True, compute_op=ALU.add),
                                [ytk, mk_] + [("xa_p", (e - 1) % 2, i_) for i_ in range(9)], [("xa_p", e % 2, mi)])
                            yield
                        prog["c_dn"] = e + 1
                run_streams([s_stream(), w_stream(), c_stream()])
            S.barrier()
            if stop_after == f"p4_{l}":
                return finish(nc, S, top)

    with contextlib.ExitStack() as es:
        fg_bc = sb(es, "fg_bc", [128, D], F32)
        DMA("sp", [], ["fg_bc"], out=fg_bc[:], in_=fg_in.to_broadcast([128, D]))

        def p5_stream(k, nk):
            P = f"p5{k}_"
            xt_r = ring(es, P + "xt", 2, [128, D], F32)
            ot_r = ring(es, P + "ot", 2, [128, D], F32)
            sm_r = ring(es, P + "sm", 2, [128, 4], F32)
            junk = sb(es, P + "junk", [128, D], BF)
            for t in range(2 + k, NT, nk):
                xt, xtk = xt_r.next()
                DMA("sp", [], [xtk], out=xt[:], in_=xa[t * 128:(t + 1) * 128, :])
                sm, smk = sm_r.next()
                I("pool", "memset", [], [smk], ap=sm[:, 0:1], constant=0.0)
                yield
                I("act", "activation", [xtk, smk], [P + "junk", smk], out=junk[:], in_=xt[:], func=AF.Square,
                  accum_out=sm[:, 0:1])
                yield
                I("dve", "tensor_scalar", [smk], [smk], out=sm[:, 1:2], in0=sm[:, 0:1], scalar1=1.0 / D, scalar2=EPS,
                  op0=ALU.mult, op1=ALU.add)
                yield
                I("act", "activation", [smk], [smk], out=sm[:, 1:2], in_=sm[:, 1:2], func=AF.Sqrt)
                yield
                I("dve", "reciprocal", [smk], [smk], out=sm[:, 2:3], in_=sm[:, 1:2])
                yield
                ot, otk = ot_r.next()
                I("dve", "scalar_tensor_tensor", [xtk, smk, "fg_bc"], [otk], out=ot[:], in0=xt[:], scalar=sm[:, 2:3],
                  in1=fg_bc[:], op0=ALU.mult, op1=ALU.mult)
                DMA("pool", [otk], [("out", t)], out=out[(t - 2) * 128:(t - 1) * 128, :], in_=ot[:])
                yield
        run_streams([p5_stream(k, 3) for k in range(3)])
    return finish(nc, S, top, True)


def finish(nc, S, top, close_top=False):
    S.barrier()
    S.emit()
    S.close()
    if close_top:
        top.close()
    return nc


Q_PERM = np.concatenate([np.arange(64) + 64 * (j + 4 * half) for j in range(4) for half in range(2)])


def rope_table():
    rows = SEQ // 64
    row = np.repeat(np.arange(rows, dtype=np.float32), 64)
    col = np.tile(np.arange(64, dtype=np.float32), rows)
    n_freq = 16
    inv = (np.float32(10000.0) ** (-np.arange(n_freq, dtype=np.float32) / np.float32(n_freq))).astype(np.float32)
    ang = np.concatenate([row[:, None] * inv, col[:, None] * inv], axis=-1).astype(np.float32)
    return np.concatenate([np.cos(ang), np.sin(ang)], axis=-1).astype(np.float32)


def fm(v):
    v = np.asarray(v)
    return np.ascontiguousarray(np.swapaxes(v.reshape(v.shape[:-1] + (v.shape[-1] // 128, 128)), -1, -2))


def prep_inputs(inp):
    f = lambda a: np.ascontiguousarray(np.asarray(a, dtype=np.float32))
    shared = {}
    shared["w_mod"] = f(inp["w_mod"])
    shared["b_modT"] = f(fm(inp["b_mod"]))
    shared["g1T"] = f(fm(inp["norm1_g"]))
    shared["g2T"] = f(fm(inp["norm2_g"]))
    w_in = np.asarray(inp["w_in"], dtype=np.float32).copy()
    w_in[:, :, 0:512] = w_in[:, :, Q_PERM]
    shared["w_in"] = f(w_in)
    shared["qkg"] = f(np.stack([inp["q_norm_g"], inp["k_norm_g"]], axis=1))
    cw = np.asarray(inp["conv_w"], dtype=np.float32)
    shared["convw"] = f(cw.reshape(2, 3, 2, 128).transpose(0, 3, 2, 1))
    lw = np.asarray(inp["lru_conv_w"], dtype=np.float32)
    lb = np.asarray(inp["lru_conv_b"], dtype=np.float32)
    lcw = np.concatenate([lw, lb[:, None, :]], axis=1)
    shared["lcw"] = f(lcw.reshape(2, 5, 2, 128).transpose(0, 3, 2, 1))
    lv = np.stack([inp["lru_ba"], inp["lru_bi"], inp["lru_lam"]], axis=-1)
    shared["lvec"] = f(np.asarray(lv, dtype=np.float32).reshape(2, 2, 2, 128, 3).transpose(0, 3, 1, 2, 4))
    shared["lwa"] = f(inp["lru_wa"])
    shared["lwi"] = f(inp["lru_wi"])
    w_out = np.asarray(inp["w_out"], dtype=np.float32).copy()
    w_out[:, 0:512, :] = w_out[:, Q_PERM, :]
    shared["w_out"] = f(w_out)
    shared["w_r"] = f(inp["w_router"])
    shared["w_gate"] = f(inp["w_gate"])
    shared["w_up"] = f(inp["w_up"])
    shared["w_down"] = f(inp["w_down"])
    shared["fg"] = f(np.asarray(inp["final_g"]).reshape(1, D))
    shared["rope"] = rope_table()
    cst = np.zeros((128, 3, 128), np.float32)
    cst[:, 0, :] = np.eye(128, dtype=np.float32)
    cst[:, 1, :] = np.triu(np.ones((128, 128), np.float32), 1)
    cst[:, 2, :] = 1.0
    shared["cst"] = cst
    x = np.asarray(inp["x"], dtype=np.float32)
    ctx = np.asarray(inp["ctx"], dtype=np.float32)
    c = np.asarray(inp["c"], dtype=np.float32)
    cc = fm(np.asarray(inp["c_ctx"], dtype=np.float32))
    maps = []
    for b in range(x.shape[0]):
        m = dict(shared)
        m["x"] = np.ascontiguousarray(x[b])
        m["ctx"] = np.ascontiguousarray(ctx[b])
        m["cT"] = f(np.stack([fm(c[b]), cc], axis=-1))
        maps.append(m)
    return maps


_NC_CACHE = {}
DBG = {"units": None, "skip": set()}


def kernel(**inputs):
    maps = prep_inputs(inputs)
    if "nc" not in _NC_CACHE:
        _NC_CACHE["nc"] = build()
    nc = _NC_CACHE["nc"]
    res = run_bass_kernel_spmd(nc, maps, core_ids=list(range(8)))
    return np.stack([np.asarray(r["out"]) for r in res.results], axis=0).astype(np.float32)
```

```python
import contextlib
import numpy as np
import concourse.bass as bass
import concourse.mybir as mybir
from concourse.bass_utils import run_bass_kernel_spmd

F32 = mybir.dt.float32
BF = mybir.dt.bfloat16
I32 = mybir.dt.int32
U32 = mybir.dt.uint32
ALU = mybir.AluOpType
AF = mybir.ActivationFunctionType
AX = mybir.AxisListType

D = 1024
SEQ = 8192
CTX = 256
NTOK = SEQ + CTX
NT = NTOK // 128
NE = 16
CAP = 1024
CAPC = 32
EPS = 1e-6
ROWW = 1064
XS_ROWS = NE * CAP + NE * CAPC
BIGIDX = 1.0e6
ENGS = ("pe", "act", "dve", "pool", "sp")


class Sched:
    def __init__(self, nc, nd=None):
        self.nc = nc
        self.q = {e: [] for e in ENGS}
        self.nd = nd or {"sp": 16, "pool": 16, "act": 4}
        self._stack = []
        self.esem = {}
        self.cnt = {}
        self.dsem = {}
        self.dcnt = {}
        self.dnext = {}
        self.waited = {e: {} for e in ENGS}
        self.last_w = {}
        self.readers = {}
        self.nsem = 0
        self.n_instr = 0
        self.excl = set()

    def _new_sem(self, name):
        cm = self.nc.semaphore(name)
        s = cm.__enter__()
        self._stack.append(cm)
        self.nsem += 1
        return s

    def start(self):
        for e in ENGS:
            self.esem[e] = (self._new_sem(f"es_{e}_0"), 0)
            self.cnt[e] = 0
        for e, n in self.nd.items():
            self.dsem[e] = [self._new_sem(f"ds_{e}_{i}") for i in range(n)]
            self.dcnt[e] = [0] * n
            self.dnext[e] = 0

    def close(self):
        for cm in reversed(self._stack):
            cm.__exit__(None, None, None)
        self._stack = []

    def _need(self, eng, tok):
        kind = tok[0]
        if kind == "e":
            a, gen, n, sem = tok[1], tok[2], tok[3], tok[4]
            if a == eng and eng == "pe":
                return
            key = ("e", a, gen)
        else:
            _, a, i, n = tok
            key = ("d", a, i)
            sem = self.dsem[a][i]
        if self.waited[eng].get(key, 0) >= n:
            return
        self.waited[eng][key] = n
        self.q[eng].append(("wait", sem, n))

    def _deps(self, eng, reads, writes):
        for r in reads:
            t = self.last_w.get(r)
            if t is not None:
                self._need(eng, t)
        for w in writes:
            t = self.last_w.get(w)
            if t is not None:
                self._need(eng, t)
            for t in self.readers.get(w, ()):
                self._need(eng, t)

    def _record(self, tok, reads, writes):
        for r in reads:
            self.readers.setdefault(r, []).append(tok)
        for w in writes:
            self.last_w[w] = tok
            self.readers[w] = []

    def op(self, eng, fn, reads=(), writes=()):
        if self.excl:
            ex = [r for r in reads if r in self.excl]
            if ex:
                reads = [r for r in reads if r not in self.excl]
                writes = list(writes) + ex
        self._deps(eng, reads, writes)
        self.cnt[eng] += 1
        sem, gen = self.esem[eng]
        self.q[eng].append(("op", fn, sem))
        tok = ("e", eng, gen, self.cnt[eng], sem)
        self._record(tok, reads, writes)
        self.n_instr += 1
        return tok

    def dma(self, eng, fn, reads=(), writes=()):
        idx = self.dnext[eng]
        self.dnext[eng] = (idx + 1) % self.nd[eng]
        if self.dcnt[eng][idx] > 0:
            self._need(eng, ("d", eng, idx, self.dcnt[eng][idx] * 16))
        self._deps(eng, reads, writes)
        self.dcnt[eng][idx] += 1
        tok = ("d", eng, idx, self.dcnt[eng][idx] * 16)
        self.q[eng].append(("dma", fn, self.dsem[eng][idx]))
        self._record(tok, reads, writes)
        self.n_instr += 1
        return tok

    def barrier(self):
        for e in ENGS:
            for e2 in ENGS:
                if e2 != e and self.cnt[e2] > 0:
                    sem, gen = self.esem[e2]
                    self._need(e, ("e", e2, gen, self.cnt[e2], sem))
            for e2 in self.nd:
                for i in range(self.nd[e2]):
                    if self.dcnt[e2][i] > 0:
                        self._need(e, ("d", e2, i, self.dcnt[e2][i] * 16))
        self.last_w = {}
        self.readers = {}
        for e in ENGS:
            if self.cnt[e] > 20000:
                gen = self.esem[e][1] + 1
                self.esem[e] = (self._new_sem(f"es_{e}_{gen}"), gen)
                self.cnt[e] = 0

    def emit(self):
        with self.nc.Block() as block:
            def play(engname):
                def body(engine):
                    for item in self.q[engname]:
                        if item[0] == "wait":
                            engine.wait_ge(item[1], item[2])
                        elif item[0] == "op":
                            item[1](engine).then_inc(item[2], 1)
                        else:
                            item[1](engine).then_inc(item[2], 16)
                return body
            block.tensor(play("pe"))
            block.scalar(play("act"))
            block.vector(play("dve"))
            block.gpsimd(play("pool"))
            block.sync(play("sp"))


def run_streams(gens):
    gens = list(gens)
    while gens:
        for g in list(gens):
            try:
                next(g)
            except StopIteration:
                gens.remove(g)


def paced(g, k):
    for _ in g:
        for _ in range(k - 1):
            yield
        yield


class Ring:
    def __init__(self, tiles, name):
        self.tiles = tiles
        self.name = name
        self.i = -1

    def next(self):
        self.i = (self.i + 1) % len(self.tiles)
        return self.tiles[self.i], (self.name, self.i)


def build(stop_after=None, debug=False, nlayers=2):
    nc = bass.Bass("TRN2", target_bir_lowering=False)
    S = Sched(nc)

    def din(name, shape, dt=F32):
        return nc.dram_tensor(name, list(shape), dt, kind="ExternalInput").ap()

    def dscratch(name, shape, dt):
        kind = "ExternalOutput" if debug else "Internal"
        return nc.dram_tensor(name, list(shape), dt, kind=kind).ap()

    x_in = din("x", [SEQ, D])
    ctx_in = din("ctx", [CTX, D])
    cT_in = din("cT", [128, 8, 2])
    wmod_in = din("w_mod", [2, D, 6 * D])
    bmodT_in = din("b_modT", [2, 128, 48])
    g1T_in = din("g1T", [2, 128, 8])
    g2T_in = din("g2T", [2, 128, 8])
    win_in = din("w_in", [2, D, 2048])
    qkg_in = din("qkg", [2, 2, 64])
    convw_in = din("convw", [2, 128, 2, 3])
    lcw_in = din("lcw", [2, 128, 2, 5])
    lvec_in = din("lvec", [2, 128, 2, 2, 3])
    lwa_in = din("lwa", [2, 2, 4, 64, 64])
    lwi_in = din("lwi", [2, 2, 4, 64, 64])
    wout_in = din("w_out", [2, D, D])
    wr_in = din("w_r", [2, D, NE])
    ew = [2, NE, D, D] if not DBG.get("small_w") else [2, NE, 8, 8]
    wg_in = din("w_gate", ew)
    wu_in = din("w_up", ew)
    wd_in = din("w_down", ew)
    fg_in = din("fg", [1, D])
    rope_in = din("rope", [SEQ, 64])
    cst_in = din("cst", [128, 3, 128])
    out = nc.dram_tensor("out", [SEQ, D], F32, kind="ExternalOutput").ap()

    xa = dscratch("xa", [NTOK, D], F32)
    qT = dscratch("qT", [4, 128, NTOK], BF)
    featT = dscratch("featT", [10, 128, NTOK], F32)
    mixT = dscratch("mixT", [8, 128, NTOK], BF)
    h2D = dscratch("h2D", [NTOK, ROWW], BF)
    xs = dscratch("xs", [XS_ROWS, ROWW], BF)
    hD = dscratch("hD", [2, 2, 128, NTOK], F32)
    wbf = nc.dram_tensor("wbf", [3, NE, D, D], BF, kind="Internal").ap() if not DBG.get("small_w") else None
    woutbf = nc.dram_tensor("woutbf", [D, D], BF, kind="Internal").ap()
    winbf = nc.dram_tensor("winbf", [D, 2048], BF, kind="Internal").ap()
    dbg = dscratch("dbg", [128, 4096], F32) if debug else None

    top = contextlib.ExitStack()

    uniq = [0]

    def sb(es, name, shape, dt):
        uniq[0] += 1
        return es.enter_context(nc.sbuf_tensor(f"s{uniq[0]}_{name}", list(shape), dt))

    def ps(es, name, shape, dt, key=None):
        nbytes = int(np.prod(shape[1:])) * (4 if dt in (F32, I32, U32) else 2)
        assert nbytes in (2048, 4096), (name, shape, nbytes)
        S.excl.add(key if key is not None else name)
        uniq[0] += 1
        return es.enter_context(nc.psum_tensor(f"p{uniq[0]}_{name}", list(shape), dt))

    def ring(es, name, n, shape, dt, psum=False):
        if psum:
            return Ring([ps(es, f"{name}{i}", shape, dt, key=(name, i)) for i in range(n)], name)
        return Ring([sb(es, f"{name}{i}", shape, dt) for i in range(n)], name)

    pool_regs = {}

    def preg(en, val):
        if val not in pool_regs:
            pool_regs[val] = en.to_reg(val)
        return pool_regs[val]

    def I(eng, name, reads, writes, **kw):
        return S.op(eng, lambda e, kw=kw, name=name: getattr(e, name)(**kw), reads, writes)

    def DMA(eng, reads, writes, **kw):
        return S.dma(eng, lambda e, kw=kw: e.dma_start(**kw), reads, writes)

    S.start()

    cst_f = sb(top, "cst_f", [128, 3, 128], F32)
    ident_b = sb(top, "ident_b", [128, 128], BF)
    ltri_b = sb(top, "ltri_b", [128, 128], BF)
    ones_b = sb(top, "ones_b", [128, 128], BF)
    modT = sb(top, "modT", [128, 2, 48, 2], F32)
    aff_all = sb(top, "aff_all", [128, NT, NE], F32)
    tokid = sb(top, "tokid", [128, NT], I32)
    zcol = sb(top, "zcol", [128, 1], F32)
    ident_f = cst_f[:, 0, :]
    ltri_f = cst_f[:, 1, :]
    ones_f = cst_f[:, 2, :]

    DMA("sp", [], ["cst_f"], out=cst_f[:], in_=cst_in)
    I("dve", "tensor_copy", ["cst_f"], ["ident_b"], out=ident_b[:], in_=ident_f)
    I("dve", "tensor_copy", ["cst_f"], ["ltri_b"], out=ltri_b[:], in_=ltri_f)
    I("dve", "tensor_copy", ["cst_f"], ["ones_b"], out=ones_b[:], in_=ones_f)
    I("pool", "iota", [], ["tokid"], out=tokid[:], pattern=[[128, NT]], base=0, channel_multiplier=1)
    I("pool", "memset", [], ["zcol"], ap=zcol[:], constant=0.0)

    def rstd_chain(ss, ssk, tmp, tmpk, rs, rsk, n, inv_n):
        I("dve", "tensor_scalar", [ssk], [tmpk], out=tmp, in0=ss, scalar1=inv_n, scalar2=EPS,
          op0=ALU.mult, op1=ALU.add)
        I("act", "activation", [tmpk], [tmpk], out=tmp, in_=tmp, func=AF.Sqrt)
        I("dve", "reciprocal", [tmpk], [rsk], out=rs, in_=tmp)

    with contextlib.ExitStack() as es:
        cT_sb = sb(es, "cT_sb", [128, 8, 2], F32)
        sc = sb(es, "sc", [128, 8, 2], F32)
        bT = sb(es, "bT", [128, 2, 48], F32)
        wm = ring(es, "wm", 2, [128, 6 * D], F32)
        pm = ring(es, "pm", 2, [128, 512], F32, psum=True)
        DMA("sp", [], ["cT_sb"], out=cT_sb[:], in_=cT_in)
        DMA("sp", [], ["bT"], out=bT[:], in_=bmodT_in.rearrange("l p n -> p l n"))
        I("act", "activation", ["cT_sb"], ["sc"], out=sc[:], in_=cT_sb[:], func=AF.Silu)
        for l in range(nlayers):
            acc = modT[:, l].rearrange("p n s -> p (n s)")
            for kc in range(8):
                w, wk = wm.next()
                DMA("sp", [], [wk], out=w[:], in_=wmod_in[l, kc * 128:(kc + 1) * 128, :])
                p, pk = pm.next()
                for n in range(48):
                    I("pe", "matmul", [wk, "sc"], [pk], out=p[:, 2 * n:2 * n + 2],
                      lhsT=w[:, n * 128:(n + 1) * 128], rhs=sc[:, kc, :], start=True, stop=True)
                if kc == 0:
                    I("dve", "tensor_copy", [pk], [("modT", l)], out=acc, in_=p[:, 0:96])
                else:
                    I("dve", "tensor_tensor", [pk, ("modT", l)], [("modT", l)], out=acc, in0=acc, in1=p[:, 0:96],
                      op=ALU.add)
            I("dve", "tensor_tensor", ["bT", ("modT", l)], [("modT", l)], out=modT[:, l], in0=modT[:, l],
              in1=bT[:, l, :].unsqueeze(2).to_broadcast([128, 48, 2]), op=ALU.add)
        if debug:
            DMA("sp", [("modT", 0), ("modT", 1)], ["dbg"], out=dbg[:, 0:192],
                in_=modT[:].rearrange("p l n s -> p (l n s)"))
    S.barrier()
    if stop_after == "p0":
        return finish(nc, S, top)

    def src_rows(l, r0, n):
        if l == 0:
            if r0 < CTX:
                return ctx_in[r0:r0 + n, :]
            return x_in[r0 - CTX:r0 - CTX + n, :]
        return xa[r0:r0 + n, :]

    def make_bc(es_unused, dst, dstk, src_col, srck, tmpD, tmpDk, pbc, pbck, eng_toggle=[0]):
        for h in range(2):
            for j in range(4):
                kc = h * 4 + j
                I("dve", "tensor_scalar", ["cst_f", srck], [tmpDk], out=tmpD[:], in0=ident_f,
                  scalar1=src_col(kc), scalar2=None, op0=ALU.mult)
                I("pe", "matmul", [tmpDk, "cst_f"], [pbck], out=pbc[:, j * 128:(j + 1) * 128], lhsT=ones_f,
                  rhs=tmpD[:], start=True, stop=True)
            I("act", "copy", [pbck], [dstk], out=dst[:, h * 512:(h + 1) * 512], in_=pbc[:])

    units = [(0, 2, True)] + [(CTX + 512 * i, 4, False) for i in range(16)]

    for l in range(nlayers):
        need_ctx = l < nlayers - 1 or (nlayers == 1 and debug)
        last_layer = (l == 1)
        with contextlib.ExitStack() as LS:
            G1T = sb(LS, "G1T", [128, 8, 2], F32)
            G2T = sb(LS, "G2T", [128, 8, 2], F32)
            gT = sb(LS, "gT", [128, 2, 8], F32)
            DMA("sp", [], ["gT"], out=gT[:, 0, :], in_=g1T_in[l])
            DMA("sp", [], ["gT"], out=gT[:, 1, :], in_=g2T_in[l])

            def modsl(i):
                return modT[:, l, i * 8:(i + 1) * 8, :]
            I("dve", "tensor_scalar", [], ["G1T"], out=G1T[:], in0=modsl(1), scalar1=1.0, scalar2=None, op0=ALU.add)
            I("dve", "tensor_tensor", ["G1T", "gT"], ["G1T"], out=G1T[:], in0=G1T[:],
              in1=gT[:, 0, :].unsqueeze(2).to_broadcast([128, 8, 2]), op=ALU.mult)
            I("dve", "tensor_scalar", [], ["G2T"], out=G2T[:], in0=modsl(4), scalar1=1.0, scalar2=None, op0=ALU.add)
            I("dve", "tensor_tensor", ["G2T", "gT"], ["G2T"], out=G2T[:], in0=G2T[:],
              in1=gT[:, 1, :].unsqueeze(2).to_broadcast([128, 8, 2]), op=ALU.mult)
            sh1T = modsl(0)
            sh2T = modsl(3)

            with contextlib.ExitStack() as KV:
                KTz = [sb(KV, f"KTz{i}", [128, NTOK], BF) for i in range(2)]
                Vp = sb(KV, "Vp", [128, NT, 2, 128], BF)
                if "memsets" not in DBG["skip"]:
                    I("pool", "memset", [], ["KTz0z"], ap=KTz[0][64:128, :], constant=0.0)
                    I("pool", "memset", [], ["KTz1z"], ap=KTz[1][0:64, :], constant=0.0)
                    I("pool", "memset", [], ["Vp1"], ap=Vp[:, :, 0, 64:128], constant=1.0)
                    I("pool", "memset", [], ["Vp1"], ap=Vp[:, :, 1, 0:64], constant=1.0)

                with contextlib.ExitStack() as es:
                    win = sb(es, "win", [128, 8, 2048], BF)
                    if l == 0:
                        for kc in range(8):
                            for hh in range(2):
                                DMA("pool", [], ["win"], out=win[:, kc, hh * 1024:(hh + 1) * 1024],
                                    in_=win_in[l, kc * 128:(kc + 1) * 128, hh * 1024:(hh + 1) * 1024])
                    else:
                        for h_ in range(2):
                            DMA("sp", [], ["win"], out=win[:, 4 * h_:4 * h_ + 4, :],
                                in_=winbf[512 * h_:512 * (h_ + 1), :].rearrange("(k p) n -> p k n", p=128))
                    gqk = sb(es, "gqk", [128, 2, 64], F32)
                    if "gqk" not in DBG["skip"]:
                        DMA("sp", [], ["gqk"], out=gqk[:].rearrange("p a b -> p (a b)"),
                            in_=qkg_in[l:l + 1].rearrange("o a b -> o (a b)").to_broadcast([128, 128]))
                    I("dve", "tensor_scalar", ["gqk"], ["gqk"], out=gqk[:, 0, :], in0=gqk[:, 0, :], scalar1=0.125,
                      scalar2=None, op0=ALU.mult)
                    xt_r = ring(es, "xt", 3, [128, D], F32)
                    xn_r = ring(es, "xn", 2, [128, D], BF)
                    junk = sb(es, "junk", [128, D], BF)
                    sm_r = ring(es, "sm", 3, [128, 4], F32)
                    NHX = 3
                    hx_t = [sb(es, f"hxT{i}", [128, 8, 512], BF) for i in range(NHX)]
                    qTs_t = [sb(es, f"qTs{i}", [128, 4, 512], BF) for i in range(2)]
                    fs_r = ring(es, "fs", 3, [128, 512], F32)
                    pT_r = ring(es, "pT", 1, [128, 8, 128], BF, psum=True)
                    pf_r = ring(es, "pf", 1, [128, 512], F32, psum=True)
                    gq_bc = gqk[:, 0, :].unsqueeze(1).to_broadcast([128, 8, 64])
                    gk_bc = gqk[:, 1, :].unsqueeze(1).to_broadcast([128, 2, 64])
                    ulist = units if DBG["units"] is None else units[:DBG["units"]]
                    prog1 = {"a": 0, "b1": 0, "b2": 0, "b1n": {}}

                    def a_stream():
                        for ui, (tok0, ntl, is_ctx) in enumerate(ulist):
                            while min(prog1["b1"], prog1["b2"]) < ui - (NHX - 1):
                                yield
                            s = 1 if is_ctx else 0
                            hx, hxk = hx_t[ui % NHX], ("hx", ui % NHX)
                            for ti in range(ntl):
                                t = tok0 // 128 + ti
                                xt, xtk = xt_r.next()
                                DMA("sp", [], [xtk], out=xt[:], in_=src_rows(l, tok0 + ti * 128, 128))
                                sm, smk = sm_r.next()
                                I("pool", "memset", [], [smk], ap=sm[:, 0:1], constant=0.0)
                                yield
                                I("act", "activation", [xtk, smk], ["junk", smk], out=junk[:], in_=xt[:], func=AF.Square,
                                  accum_out=sm[:, 0:1])
                                yield
                                I("dve", "tensor_scalar", [smk], [smk], out=sm[:, 1:2], in0=sm[:, 0:1], scalar1=1.0 / D,
                                  scalar2=EPS, op0=ALU.mult, op1=ALU.add)
                                yield
                                I("act", "activation", [smk], [smk], out=sm[:, 1:2], in_=sm[:, 1:2], func=AF.Sqrt)
                                yield
                                I("dve", "reciprocal", [smk], [smk], out=sm[:, 2:3], in_=sm[:, 1:2])
                                yield
                                xn, xnk = xn_r.next()
                                I("dve", "tensor_scalar", [xtk, smk], [xnk], out=xn[:], in0=xt[:], scalar1=sm[:, 2:3],
                                  scalar2=None, op0=ALU.mult)
                                yield
                                pT, pTk = pT_r.next()
                                for kc in range(8):
                                    I("pe", "transpose", [xnk, "ident_b"], [pTk], out=pT[:, kc, :],
                                      in_=xn[:, kc * 128:(kc + 1) * 128], identity=ident_b[:])
                                yield
                                for kc in range(8):
                                    dst = hx[:, kc, ti * 128:(ti + 1) * 128]
                                    if t % 2 == 0:
                                        I("act", "activation", [pTk, "G1T"], [hxk], out=dst, in_=pT[:, kc, :],
                                          func=AF.Identity, scale=G1T[:, kc, s:s + 1], bias=sh1T[:, kc, s:s + 1])
                                    else:
                                        I("dve", "tensor_scalar", [pTk, "G1T"], [hxk], out=dst, in0=pT[:, kc, :],
                                          scalar1=G1T[:, kc, s:s + 1], scalar2=sh1T[:, kc, s:s + 1],
                                          op0=ALU.mult, op1=ALU.add)
                                    if kc % 2 == 1:
                                        yield
                            prog1["a"] = ui + 1

                    def b1_stream(par):
                        P = f"b1{par}_"
                        pq = ps(es, P + "pq", [128, 512], F32)
                        pkv = ps(es, P + "pkv", [128, 512], F32)
                        pqT = ps(es, P + "pqT", [128, 8, 128], BF)
                        pqk, pkvk, pqTk = P + "pq", P + "pkv", P + "pqT"
                        sq_r = ring(es, P + "sq", 1, [128, 640], F32)
                        qs_r = ring(es, P + "qs", 1, [128, 32], F32)
                        qn_r = ring(es, P + "qn", 1, [128, 10, 64], F32)
                        ra_r = ring(es, P + "ra", 1, [128, 10, 32], F32)
                        rb_r = ring(es, P + "rb", 1, [128, 10, 32], F32)
                        qr_r = ring(es, P + "qr", 1, [128, 10, 64], BF)
                        rope_r = ring(es, P + "rope", 1, [128, 64], F32)
                        for ui, (tok0, ntl, is_ctx) in enumerate(ulist):
                            while prog1["a"] <= ui:
                                yield
                            NTu = ntl * 128
                            hx, hxk = hx_t[ui % NHX], ("hx", ui % NHX)
                            qTs, qTsk = qTs_t[ui % 2], ("qTs", ui % 2)
                            for ti in range(par, ntl, 2):
                                t = tok0 // 128 + ti
                                for kc in range(8):
                                    I("pe", "matmul", [hxk, "win"], [pqk], out=pq[:],
                                      lhsT=hx[:, kc, ti * 128:(ti + 1) * 128], rhs=win[:, kc, 0:512],
                                      start=(kc == 0), stop=(kc == 7))
                                yield
                                for kc in range(8):
                                    I("pe", "matmul", [hxk, "win"], [pkvk], out=pkv[:, 0:256],
                                      lhsT=hx[:, kc, ti * 128:(ti + 1) * 128], rhs=win[:, kc, 512:768],
                                      start=(kc == 0), stop=(kc == 7))
                                yield
                                sq, sqk = sq_r.next()
                                qs, qsk = qs_r.next()
                                qn, qnk = qn_r.next()
                                qr, qrk = qr_r.next()
                                I("act", "activation", [pqk], [sqk], out=sq[:, 0:512], in_=pq[:], func=AF.Square)
                                I("act", "activation", [pkvk], [sqk], out=sq[:, 512:640], in_=pkv[:, 0:128],
                                  func=AF.Square)
                                yield
                                I("dve", "reduce_sum", [sqk], [qsk], out=qs[:, 0:10],
                                  in_=sq[:].rearrange("p (h d) -> p h d", d=64), axis=AX.X)
                                yield
                                I("dve", "tensor_scalar", [qsk], [qsk], out=qs[:, 10:20], in0=qs[:, 0:10], scalar1=1.0 / 64,
                                  scalar2=EPS, op0=ALU.mult, op1=ALU.add)
                                yield
                                I("act", "activation", [qsk], [qsk], out=qs[:, 10:20], in_=qs[:, 10:20], func=AF.Sqrt)
                                yield
                                I("dve", "reciprocal", [qsk], [qsk], out=qs[:, 20:30], in_=qs[:, 10:20])
                                yield
                                I("dve", "tensor_tensor", [pqk, qsk], [qnk], out=qn[:, 0:8, :],
                                  in0=pq[:].rearrange("p (h d) -> p h d", d=64),
                                  in1=qs[:, 20:28].unsqueeze(2).to_broadcast([128, 8, 64]), op=ALU.mult)
                                I("dve", "tensor_tensor", [pkvk, qsk, qnk], [qnk], out=qn[:, 8:10, :],
                                  in0=pkv[:, 0:128].rearrange("p (h d) -> p h d", d=64),
                                  in1=qs[:, 28:30].unsqueeze(2).to_broadcast([128, 2, 64]), op=ALU.mult)
                                I("act", "copy", [pkvk], [("Vp", t)], out=Vp[:, t, 0, 0:64], in_=pkv[:, 128:192])
                                I("act", "copy", [pkvk, ("Vp", t)], [("Vp", t)], out=Vp[:, t, 1, 64:128],
                                  in_=pkv[:, 192:256])
                                yield
                                if is_ctx:
                                    I("pool", "tensor_tensor", [qnk, "gqk"], [qrk], out=qr[:, 0:8, :], in0=qn[:, 0:8, :],
                                      in1=gq_bc, op=ALU.mult)
                                    I("pool", "tensor_tensor", [qnk, "gqk", qrk], [qrk], out=qr[:, 8:10, :],
                                      in0=qn[:, 8:10, :], in1=gk_bc, op=ALU.mult)
                                    yield
                                else:
                                    I("pool", "tensor_tensor", [qnk, "gqk"], [qnk], out=qn[:, 0:8, :], in0=qn[:, 0:8, :],
                                      in1=gq_bc, op=ALU.mult)
                                    I("pool", "tensor_tensor", [qnk, "gqk"], [qnk], out=qn[:, 8:10, :], in0=qn[:, 8:10, :],
                                      in1=gk_bc, op=ALU.mult)
                                    rp, rpk = rope_r.next()
                                    DMA("sp", [], [rpk], out=rp[:], in_=rope_in[(t - 2) * 128:(t - 1) * 128, :])
                                    yield
                                    ra, rak = ra_r.next()
                                    rb, rbk = rb_r.next()
                                    cosb = rp[:, 0:32].unsqueeze(1).to_broadcast([128, 10, 32])
                                    sinb = rp[:, 32:64].unsqueeze(1).to_broadcast([128, 10, 32])
                                    t1 = qn[:, :, 0:32]
                                    t2 = qn[:, :, 32:64]
                                    I("dve", "tensor_tensor", [qnk, rpk], [rak], out=ra[:], in0=t1, in1=cosb, op=ALU.mult)
                                    I("pool", "tensor_tensor", [qnk, rpk], [rbk], out=rb[:], in0=t2, in1=sinb, op=ALU.mult)
                                    yield
                                    I("dve", "tensor_tensor", [rak, rbk], [qrk], out=qr[:, :, 0:32], in0=ra[:], in1=rb[:],
                                      op=ALU.subtract)
                                    yield
                                    I("dve", "tensor_tensor", [qnk, rpk, rak], [rak], out=ra[:], in0=t1, in1=sinb,
                                      op=ALU.mult)
                                    I("pool", "tensor_tensor", [qnk, rpk, rbk], [rbk], out=rb[:], in0=t2, in1=cosb,
                                      op=ALU.mult)
                                    yield
                                    I("dve", "tensor_tensor", [rak, rbk, qrk], [qrk], out=qr[:, :, 32:64], in0=ra[:],
                                      in1=rb[:], op=ALU.add)
                                    yield
                                qrf = qr[:].rearrange("p h d -> p (h d)")
                                for j in range(5):
                                    I("pe", "transpose", [qrk, "ident_b"], [pqTk], out=pqT[:, j, :],
                                      in_=qrf[:, j * 128:(j + 1) * 128], identity=ident_b[:])
                                yield
                                I("act", "copy", [pqTk], [(qTsk, ti)], out=qTs[:, :, ti * 128:(ti + 1) * 128],
                                  in_=pqT[:, 0:4, :])
                                I("act", "copy", [pqTk], [("KT", t)], out=KTz[0][0:64, t * 128:(t + 1) * 128],
                                  in_=pqT[0:64, 4, :])
                                I("act", "copy", [pqTk, ("KT", t)], [("KT", t)],
                                  out=KTz[1][64:128, t * 128:(t + 1) * 128], in_=pqT[64:128, 4, :])
                                yield
                            prog1["b1n"][ui] = prog1["b1n"].get(ui, 0) + 1
                            if prog1["b1n"][ui] == 2:
                                DMA("pool", [(qTsk, ti) for ti in range(ntl)], [("qT", tok0)],
                                    out=qT[:, :, tok0:tok0 + NTu].rearrange("j p n -> p j n"), in_=qTs[:, :, 0:NTu])
                                prog1["b1"] = ui + 1

                    def b2_stream():
                        for ui, (tok0, ntl, is_ctx) in enumerate(ulist):
                            while prog1["a"] <= ui:
                                yield
                            NTu = ntl * 128
                            hx, hxk = hx_t[ui % NHX], ("hx", ui % NHX)
                            for n in range(10):
                                pf, pfk = pf_r.next()
                                for kc in range(8):
                                    I("pe", "matmul", [hxk, "win"], [pfk], out=pf[:, 0:NTu],
                                      lhsT=win[:, kc, 768 + n * 128:768 + (n + 1) * 128], rhs=hx[:, kc, 0:NTu],
                                      start=(kc == 0), stop=(kc == 7))
                                    if kc % 4 == 3:
                                        yield
                                fs, fsk = fs_r.next()
                                if n % 2 == 0:
                                    I("act", "copy", [pfk], [fsk], out=fs[:, 0:NTu], in_=pf[:, 0:NTu])
                                else:
                                    I("dve", "tensor_copy", [pfk], [fsk], out=fs[:, 0:NTu], in_=pf[:, 0:NTu])
                                DMA("pool", [fsk], [("featT", n, tok0)], out=featT[n, :, tok0:tok0 + NTu],
                                    in_=fs[:, 0:NTu])
                                yield
                            prog1["b2"] = ui + 1
                    run_streams([a_stream(), b1_stream(0), b1_stream(1), b2_stream()])
                S.barrier()
                if stop_after == f"p1_{l}":
                    if debug:
                        with contextlib.ExitStack() as es:
                            kd = sb(es, "kd", [128, 1024], F32)
                            I("dve", "tensor_copy", [], ["kd"], out=kd[:, 0:512], in_=KTz[0][:, 0:512])
                            I("dve", "tensor_copy", ["kd"], ["kd"], out=kd[:, 512:1024], in_=KTz[1][:, 0:512])
                            DMA("sp", ["kd"], ["dbg"], out=dbg[:, 0:1024], in_=kd[:])
                            vd = sb(es, "vd", [128, 1024], F32)
                            I("dve", "tensor_copy", [], ["vd"], out=vd[:],
                              in_=Vp[:, 0:4].rearrange("p t h d -> p (t h d)"))
                            DMA("sp", ["vd"], ["dbg"], out=dbg[:, 1024:2048], in_=vd[:])
                    return finish(nc, S, top)

                def seq_bounds(t0):
                    return (0, CTX) if t0 < CTX else (CTX, NTOK)

                CSEG = 512
                segs_c = [(0, CTX)] + [(CTX + CSEG * i, CSEG) for i in range(SEQ // CSEG)]
                SEG = 256
                segs_l = [(0, CTX)] + [(CTX + SEG * i, SEG) for i in range(SEQ // SEG)]
                GSEG = 512
                segs_g = [(0, CTX)] + [(CTX + GSEG * i, GSEG) for i in range(SEQ // GSEG)]
                with contextlib.ExitStack() as es:
                    cw = sb(es, "cw", [128, 2, 3], F32)
                    DMA("sp", [], ["cw"], out=cw[:], in_=convw_in[l])
                    lcw = sb(es, "lcw", [128, 2, 5], F32)
                    lvec = sb(es, "lvec", [128, 2, 2, 3], F32)
                    nlv = sb(es, "nlv", [128, 2, 2, 2], F32)
                    cneg = sb(es, "cneg", [128, 2, 2], F32)
                    DMA("sp", [], ["lcw"], out=lcw[:], in_=lcw_in[l])
                    DMA("sp", [], ["lvec"], out=lvec[:], in_=lvec_in[l])
                    I("dve", "tensor_scalar", ["lvec"], ["nlv"], out=nlv[:], in0=lvec[:, :, :, 0:2], scalar1=-1.0,
                      scalar2=None, op0=ALU.mult)
                    I("act", "activation", ["lvec"], ["cneg"], out=cneg[:], in_=lvec[:, :, :, 2], func=AF.Exp, scale=-1.0)
                    I("dve", "tensor_scalar", ["cneg"], ["cneg"], out=cneg[:], in0=cneg[:], scalar1=1.0, scalar2=None,
                      op0=ALU.add)
                    I("act", "activation", ["cneg"], ["cneg"], out=cneg[:], in_=cneg[:], func=AF.Ln)
                    I("dve", "tensor_scalar", ["cneg"], ["cneg"], out=cneg[:], in0=cneg[:], scalar1=-8.0, scalar2=None,
                      op0=ALU.mult)
                    Wblk = sb(es, "Wblk", [128, 2, 2, 2, 128], F32)
                    I("pool", "memset", [], ["Wblk"], ap=Wblk[:], constant=0.0)
                    for c in range(2):
                        for d in range(2):
                            for bi_ in range(2):
                                blk = 2 * c + bi_
                                DMA("sp", [], ["Wblk"],
                                    out=Wblk[bi_ * 64:(bi_ + 1) * 64, c, d, 0, bi_ * 64:(bi_ + 1) * 64], in_=lwa_in[l, d, blk])
                                DMA("sp", [], ["Wblk"],
                                    out=Wblk[bi_ * 64:(bi_ + 1) * 64, c, d, 1, bi_ * 64:(bi_ + 1) * 64], in_=lwi_in[l, d, blk])
                    prog2 = {"scan": [0, 0]}

                    def conv_stream(c):
                        P = f"cv{c}_"
                        ccs = sb(es, P + "ccs", [128, CSEG + 2], F32)
                        chs = sb(es, P + "chs", [128, CSEG + 2], F32)
                        cbs = sb(es, P + "cbs", [128, CSEG], F32)
                        uu = sb(es, P + "uu", [128, CSEG + 2], F32)
                        yy = sb(es, P + "yy", [128, CSEG], F32)
                        oo = sb(es, P + "oo", [128, CSEG], BF)
                        for (t0, n) in segs_c:
                            if t0 < CTX and not need_ctx:
                                continue
                            lo_s, hi_s = seq_bounds(t0)
                            lo = max(t0 - 1, lo_s)
                            hi = min(t0 + n + 1, hi_s)
                            off = lo - (t0 - 1)
                            DMA("sp", [], [P + "ccs"], out=ccs[:, off:off + hi - lo], in_=featT[2 + c, :, lo:hi])
                            DMA("sp", [], [P + "chs"], out=chs[:, off:off + hi - lo], in_=featT[4 + c, :, lo:hi])
                            DMA("sp", [], [P + "cbs"], out=cbs[:, 0:n], in_=featT[0 + c, :, t0:t0 + n])
                            yield
                            I("pool", "tensor_tensor", [P + "ccs", P + "chs"], [P + "uu"], out=uu[:, off:off + hi - lo],
                              in0=ccs[:, off:off + hi - lo], in1=chs[:, off:off + hi - lo], op=ALU.mult)
                            if off == 1:
                                I("pool", "memset", [P + "uu"], [P + "uu"], ap=uu[:, 0:1], constant=0.0)
                            if hi < t0 + n + 1:
                                I("pool", "memset", [P + "uu"], [P + "uu"], ap=uu[:, n + 1:n + 2], constant=0.0)
                            yield
                            I("dve", "tensor_scalar", [P + "uu", "cw"], [P + "yy"], out=yy[:, 0:n], in0=uu[:, 1:n + 1],
                              scalar1=cw[:, c, 1:2], scalar2=None, op0=ALU.mult)
                            yield
                            I("dve", "scalar_tensor_tensor", [P + "uu", "cw", P + "yy"], [P + "yy"], out=yy[:, 0:n],
                              in0=uu[:, 0:n], scalar=cw[:, c, 0:1], in1=yy[:, 0:n], op0=ALU.mult, op1=ALU.add)
                            yield
                            I("dve", "scalar_tensor_tensor", [P + "uu", "cw", P + "yy"], [P + "yy"], out=yy[:, 0:n],
                              in0=uu[:, 2:n + 2], scalar=cw[:, c, 2:3], in1=yy[:, 0:n], op0=ALU.mult, op1=ALU.add)
                            yield
                            I("dve", "tensor_tensor", [P + "yy", P + "cbs"], [P + "oo"], out=oo[:, 0:n], in0=yy[:, 0:n],
                              in1=cbs[:, 0:n], op=ALU.mult)
                            DMA("pool", [P + "oo"], [("mixT", 4 + c, t0)], out=mixT[4 + c, :, t0:t0 + n], in_=oo[:, 0:n])
                            yield

                    def scan_lane(d):
                        P = f"ls{d}_"
                        lus = sb(es, P + "lus", [128, SEG + 3], F32)
                        uc = sb(es, P + "uc", [128, SEG], F32)
                        rr = sb(es, P + "rr", [128, SEG], F32)
                        ii = sb(es, P + "ii", [128, SEG], F32)
                        tt = sb(es, P + "tt", [128, SEG], F32)
                        hh = sb(es, P + "hh", [128, SEG], F32)
                        carry = sb(es, P + "carry", [128, 1], F32)
                        pp = ps(es, P + "pp", [128, 512], F32)
                        order = segs_l if d == 0 else [segs_l[0]] + segs_l[:0:-1]
                        for c in range(2):
                            I("dve", "tensor_copy", ["zcol"], [P + "carry"], out=carry[:], in_=zcol[:])
                            for (t0, n) in order:
                                lo_s, hi_s = seq_bounds(t0)
                                lo = max(t0 - 2, lo_s)
                                hi = min(t0 + n + 1, hi_s)
                                off = lo - (t0 - 2)
                                if off > 0:
                                    I("pool", "memset", [], [P + "lus"], ap=lus[:, 0:2], constant=0.0)
                                if hi < t0 + n + 1:
                                    I("pool", "memset", [], [P + "lus"], ap=lus[:, n + 2:n + 3], constant=0.0)
                                DMA("sp", [], [P + "lus"], out=lus[:, off:off + hi - lo], in_=featT[6 + c, :, lo:hi])
                                yield
                                I("dve", "tensor_scalar", [P + "lus", "lcw"], [P + "uc"], out=uc[:, 0:n], in0=lus[:, 2:n + 2],
                                  scalar1=lcw[:, c, 2:3], scalar2=lcw[:, c, 4:5], op0=ALU.mult, op1=ALU.add)
                                yield
                                for k_, o_ in ((0, 0), (1, 1), (3, 3)):
                                    I("dve", "scalar_tensor_tensor", [P + "lus", "lcw", P + "uc"], [P + "uc"], out=uc[:, 0:n],
                                      in0=lus[:, o_:o_ + n], scalar=lcw[:, c, k_:k_ + 1], in1=uc[:, 0:n],
                                      op0=ALU.mult, op1=ALU.add)
                                    yield
                                I("pe", "matmul", ["Wblk", P + "uc"], [P + "pp"], out=pp[:, 0:n], lhsT=Wblk[:, c, d, 0, :],
                                  rhs=uc[:, 0:n], start=True, stop=True)
                                I("pe", "matmul", ["Wblk", P + "uc"], [P + "pp"], out=pp[:, 256:256 + n],
                                  lhsT=Wblk[:, c, d, 1, :], rhs=uc[:, 0:n], start=True, stop=True)
                                yield
                                I("act", "activation", [P + "pp", "nlv"], [P + "rr"], out=rr[:, 0:n], in_=pp[:, 0:n],
                                  func=AF.Exp, scale=-1.0, bias=nlv[:, d, c, 0:1])
                                I("act", "activation", [P + "pp", "nlv"], [P + "ii"], out=ii[:, 0:n], in_=pp[:, 256:256 + n],
                                  func=AF.Exp, scale=-1.0, bias=nlv[:, d, c, 1:2])
                                yield
                                I("dve", "tensor_scalar", [P + "rr"], [P + "rr"], out=rr[:, 0:n], in0=rr[:, 0:n], scalar1=1.0,
                                  scalar2=None, op0=ALU.add)
                                yield
                                I("dve", "reciprocal", [P + "rr"], [P + "rr"], out=rr[:, 0:n], in_=rr[:, 0:n])
                                yield
                                I("pool", "tensor_scalar", [P + "ii"], [P + "ii"], out=ii[:, 0:n], in0=ii[:, 0:n], scalar1=1.0,
                                  scalar2=None, op0=ALU.add)
                                yield
                                I("dve", "reciprocal", [P + "ii"], [P + "ii"], out=ii[:, 0:n], in_=ii[:, 0:n])
                                yield
                                I("act", "activation", [P + "rr", "cneg"], [P + "rr"], out=rr[:, 0:n], in_=rr[:, 0:n],
                                  func=AF.Exp, scale=cneg[:, d, c:c + 1])
                                I("pool", "tensor_tensor", [P + "ii", P + "uc"], [P + "ii"], out=ii[:, 0:n], in0=ii[:, 0:n],
                                  in1=uc[:, 0:n], op=ALU.mult)
                                yield
                                I("pool", "tensor_tensor", [P + "rr"], [P + "tt"], out=tt[:, 0:n], in0=rr[:, 0:n],
                                  in1=rr[:, 0:n], op=ALU.mult)
                                yield
                                I("act", "activation", [P + "tt"], [P + "tt"], out=tt[:, 0:n], in_=tt[:, 0:n], func=AF.Ln,
                                  scale=-1.0, bias=1.0)
                                I("act", "activation", [P + "tt"], [P + "tt"], out=tt[:, 0:n], in_=tt[:, 0:n], func=AF.Exp,
                                  scale=0.5)
                                yield
                                I("dve", "tensor_tensor", [P + "ii", P + "tt"], [P + "ii"], out=ii[:, 0:n], in0=ii[:, 0:n],
                                  in1=tt[:, 0:n], op=ALU.mult)
                                yield
                                if d == 0:
                                    I("dve", "tensor_tensor_scan", [P + "rr", P + "ii", P + "carry"], [P + "hh"],
                                      out=hh[:, 0:n], data0=rr[:, 0:n], data1=ii[:, 0:n], initial=carry[:, 0:1],
                                      op0=ALU.mult, op1=ALU.add)
                                    I("dve", "tensor_copy", [P + "hh", P + "carry"], [P + "carry"], out=carry[:],
                                      in_=hh[:, n - 1:n])
                                else:
                                    I("dve", "tensor_tensor_scan", [P + "rr", P + "ii", P + "carry"], [P + "hh"],
                                      out=hh[:, 0:n][:, ::-1], data0=rr[:, 0:n][:, ::-1], data1=ii[:, 0:n][:, ::-1],
                                      initial=carry[:, 0:1], op0=ALU.mult, op1=ALU.add)
                                    I("dve", "tensor_copy", [P + "hh", P + "carry"], [P + "carry"], out=carry[:],
                                      in_=hh[:, 0:1])
                                if not (t0 < CTX and not need_ctx):
                                    DMA("pool", [P + "hh"], [("hD", d, c, t0)], out=hD[d, c, :, t0:t0 + n], in_=hh[:, 0:n])
                                yield
                            prog2["scan"][c] += 1

                    def comb_stream(c):
                        P = f"cb{c}_"
                        h0 = sb(es, P + "h0", [128, GSEG], F32)
                        h1 = sb(es, P + "h1", [128, GSEG], F32)
                        lgs = sb(es, P + "lgs", [128, GSEG], F32)
                        tt = sb(es, P + "tt", [128, GSEG], F32)
                        ob = sb(es, P + "ob", [128, GSEG], BF)
                        while prog2["scan"][c] < 2:
                            yield
                        for si, (t0, n) in enumerate(segs_g):
                            if t0 < CTX and not need_ctx:
                                continue
                            rk = [("hD", d_, c, t0 + o_) for d_ in range(2) for o_ in range(0, n, SEG)]
                            DMA("sp", rk, [P + "h0"], out=h0[:, 0:n], in_=hD[0, c, :, t0:t0 + n])
                            DMA("sp", rk, [P + "h1"], out=h1[:, 0:n], in_=hD[1, c, :, t0:t0 + n])
                            DMA("sp", [], [P + "lgs"], out=lgs[:, 0:n], in_=featT[8 + c, :, t0:t0 + n])
                            yield
                            I("pool", "tensor_tensor", [P + "h0", P + "h1"], [P + "h0"], out=h0[:, 0:n], in0=h0[:, 0:n],
                              in1=h1[:, 0:n], op=ALU.add)
                            I("dve", "tensor_tensor", [P + "lgs"], [P + "tt"], out=tt[:, 0:n], in0=lgs[:, 0:n],
                              in1=lgs[:, 0:n], op=ALU.mult)
                            yield
                            I("dve", "tensor_scalar", [P + "tt"], [P + "tt"], out=tt[:, 0:n], in0=tt[:, 0:n],
                              scalar1=0.044715, scalar2=1.0, op0=ALU.mult, op1=ALU.add)
                            yield
                            I("pool", "tensor_tensor", [P + "tt", P + "lgs"], [P + "tt"], out=tt[:, 0:n], in0=tt[:, 0:n],
                              in1=lgs[:, 0:n], op=ALU.mult)
                            yield
                            I("act", "activation", [P + "tt"], [P + "tt"], out=tt[:, 0:n], in_=tt[:, 0:n],
                              func=AF.Exp, scale=-1.5957691216057308)
                            yield
                            I("dve", "tensor_scalar", [P + "tt"], [P + "tt"], out=tt[:, 0:n], in0=tt[:, 0:n], scalar1=1.0,
                              scalar2=None, op0=ALU.add)
                            yield
                            I("dve", "reciprocal", [P + "tt"], [P + "tt"], out=tt[:, 0:n], in_=tt[:, 0:n])
                            yield
                            I("pool", "tensor_tensor", [P + "tt", P + "lgs"], [P + "tt"], out=tt[:, 0:n], in0=tt[:, 0:n],
                              in1=lgs[:, 0:n], op=ALU.mult)
                            yield
                            I("dve", "tensor_tensor", [P + "tt", P + "h0"], [P + "ob"], out=ob[:, 0:n], in0=tt[:, 0:n],
                              in1=h0[:, 0:n], op=ALU.mult)
                            DMA("pool", [P + "ob"], [("mixT", 6 + c, t0)], out=mixT[6 + c, :, t0:t0 + n], in_=ob[:, 0:n])
                            yield

                    def att_stream():
                        qt_r = ring(es, "qt", 2, [128, 512], BF)
                        mo_r = ring(es, "mo", 2, [128, 512], BF)
                        pe_r = ring(es, "pex", 2, [128, 2, 512], BF)
                        rec_r = ring(es, "rec", 2, [128, 512], F32)
                        ps_r = ring(es, "pss", 2, [128, 2, 512], F32, psum=True)
                        po_r = ring(es, "po", 2, [128, 512], F32, psum=True)
                        qblocks = []
                        if need_ctx:
                            qblocks.append((0, CTX, [0, 1]))
                        for i in range(16):
                            qblocks.append((CTX + 512 * i, 512, list(range(NT))))
                        for (q0, N, kts) in qblocks:
                            for j in range(4):
                                qt, qtk = qt_r.next()
                                DMA("sp", [], [qtk], out=qt[:, 0:N], in_=qT[j, :, q0:q0 + N])
                                mo, mok = mo_r.next()
                                for half in range(2):
                                    po, pok = po_r.next()
                                    npair = len(kts) // 2

                                    def emit_s(pi_):
                                        p_, pk_ = ps_r.next()
                                        for u_ in range(2):
                                            kt = kts[2 * pi_ + u_]
                                            I("pe", "matmul", [qtk, ("KTz", half)], [pk_], out=p_[:, u_, 0:N],
                                              lhsT=KTz[half][:, kt * 128:(kt + 1) * 128], rhs=qt[:, 0:N], start=True, stop=True)
                                        return p_, pk_
                                    cur = emit_s(0)
                                    for pi_ in range(npair):
                                        nxt = emit_s(pi_ + 1) if pi_ + 1 < npair else None
                                        p_, pk_ = cur
                                        ex, exk = pe_r.next()
                                        I("act", "activation", [pk_], [exk], out=ex[:, :, 0:N], in_=p_[:, :, 0:N], func=AF.Exp)
                                        for u_ in range(2):
                                            i = 2 * pi_ + u_
                                            I("pe", "matmul", [exk, "Vp"], [pok], out=po[:, 0:N], lhsT=Vp[:, kts[i], half, :],
                                              rhs=ex[:, u_, 0:N], start=(i == 0), stop=(i == len(kts) - 1))
                                        cur = nxt
                                        yield
                                    rec, reck = rec_r.next()
                                    o_sl = slice(0, 64) if half == 0 else slice(64, 128)
                                    s_sl = slice(64, 128) if half == 0 else slice(0, 64)
                                    I("dve", "reciprocal", [pok], [reck], out=rec[o_sl, 0:N], in_=po[s_sl, 0:N])
                                    I("dve", "tensor_tensor", [pok, reck], [mok], out=mo[o_sl, 0:N], in0=po[o_sl, 0:N],
                                      in1=rec[o_sl, 0:N], op=ALU.mult)
                                DMA("pool", [mok], [("mixT", j, q0)], out=mixT[j, :, q0:q0 + N], in_=mo[:, 0:N])
                                yield

                    def wconv_stream():
                        for q4 in range(4):
                            DMA("pool", [], [("woutbf", q4)], out=woutbf[q4 * 256:(q4 + 1) * 256, :],
                                in_=wout_in[l, q4 * 256:(q4 + 1) * 256, :])
                            yield
                        if l + 1 < nlayers:
                            for kc in range(8):
                                DMA("pool", [], [("winbf", kc)], out=winbf[kc * 128:(kc + 1) * 128, :],
                                    in_=win_in[l + 1, kc * 128:(kc + 1) * 128, :])
                                yield
                        if wbf is None:
                            return
                        for e in range(NE):
                            for m_, src in enumerate((wg_in, wu_in, wd_in)):
                                for q4 in range(4):
                                    for _ in range(18):
                                        yield
                                    DMA("pool", [], [("wbf", m_, e, q4)], out=wbf[m_, e, q4 * 256:(q4 + 1) * 256, :],
                                        in_=src[l, e, q4 * 256:(q4 + 1) * 256, :])

                    run_streams([att_stream(), paced(conv_stream(0), 3), paced(conv_stream(1), 3),
                                 paced(scan_lane(0), 3), paced(scan_lane(1), 3),
                                 paced(comb_stream(0), 3), paced(comb_stream(1), 3), wconv_stream()])
            S.barrier()
            if stop_after == f"p2c_{l}":
                return finish(nc, S, top)

            with contextlib.ExitStack() as es:
                wr = sb(es, "wr", [128, 8, NE], F32)
                DMA("sp", [], ["wr"], out=wr[:], in_=wr_in[l].rearrange("(k p) e -> p k e", p=128))
                nstream = 2 if need_ctx else 1
                wout_s = [sb(es, f"wout_s{s}", [128, 8, D], BF) for s in range(nstream)]
                G2_bc = [sb(es, f"G2_bc{s}", [128, D], F32) for s in range(nstream)]
                sh2_bc = [sb(es, f"sh2_bc{s}", [128, D], F32) for s in range(nstream)]
                with contextlib.ExitStack() as es2:
                    wout = sb(es2, "wout", [128, 8, D], BF)
                    for h_ in range(2):
                        DMA("sp", [], ["wout"], out=wout[:, 4 * h_:4 * h_ + 4, :],
                            in_=woutbf[512 * h_:512 * (h_ + 1), :].rearrange("(k p) n -> p k n", p=128))
                    gate_bc = [sb(es2, f"gate_bc{s}", [128, D], F32) for s in range(nstream)]
                    tmpD = sb(es2, "tmpD", [128, 128], F32)
                    pbc = ps(es2, "pbc", [128, 512], F32)
                    for s in range(nstream):
                        make_bc(es, gate_bc[s], f"gate_bc{s}", lambda kc, s=s: modsl(2)[:, kc, s:s + 1], ("modT", l), tmpD,
                                "tmpD", pbc, "pbc")
                        make_bc(es, G2_bc[s], f"G2_bc{s}", lambda kc, s=s: G2T[:, kc, s:s + 1], "G2T", tmpD, "tmpD", pbc,
                                "pbc")
                        make_bc(es, sh2_bc[s], f"sh2_bc{s}", lambda kc, s=s: sh2T[:, kc, s:s + 1], ("modT", l), tmpD,
                                "tmpD", pbc, "pbc")
                        for kc in range(8):
                            I("dve" if kc % 2 == 0 else "pool", "tensor_tensor", ["wout", f"gate_bc{s}"], [f"wout_s{s}"],
                              out=wout_s[s][:, kc, :], in0=wout[:, kc, :], in1=gate_bc[s][:], op=ALU.mult)
                    S.barrier()
                junk3 = sb(es, "junk3", [128, D], BF)

                def p3_stream(k, nk):
                    P = f"p3{k}_"
                    mx = sb(es, P + "mx", [128, 8, 512], BF)
                    xt = sb(es, P + "xt", [128, D], F32)
                    x1 = sb(es, P + "x1", [128, D], F32)
                    h2 = sb(es, P + "h2", [128, D], F32)
                    row = sb(es, P + "row", [128, ROWW], BF)
                    h2T = sb(es, P + "h2T", [128, 8, 128], F32)
                    sm = sb(es, P + "sm", [128, 8], F32)
                    ex = sb(es, P + "ex", [128, NE], F32)
                    py = ps(es, P + "py", [128, 512], F32)
                    pta = ps(es, P + "pta", [128, 4, 128], F32)
                    ptb = ps(es, P + "ptb", [128, 4, 128], F32)
                    plog_t = ps(es, P + "plog", [128, 512], F32)
                    plog = plog_t[:, 0:NE]
                    mxk, xtk, x1k, h2k, rowk, h2Tk, smk, exk = (P + n_ for n_ in ("mx", "xt", "x1", "h2", "row", "h2T", "sm", "ex"))
                    pyk, ptak, ptbk, plogk = P + "py", P + "pta", P + "ptb", P + "plog"
                    ulist = [u_ for u_ in units if not (u_[2] and not need_ctx)]
                    for ui, (tok0, ntl, is_ctx) in enumerate(ulist):
                        if ui % nk != k:
                            continue
                        s = 1 if is_ctx else 0
                        NTu = ntl * 128
                        DMA("sp", [], [mxk], out=mx[:, :, 0:NTu], in_=mixT[:, :, tok0:tok0 + NTu].rearrange("c p n -> p c n"))
                        for ti in range(ntl):
                            t = tok0 // 128 + ti
                            DMA("sp", [], [xtk], out=xt[:], in_=src_rows(l, tok0 + ti * 128, 128))
                            yield
                            for nh in range(2):
                                cs = slice(nh * 512, (nh + 1) * 512)
                                for kc in range(8):
                                    I("pe", "matmul", [mxk, f"wout_s{s}"], [pyk], out=py[:],
                                      lhsT=mx[:, kc, ti * 128:(ti + 1) * 128], rhs=wout_s[s][:, kc, cs],
                                      start=(kc == 0), stop=(kc == 7))
                                yield
                                I("dve", "tensor_tensor", [pyk, xtk], [x1k], out=x1[:, cs], in0=py[:], in1=xt[:, cs],
                                  op=ALU.add)
                                yield
                            DMA("pool", [x1k], [("xa", t)], out=xa[tok0 + ti * 128:tok0 + (ti + 1) * 128, :], in_=x1[:])
                            I("pool", "memset", [], [smk], ap=sm[:], constant=0.0)
                            yield
                            I("act", "activation", [x1k, smk], ["junk3", smk], out=junk3[:], in_=x1[:], func=AF.Square,
                              accum_out=sm[:, 0:1])
                            yield
                            I("dve", "tensor_scalar", [smk], [smk], out=sm[:, 1:2], in0=sm[:, 0:1], scalar1=1.0 / D,
                              scalar2=EPS, op0=ALU.mult, op1=ALU.add)
                            yield
                            I("act", "activation", [smk], [smk], out=sm[:, 1:2], in_=sm[:, 1:2], func=AF.Sqrt)
                            yield
                            I("dve", "reciprocal", [smk], [smk], out=sm[:, 2:3], in_=sm[:, 1:2])
                            yield
                            I("dve", "scalar_tensor_tensor", [x1k, smk, f"G2_bc{s}"], [h2k], out=h2[:], in0=x1[:],
                              scalar=sm[:, 2:3], in1=G2_bc[s][:], op0=ALU.mult, op1=ALU.mult)
                            yield
                            I("pool", "tensor_tensor", [h2k, f"sh2_bc{s}"], [h2k], out=h2[:], in0=h2[:], in1=sh2_bc[s][:],
                              op=ALU.add)
                            yield
                            I("act", "copy", [h2k], [rowk], out=row[:, 0:D], in_=h2[:])
                            for kc in range(8):
                                pt_ = pta if kc < 4 else ptb
                                I("pe", "transpose", [h2k, "cst_f"], [ptak if kc < 4 else ptbk], out=pt_[:, kc % 4, :],
                                  in_=h2[:, kc * 128:(kc + 1) * 128], identity=ident_f)
                            yield
                            I("act", "copy", [ptak], [h2Tk], out=h2T[:, 0:4, :], in_=pta[:])
                            I("dve", "tensor_copy", [ptbk, h2Tk], [h2Tk], out=h2T[:, 4:8, :], in_=ptb[:])
                            yield
                            for kc in range(8):
                                I("pe", "matmul", [h2Tk, "wr"], [plogk], out=plog, lhsT=h2T[:, kc, :], rhs=wr[:, kc, :],
                                  start=(kc == 0), stop=(kc == 7))
                            yield
                            I("dve", "reduce_max", [plogk, smk], [smk], out=sm[:, 3:4], in_=plog, axis=AX.X)
                            yield
                            I("dve", "tensor_scalar", [smk], [smk], out=sm[:, 3:4], in0=sm[:, 3:4], scalar1=-1.0, scalar2=None,
                              op0=ALU.mult)
                            yield
                            I("act", "activation", [plogk, smk], [exk, smk], out=ex[:], in_=plog, func=AF.Exp,
                              bias=sm[:, 3:4], accum_out=sm[:, 4:5])
                            yield
                            I("dve", "reciprocal", [smk], [smk], out=sm[:, 5:6], in_=sm[:, 4:5])
                            yield
                            I("dve", "tensor_scalar", [exk, smk], [("aff", t)], out=aff_all[:, t, :], in0=ex[:],
                              scalar1=sm[:, 5:6], scalar2=None, op0=ALU.mult)
                            yield
                            I("dve", "tensor_copy", [("aff", t), rowk], [rowk], out=row[:, D:D + 32].bitcast(F32),
                              in_=aff_all[:, t, :])
                            I("pool", "tensor_copy", ["tokid", rowk], [rowk], out=row[:, D + 32:D + 34].bitcast(I32),
                              in_=tokid[:, t:t + 1])
                            DMA("pool", [rowk], [("h2D", t)], out=h2D[t * 128:(t + 1) * 128, :], in_=row[:])
                            yield
                run_streams([p3_stream(k, 2) for k in range(2)])
            S.barrier()
            if stop_after == f"p3_{l}":
                if debug:
                    DMA("sp", [], ["dbg"], out=dbg[:, 0:NT * NE], in_=aff_all[:].rearrange("p t e -> p (t e)"))
                return finish(nc, S, top)

            groups = [(2, NT, CAP, 0)]
            if need_ctx:
                groups.append((0, 2, CAPC, NE * CAP))
            ng = len(groups)
            idxi = sb(LS, "idxi", [128, NE, NT], I32)
            with contextlib.ExitStack() as es:
                lo_t = sb(es, "lo_t", [128, 2, NE], F32)
                hi_t = sb(es, "hi_t", [128, 2, NE], F32)
                mid = sb(es, "mid", [128, 2, NE], F32)
                kk = sb(es, "kk", [128, 2, NE], F32)
                cmp = sb(es, "cmp", [128, NT, NE], F32)
                cntb = sb(es, "cntb", [128, 2, NE], BF)
                cntf = sb(es, "cntf", [128, 2, NE], F32)
                geu = sb(es, "geu", [128, 2, NE], U32)
                ltu = sb(es, "ltu", [128, 2, NE], U32)
                ptot_t = ps(es, "ptot", [128, 512], F32)
                ptot = ptot_t[:, 0:2 * NE]
                I("dve", "memset", [], ["lo_t"], ap=lo_t[:], constant=0.0)
                I("dve", "memset", [], ["hi_t"], ap=hi_t[:], constant=2.0)
                I("dve", "memset", [], ["kk"], ap=kk[:, 0, :], constant=float(CAP))
                I("dve", "memset", ["kk"], ["kk"], ap=kk[:, 1, :], constant=float(CAPC))
                I("dve", "memset", [], ["cntf"], ap=cntf[:], constant=0.0)

                def count_ge(thr, thrk):
                    for g, (tl, th, cap, rb) in enumerate(groups):
                        I("dve", "tensor_tensor", ["aff", thrk], ["cmp"], out=cmp[:, tl:th, :], in0=aff_all[:, tl:th, :],
                          in1=thr[:, g, :].unsqueeze(1).to_broadcast([128, th - tl, NE]), op=ALU.is_ge)
                        I("dve", "reduce_sum", ["cmp"], ["cntf"], out=cntf[:, g, :],
                          in_=cmp[:, tl:th, :].rearrange("p t e -> p e t"), axis=AX.X)
                    I("dve", "tensor_copy", ["cntf"], ["cntb"], out=cntb[:], in_=cntf[:])
                    I("pe", "matmul", ["cntb", "ones_b"], ["ptot"], out=ptot,
                      lhsT=ones_b[:], rhs=cntb[:].rearrange("p g e -> p (g e)"), start=True, stop=True)

                NIT = 34
                for it in range(NIT):
                    I("dve", "tensor_tensor", ["lo_t", "hi_t"], ["mid"], out=mid[:], in0=lo_t[:], in1=hi_t[:], op=ALU.add)
                    I("dve", "tensor_scalar", ["mid"], ["mid"], out=mid[:], in0=mid[:], scalar1=0.5, scalar2=None,
                      op0=ALU.mult)
                    count_ge(mid, "mid")
                    ptv = ptot.rearrange("p (g e) -> p g e", g=2)
                    I("dve", "tensor_tensor", ["ptot", "kk"], ["geu"], out=geu[:], in0=ptv, in1=kk[:], op=ALU.is_ge)
                    I("dve", "tensor_tensor", ["ptot", "kk"], ["ltu"], out=ltu[:], in0=ptv, in1=kk[:], op=ALU.is_lt)
                    I("dve", "copy_predicated", ["geu", "mid", "lo_t"], ["lo_t"], out=lo_t[:], mask=geu[:], data=mid[:])
                    I("dve", "copy_predicated", ["ltu", "mid", "hi_t"], ["hi_t"], out=hi_t[:], mask=ltu[:], data=mid[:])
                count_ge(lo_t, "lo_t")
                offp = sb(es, "offp", [128, 2, NE], F32)
                poff_t = ps(es, "poff", [128, 512], F32)
                poff = poff_t[:, 0:2 * NE]
                I("pe", "matmul", ["cntb", "ltri_b"], ["poff"], out=poff, lhsT=ltri_b[:],
                  rhs=cntb[:].rearrange("p g e -> p (g e)"), start=True, stop=True)
                I("act", "copy", ["poff"], ["offp"], out=offp[:].rearrange("p g e -> p (g e)"), in_=poff)
                Mt = sb(es, "Mt", [128, NE, NT], F32)
                cs_ = sb(es, "cs_", [128, NE, NT], F32)
                onesr = sb(es, "onesr", [128, NE * NT], F32)
                idxf = sb(es, "idxf", [128, NE, NT], F32)
                ebase = sb(es, "ebase", [128, 2, NE], F32)
                I("pool", "memset", [], ["onesr"], ap=onesr[:], constant=1.0)
                I("pool", "iota", [], ["ebase0"], out=idxi[:, :, 0], pattern=[[1, NE]], base=0, channel_multiplier=0)
                I("dve", "tensor_copy", ["ebase0"], ["ebase"], out=ebase[:, 0, :], in_=idxi[:, :, 0])
                I("dve", "tensor_scalar", ["ebase"], ["ebase"], out=ebase[:, 1, :], in0=ebase[:, 0, :], scalar1=float(CAPC),
                  scalar2=float(NE * CAP), op0=ALU.mult, op1=ALU.add)
                I("dve", "tensor_scalar", ["ebase"], ["ebase"], out=ebase[:, 0, :], in0=ebase[:, 0, :], scalar1=float(CAP),
                  scalar2=None, op0=ALU.mult)
                I("dve", "tensor_copy", ["cmp"], ["Mt"], out=Mt[:], in_=cmp[:].rearrange("p t e -> p e t"))
                for g, (tl, th, cap, rb) in enumerate(groups):
                    ntl_ = th - tl
                    for e in range(NE):
                        I("dve", "tensor_tensor_scan", ["Mt", "onesr", "zcol"], ["cs_"], out=cs_[:, e, tl:th],
                          data0=onesr[:, 0:ntl_], data1=Mt[:, e, tl:th], initial=zcol[:, 0:1], op0=ALU.mult, op1=ALU.add)
                    I("dve", "tensor_tensor", ["cs_", "Mt"], ["cs_"], out=cs_[:, :, tl:th], in0=cs_[:, :, tl:th],
                      in1=Mt[:, :, tl:th], op=ALU.subtract)
                    I("dve", "tensor_tensor", ["cs_", "offp"], ["cs_"], out=cs_[:, :, tl:th], in0=cs_[:, :, tl:th],
                      in1=offp[:, g, :].unsqueeze(2).to_broadcast([128, NE, ntl_]), op=ALU.add)
                    I("dve", "tensor_scalar", ["cs_"], ["idxf"], out=idxf[:, :, tl:th], in0=cs_[:, :, tl:th],
                      scalar1=float(cap), scalar2=None, op0=ALU.is_lt)
                    I("dve", "tensor_tensor", ["idxf", "Mt"], ["idxf"], out=idxf[:, :, tl:th], in0=idxf[:, :, tl:th],
                      in1=Mt[:, :, tl:th], op=ALU.mult)
                    I("dve", "tensor_tensor", ["cs_", "ebase"], ["cs_"], out=cs_[:, :, tl:th], in0=cs_[:, :, tl:th],
                      in1=ebase[:, g, :].unsqueeze(2).to_broadcast([128, NE, ntl_]), op=ALU.add)
                    I("dve", "tensor_scalar", ["cs_"], ["cs_"], out=cs_[:, :, tl:th], in0=cs_[:, :, tl:th],
                      scalar1=-BIGIDX, scalar2=None, op0=ALU.add)
                    I("dve", "tensor_tensor", ["idxf", "cs_"], ["idxf"], out=idxf[:, :, tl:th], in0=idxf[:, :, tl:th],
                      in1=cs_[:, :, tl:th], op=ALU.mult)
                    I("dve", "tensor_scalar", ["idxf"], ["idxf"], out=idxf[:, :, tl:th], in0=idxf[:, :, tl:th],
                      scalar1=BIGIDX, scalar2=None, op0=ALU.add)
                I("dve", "tensor_copy", ["idxf", "ebase0"], ["idxi"], out=idxi[:], in_=idxf[:])
                if debug:
                    DMA("sp", ["lo_t"], ["dbg"], out=dbg[:, 0:2 * NE], in_=lo_t[:].rearrange("p g e -> p (g e)"))
                    DMA("sp", ["idxf"], ["dbg"], out=dbg[:, 64:64 + NE * NT], in_=idxf[:].rearrange("p e t -> p (e t)"))
            S.barrier()
            if stop_after == f"p4c_{l}":
                return finish(nc, S, top)

            with contextlib.ExitStack() as es:
                mod5_bc = [sb(es, f"mod5_bc{s}", [128, D], F32) for s in range(ng)]
                with contextlib.ExitStack() as es2:
                    tmpD = sb(es2, "tmpD4", [128, 128], F32)
                    pbc = ps(es2, "pbc4", [128, 512], F32)
                    for s in range(ng):
                        make_bc(es, mod5_bc[s], f"mod5_bc{s}", lambda kc, s=s: modsl(5)[:, kc, s:s + 1], ("modT", l), tmpD,
                                "tmpD4", pbc, "pbc4")
                    S.barrier()
                wg_t = [sb(es, f"wg{i}", [128, 8, D], BF) for i in range(2)]
                wu_t = [sb(es, f"wu{i}", [128, 8, D], BF) for i in range(2)]
                wd_t = [sb(es, "wd0", [128, 8, D], BF)]
                xsb = sb(es, "xsb", [128, 8, ROWW], BF)
                xsc = sb(es, "xsc", [CAPC, ROWW], BF)
                meta_t = [sb(es, f"meta{i}", [128, 9, 40], BF) for i in range(2)]
                xsT_t = [sb(es, f"xsT{i}", [128, 8, CAP + CAPC], BF) for i in range(2)]
                actT = sb(es, "actT", [128, 8, CAP + CAPC], BF)
                sg_r = ring(es, "sg", 2, [128, 512], F32)
                yt_r = ring(es, "yt", 4, [128, D], F32)
                pxt_r = ring(es, "pxt", 2, [128, 8, 128], BF, psum=True)
                pg_r = ring(es, "pg", 2, [128, 512], F32, psum=True)
                pu_r = ring(es, "pu", 2, [128, 512], F32, psum=True)
                pyy_r = ring(es, "pyy", 2, [128, 512], F32, psum=True)
                rw_r = ring(es, "rw", 4, [128, ROWW], BF)
                prog = {"w_g": 0, "w_d": 0, "c_gu": 0, "c_dn": 0, "s_done": 0, "c_start": 0}
                NPASS = 4
                EPP = NE // NPASS

                def s_stream():
                    for g_ in range(NPASS):
                        while prog["c_start"] < EPP * (g_ - 1):
                            yield
                        for (tl, th, cap, rb) in groups:
                            for t in range(tl, th):
                                rw, rwk = rw_r.next()
                                DMA("sp", [], [rwk], out=rw[:], in_=h2D[t * 128:(t + 1) * 128, :])
                                for e in range(EPP * g_, EPP * (g_ + 1)):
                                    S.dma("pool", lambda en, rw=rw, e=e, t=t: en.indirect_dma_start(
                                        out=xs, out_offset=bass.IndirectOffsetOnAxis(ap=idxi[:, e, t:t + 1], axis=0),
                                        in_=rw[:], in_offset=None, bounds_check=preg(en, XS_ROWS - 1), oob_is_err=False),
                                        [rwk, "idxi"], [("xs", e, t)])
                                    yield
                        prog["s_done"] = EPP * (g_ + 1)


                def load_mat(m_, e, w, wk):
                    for h_ in range(2):
                        DMA("sp", [], [wk], out=w[:, 4 * h_:4 * h_ + 4, :],
                            in_=wbf[m_, e, 512 * h_:512 * (h_ + 1), :].rearrange("(k p) n -> p k n", p=128))
                        yield

                def w_stream():
                    for e in range(NE):
                        while prog["c_gu"] < e - 1:
                            yield
                        yield from load_mat(0, e, wg_t[e % 2], ("wg", e % 2))
                        yield from load_mat(1, e, wu_t[e % 2], ("wu", e % 2))
                        prog["w_g"] = e + 1
                        while prog["c_dn"] < e:
                            yield
                        yield from load_mat(2, e, wd_t[0], ("wd", 0))
                        prog["w_d"] = e + 1

                def load_x(e):
                    DMA("sp", [("xs", e, t) for t in range(2, NT)], ["xsb"], out=xsb[:],
                        in_=xs[e * CAP:(e + 1) * CAP, :].rearrange("(t p) w -> p t w", p=128))
                    if need_ctx:
                        DMA("sp", [("xs", e, t) for t in range(0, 2)], ["xsc"], out=xsc[:],
                            in_=xs[NE * CAP + e * CAPC:NE * CAP + (e + 1) * CAPC, :])

                def transposes(e):
                    xsT = xsT_t[e % 2]
                    xk = ("xsT", e % 2)
                    meta = meta_t[e % 2]
                    mk_ = ("meta", e % 2)
                    I("dve", "tensor_copy", ["xsb"], [mk_], out=meta[:, 0:8, :], in_=xsb[:, :, D:D + 40])
                    if need_ctx:
                        I("dve", "tensor_copy", ["xsc", mk_], [mk_], out=meta[0:CAPC, 8, :], in_=xsc[:, D:D + 40])
                    for st in range(8):
                        pxt, pxtk = pxt_r.next()
                        for kc in range(8):
                            I("pe", "transpose", ["xsb", "ident_b"], [pxtk], out=pxt[:, kc, :],
                              in_=xsb[:, st, kc * 128:(kc + 1) * 128], identity=ident_b[:])
                        if st % 2 == 0:
                            I("act", "copy", [pxtk], [xk], out=xsT[:, :, st * 128:(st + 1) * 128], in_=pxt[:])
                        else:
                            I("dve", "tensor_copy", [pxtk], [xk], out=xsT[:, :, st * 128:(st + 1) * 128], in_=pxt[:])
                        yield
                    if need_ctx:
                        pxt, pxtk = pxt_r.next()
                        for kc in range(8):
                            I("pe", "transpose", ["xsc", "ident_b"], [pxtk], out=pxt[:, kc, 0:CAPC],
                              in_=xsc[:, kc * 128:(kc + 1) * 128], identity=ident_b[0:CAPC, 0:CAPC])
                        I("act", "copy", [pxtk], [xk], out=xsT[:, :, CAP:CAP + CAPC], in_=pxt[:, :, 0:CAPC])
                        yield

                def c_stream():
                    while prog["s_done"] <= 0:
                        yield
                    load_x(0)
                    yield from transposes(0)
                    slabs = [(0, 512), (512, 512)] + ([(CAP, CAPC)] if need_ctx else [])
                    for e in range(NE):
                        xsT = xsT_t[e % 2]
                        xk = ("xsT", e % 2)
                        meta = meta_t[e % 2]
                        mk_ = ("meta", e % 2)
                        prog["c_start"] = e + 1
                        if e + 1 < NE:
                            while prog["s_done"] <= e + 1:
                                yield
                            load_x(e + 1)
                        while prog["w_g"] <= e:
                            yield
                        wg, wgk = wg_t[e % 2], ("wg", e % 2)
                        wu, wuk = wu_t[e % 2], ("wu", e % 2)
                        wd, wdk = wd_t[0], ("wd", 0)
                        for fc in range(8):
                            for (c0, w_) in slabs:
                                pg, pgk = pg_r.next()
                                pu, puk = pu_r.next()
                                for kc in range(8):
                                    I("pe", "matmul", [xk, wgk], [pgk], out=pg[:, 0:w_], lhsT=wg[:, kc, fc * 128:(fc + 1) * 128],
                                      rhs=xsT[:, kc, c0:c0 + w_], start=(kc == 0), stop=(kc == 7))
                                for kc in range(8):
                                    I("pe", "matmul", [xk, wuk], [puk], out=pu[:, 0:w_], lhsT=wu[:, kc, fc * 128:(fc + 1) * 128],
                                      rhs=xsT[:, kc, c0:c0 + w_], start=(kc == 0), stop=(kc == 7))
                                sg, sgk = sg_r.next()
                                I("act", "activation", [pgk], [sgk], out=sg[:, 0:w_], in_=pg[:, 0:w_], func=AF.Silu)
                                I("dve", "tensor_tensor", [sgk, puk], [("actT", fc)], out=actT[:, fc, c0:c0 + w_],
                                  in0=sg[:, 0:w_], in1=pu[:, 0:w_], op=ALU.mult)
                                yield
                        prog["c_gu"] = e + 1
                        if e + 1 < NE:
                            yield from transposes(e + 1)
                        while prog["w_d"] <= e:
                            yield
                        tiles = [(st * 128, 128, st, 0) for st in range(8)]
                        if need_ctx:
                            tiles.append((CAP, CAPC, 8, 1))
                        for (c0, m_, mi, g) in tiles:
                            yt, ytk = yt_r.next()
                            gate_ap = meta[0:m_, mi, 2 * e:2 * e + 2].bitcast(F32)
                            for nh in range(2):
                                cs = slice(nh * 512, (nh + 1) * 512)
                                pyy, pyk = pyy_r.next()
                                for fc in range(8):
                                    I("pe", "matmul", [("actT", fc), wdk], [pyk], out=pyy[0:m_, :],
                                      lhsT=actT[:, fc, c0:c0 + m_], rhs=wd[:, fc, cs], start=(fc == 0), stop=(fc == 7))
                                I("dve", "scalar_tensor_tensor", [pyk, mk_, f"mod5_bc{g}"], [ytk], out=yt[0:m_, cs],
                                  in0=pyy[0:m_, :], scalar=gate_ap, in1=mod5_bc[g][0:m_, cs], op0=ALU.mult, op1=ALU.mult)
                                yield
                            idx_ap = meta[0:m_, mi, 32:34].bitcast(I32)
                            S.dma("pool", lambda en, yt=yt, m_=m_, idx_ap=idx_ap: en.indirect_dma_start(
                                out=xa, out_offset=bass.IndirectOffsetOnAxis(ap=idx_ap, axis=0), in_=yt[0:m_, :],
                                in_offset=None, bounds_check=preg(en, NTOK - 1), oob_is_err=True, compute_op=ALU.add),
                                [ytk, mk_] + [("xa_p", (e - 1) % 2, i_) for i_ in range(9)], [("xa_p", e % 2, mi)])
                            yield
                        prog["c_dn"] = e + 1
                run_streams([s_stream(), w_stream(), c_stream()])
            S.barrier()
            if stop_after == f"p4_{l}":
                return finish(nc, S, top)

    with contextlib.ExitStack() as es:
        fg_bc = sb(es, "fg_bc", [128, D], F32)
        DMA("sp", [], ["fg_bc"], out=fg_bc[:], in_=fg_in.to_broadcast([128, D]))
        xt_r = ring(es, "xt5", 3, [128, D], F32)
        ot_r = ring(es, "ot5", 3, [128, D], F32)
        junk = sb(es, "junk5", [128, D], BF)
        sm_r = ring(es, "sm5", 3, [128, 4], F32)
        for t in range(2, NT):
            xt, xtk = xt_r.next()
            DMA("sp", [], [xtk], out=xt[:], in_=xa[t * 128:(t + 1) * 128, :])
            sm, smk = sm_r.next()
            I("pool", "memset", [], [smk], ap=sm[:, 0:1], constant=0.0)
            I("act", "activation", [xtk, smk], ["junk5", smk], out=junk[:], in_=xt[:], func=AF.Square, accum_out=sm[:, 0:1])
            rstd_chain(sm[:, 0:1], smk, sm[:, 1:2], smk, sm[:, 2:3], smk, 1, 1.0 / D)
            ot, otk = ot_r.next()
            I("dve", "scalar_tensor_tensor", [xtk, smk, "fg_bc"], [otk], out=ot[:], in0=xt[:], scalar=sm[:, 2:3],
              in1=fg_bc[:], op0=ALU.mult, op1=ALU.mult)
            DMA("pool", [otk], [("out", t)], out=out[(t - 2) * 128:(t - 1) * 128, :], in_=ot[:])
    return finish(nc, S, top, True)


def finish(nc, S, top, close_top=False):
    S.barrier()
    S.emit()
    S.close()
    if close_top:
        top.close()
    return nc


Q_PERM = np.concatenate([np.arange(64) + 64 * (j + 4 * half) for j in range(4) for half in range(2)])


def rope_table():
    rows = SEQ // 64
    row = np.repeat(np.arange(rows, dtype=np.float32), 64)
    col = np.tile(np.arange(64, dtype=np.float32), rows)
    n_freq = 16
    inv = (np.float32(10000.0) ** (-np.arange(n_freq, dtype=np.float32) / np.float32(n_freq))).astype(np.float32)
    ang = np.concatenate([row[:, None] * inv, col[:, None] * inv], axis=-1).astype(np.float32)
    return np.concatenate([np.cos(ang), np.sin(ang)], axis=-1).astype(np.float32)


def fm(v):
    v = np.asarray(v)
    return np.ascontiguousarray(np.swapaxes(v.reshape(v.shape[:-1] + (v.shape[-1] // 128, 128)), -1, -2))


def prep_inputs(inp):
    f = lambda a: np.ascontiguousarray(np.asarray(a, dtype=np.float32))
    shared = {}
    shared["w_mod"] = f(inp["w_mod"])
    shared["b_modT"] = f(fm(inp["b_mod"]))
    shared["g1T"] = f(fm(inp["norm1_g"]))
    shared["g2T"] = f(fm(inp["norm2_g"]))
    w_in = np.asarray(inp["w_in"], dtype=np.float32).copy()
    w_in[:, :, 0:512] = w_in[:, :, Q_PERM]
    shared["w_in"] = f(w_in)
    shared["qkg"] = f(np.stack([inp["q_norm_g"], inp["k_norm_g"]], axis=1))
    cw = np.asarray(inp["conv_w"], dtype=np.float32)
    shared["convw"] = f(cw.reshape(2, 3, 2, 128).transpose(0, 3, 2, 1))
    lw = np.asarray(inp["lru_conv_w"], dtype=np.float32)
    lb = np.asarray(inp["lru_conv_b"], dtype=np.float32)
    lcw = np.concatenate([lw, lb[:, None, :]], axis=1)
    shared["lcw"] = f(lcw.reshape(2, 5, 2, 128).transpose(0, 3, 2, 1))
    lv = np.stack([inp["lru_ba"], inp["lru_bi"], inp["lru_lam"]], axis=-1)
    shared["lvec"] = f(np.asarray(lv, dtype=np.float32).reshape(2, 2, 2, 128, 3).transpose(0, 3, 1, 2, 4))
    shared["lwa"] = f(inp["lru_wa"])
    shared["lwi"] = f(inp["lru_wi"])
    w_out = np.asarray(inp["w_out"], dtype=np.float32).copy()
    w_out[:, 0:512, :] = w_out[:, Q_PERM, :]
    shared["w_out"] = f(w_out)
    shared["w_r"] = f(inp["w_router"])
    shared["w_gate"] = f(inp["w_gate"])
    shared["w_up"] = f(inp["w_up"])
    shared["w_down"] = f(inp["w_down"])
    shared["fg"] = f(np.asarray(inp["final_g"]).reshape(1, D))
    shared["rope"] = rope_table()
    cst = np.zeros((128, 3, 128), np.float32)
    cst[:, 0, :] = np.eye(128, dtype=np.float32)
    cst[:, 1, :] = np.triu(np.ones((128, 128), np.float32), 1)
    cst[:, 2, :] = 1.0
    shared["cst"] = cst
    x = np.asarray(inp["x"], dtype=np.float32)
    ctx = np.asarray(inp["ctx"], dtype=np.float32)
    c = np.asarray(inp["c"], dtype=np.float32)
    cc = fm(np.asarray(inp["c_ctx"], dtype=np.float32))
    maps = []
    for b in range(x.shape[0]):
        m = dict(shared)
        m["x"] = np.ascontiguousarray(x[b])
        m["ctx"] = np.ascontiguousarray(ctx[b])
        m["cT"] = f(np.stack([fm(c[b]), cc], axis=-1))
        maps.append(m)
    return maps


_NC_CACHE = {}
DBG = {"units": None, "skip": set()}


def kernel(**inputs):
    maps = prep_inputs(inputs)
    if "nc" not in _NC_CACHE:
        _NC_CACHE["nc"] = build()
    nc = _NC_CACHE["nc"]
    res = run_bass_kernel_spmd(nc, maps, core_ids=list(range(8)))
    return np.stack([np.asarray(r["out"]) for r in res.results], axis=0).astype(np.float32)
```
